# Optimizing a Trainium2 kernel written in Bass

```python
import jax, jax.numpy as jnp
from jax import lax
import numpy as np

D_MODEL = 2048
BATCH = 8
SEQ = 2048
DEPTH = 2

GRID_W = 64
CTX_LEN = 256
N_MIXERS = 2
N_MOD = 6
EPS = 1e-6

SSD_D_INNER = 2 * D_MODEL
SSD_HEAD_DIM = 64
SSD_HEADS = SSD_D_INNER // SSD_HEAD_DIM
SSD_GROUPS = 8
SSD_D_STATE = 128
SSD_CONV_W = 5
SSD_CHUNK = 128
SSD_BC = SSD_GROUPS * SSD_D_STATE
SSD_XBC = SSD_D_INNER + 2 * SSD_BC
SSD_IN = SSD_D_INNER + SSD_XBC + 2 * SSD_HEADS

RET_HEADS = 8
RET_QK_DIM = D_MODEL // RET_HEADS
RET_V_DIM = 2 * RET_QK_DIM
RET_D_V = RET_HEADS * RET_V_DIM
RET_CHUNK = 128
RET_IN = 2 * D_MODEL + 2 * RET_D_V
ROPE_BASE = 10000.0

N_EXPERTS = 16
EC_CAPACITY = 2
D_EXPERT = D_MODEL

N_SSD_LAYERS = (DEPTH + N_MIXERS - 1) // N_MIXERS
N_RET_LAYERS = DEPTH // N_MIXERS

F32 = jnp.float32

kernel_name = "hybrid_ssd_retention_ecmoe_diffusion"


def rmsnorm(x, g):
    xf = x.astype(F32)
    y = xf * lax.rsqrt(jnp.mean(xf * xf, axis=-1, keepdims=True) + EPS)
    return (y * g.astype(F32)).astype(x.dtype)


def flip(t):
    return jnp.flip(t, axis=1)


def dwconv_centred(u, w, b):
    pad = w.shape[0] // 2
    y = lax.conv_general_dilated(u, w[:, None, :].astype(u.dtype), window_strides=(1,), padding=[(pad, pad)],
                                 dimension_numbers=('NWC', 'WIO', 'NWC'), feature_group_count=u.shape[-1])
    return y + b.astype(u.dtype)


def to_chunks(t, q):
    b, l = t.shape[:2]
    return jnp.moveaxis(t.reshape(b, l // q, q, *t.shape[2:]), 1, 0)


def from_chunks(t):
    nc, b, q = t.shape[:3]
    return jnp.moveaxis(t, 0, 1).reshape(b, nc * q, *t.shape[3:])


def ssd_final_state(x, dt, A, bm):
    bsz, L, H, P = x.shape
    G, N = bm.shape[2:]
    cs = jnp.cumsum(dt * A, axis=1)
    w = (jnp.exp(cs[:, -1:] - cs) * dt).reshape(bsz, L, G, H // G)
    h = jnp.einsum('blgn,blge,blgep->bgepn', bm.astype(F32), w, x.astype(F32).reshape(bsz, L, G, H // G, P))
    return h.reshape(bsz, H, P, N)


def ssd_scan(x, dt, A, bm, cm, h0):
    bsz, L, H, P = x.shape
    G, N = bm.shape[2:]
    E = H // G
    q = SSD_CHUNK
    mask = jnp.tril(jnp.ones((q, q), dtype=bool))[None, :, :, None, None]
    a_ge = A.reshape(G, E)
    xdt = (x.astype(F32) * dt[..., None]).reshape(bsz, L, G, E, P)

    def step(h, inp):
        xc, dtc, bc, cc = inp
        cs = jnp.cumsum(dtc * a_ge, axis=1)
        seg = jnp.exp(jnp.where(mask, cs[:, :, None] - cs[:, None, :], -jnp.inf))
        cb = jnp.einsum('blgn,bsgn->blsg', cc, bc)
        y = (jnp.einsum('blsg,blsge,bsgep->blgep', cb, seg, xc)
             + jnp.einsum('blgn,bgepn,blge->blgep', cc, h, jnp.exp(cs)))
        h = (h * jnp.exp(cs[:, -1])[..., None, None]
             + jnp.einsum('bsgn,bsge,bsgep->bgepn', bc, jnp.exp(cs[:, -1:] - cs), xc))
        return h, y

    _, y = lax.scan(step, h0.reshape(bsz, G, E, P, N),
                    (to_chunks(xdt, q), to_chunks(dt.reshape(bsz, L, G, E), q),
                     to_chunks(bm.astype(F32), q), to_chunks(cm.astype(F32), q)))
    return from_chunks(y).reshape(bsz, L, H, P)


def ssd_mixer(a_l, a_c, w_in, conv_w, conv_b, dt_bias, a_log, d_skip, norm_g, w_out, need_ctx):
    A = -jnp.exp(a_log.astype(F32))

    def streams(p):
        bsz, L = p.shape[:2]
        xbc = jax.nn.silu(dwconv_centred(p[..., :SSD_XBC], conv_w, conv_b))
        xs = xbc[..., :SSD_D_INNER].reshape(bsz, L, SSD_HEADS, SSD_HEAD_DIM)
        bm = xbc[..., SSD_D_INNER:SSD_D_INNER + SSD_BC].reshape(bsz, L, SSD_GROUPS, SSD_D_STATE)
        cm = xbc[..., SSD_D_INNER + SSD_BC:].reshape(bsz, L, SSD_GROUPS, SSD_D_STATE)
        dt = jax.nn.softplus(p[..., SSD_XBC:].astype(F32).reshape(bsz, L, 2, SSD_HEADS) + dt_bias.astype(F32))
        return xs, bm, cm, dt

    def output(y, xs, z):
        bsz, L = y.shape[:2]
        y = (y + d_skip.astype(F32)[:, None] * xs.astype(F32)).astype(z.dtype).reshape(bsz, L, SSD_D_INNER)
        return rmsnorm(y * jax.nn.silu(z), norm_g) @ w_out

    def bidir(xs, bm, cm, dt, s_f, s_b):
        return (ssd_scan(xs, dt[:, :, 0], A[0], bm, cm, s_f)
                + flip(ssd_scan(flip(xs), flip(dt[:, :, 1]), A[1], flip(bm), flip(cm), s_b)))

    p_l = a_l @ w_in
    z_l = p_l[..., :SSD_D_INNER]
    x_l, b_l, c_l, dt_l = streams(p_l[..., SSD_D_INNER:])
    x_c, b_c, c_c, dt_c = streams(a_c @ w_in[:, SSD_D_INNER:])
    s_f = ssd_final_state(x_c, dt_c[:, :, 0], A[0], b_c)
    s_b = ssd_final_state(flip(x_c), flip(dt_c[:, :, 1]), A[1], flip(b_c))
    out_l = output(bidir(x_l, b_l, c_l, dt_l, s_f, s_b), x_l, z_l)
    out_c = None
    if need_ctx:
        z_c = a_c @ w_in[:, :SSD_D_INNER]
        zero = jnp.zeros_like(s_f)
        out_c = output(bidir(x_c, b_c, c_c, dt_c, zero, zero), x_c, z_c)
    return out_l, out_c


def rope_axis(u, pos):
    half = u.shape[-1] // 2
    freqs = ROPE_BASE ** (-jnp.arange(half, dtype=F32) / half)
    ang = pos.astype(F32)[:, None] * freqs[None, :]
    cos = jnp.cos(ang)[None, :, None, :]
    sin = jnp.sin(ang)[None, :, None, :]
    u1, u2 = u[..., :half], u[..., half:]
    return jnp.concatenate([u1 * cos - u2 * sin, u1 * sin + u2 * cos], axis=-1).astype(u.dtype)


def rope_2d(u, row_ids, col_ids):
    d2 = u.shape[-1] // 2
    return jnp.concatenate([rope_axis(u[..., :d2], row_ids), rope_axis(u[..., d2:], col_ids)], axis=-1)


def retention_final_state(k, v, log_decay):
    l = k.shape[1]
    w = jnp.exp((l - 1.0 - jnp.arange(l, dtype=F32))[None, :] * log_decay[:, None])
    return jnp.einsum('blhd,hl,blhv->bhdv', k.astype(F32), w, v.astype(F32))


def retention_scan(q, k, v, log_decay, s0):
    qn = RET_CHUNK
    pos = jnp.arange(qn, dtype=F32)
    rel = pos[:, None] - pos[None, :]
    decay_in = jnp.exp(jnp.where(rel[None] >= 0, rel[None] * log_decay[:, None, None], -jnp.inf))
    decay_from_state = jnp.exp((pos[:, None] + 1.0) * log_decay[None, :])
    decay_to_end = jnp.exp((qn - 1.0 - pos)[None, :] * log_decay[:, None])
    decay_chunk = jnp.exp(qn * log_decay)

    def step(s, inp):
        qc, kc, vc = inp
        scores = jnp.einsum('blhd,bshd->bhls', qc, kc) * decay_in
        o = (jnp.einsum('bhls,bshv->blhv', scores, vc)
             + jnp.einsum('blhd,bhdv->blhv', qc, s) * decay_from_state[None, :, :, None])
        s = s * decay_chunk[None, :, None, None] + jnp.einsum('bshd,hs,bshv->bhdv', kc, decay_to_end, vc)
        return s, o

    _, o = lax.scan(step, s0, (to_chunks(q.astype(F32), qn), to_chunks(k.astype(F32), qn),
                               to_chunks(v.astype(F32), qn)))
    return from_chunks(o)


def retention_output(o, g, gn_g, w_out):
    b, n = o.shape[:2]
    mu = jnp.mean(o, axis=-1, keepdims=True)
    var = jnp.mean(jnp.square(o - mu), axis=-1, keepdims=True)
    on = ((o - mu) * lax.rsqrt(var + EPS)).reshape(b, n, RET_D_V) * gn_g.astype(F32)
    return (on.astype(g.dtype) * jax.nn.silu(g)) @ w_out


def retention_mixer(a_l, a_c, row_ids, col_ids, w_in, decay_param, gn_g, w_out, need_ctx):
    b, n, _ = a_l.shape
    m = a_c.shape[1]
    D = D_MODEL
    ld = -jnp.exp(decay_param.astype(F32))
    scale = RET_QK_DIM ** -0.5

    def heads(t, L, dh):
        return t.reshape(b, L, RET_HEADS, dh)

    def bidir(qq, kk, vv, s_f, s_b):
        return (retention_scan(qq, kk, vv, ld[0], s_f)
                + flip(retention_scan(flip(qq), flip(kk), flip(vv), ld[1], s_b)))

    p_l = a_l @ w_in
    q_l = rope_2d(heads(p_l[..., :D], n, RET_QK_DIM), row_ids, col_ids)
    k_l = rope_2d(heads(p_l[..., D:2 * D], n, RET_QK_DIM), row_ids, col_ids) * scale
    v_l = heads(p_l[..., 2 * D:2 * D + RET_D_V], n, RET_V_DIM)
    g_l = p_l[..., 2 * D + RET_D_V:]
    p_c = a_c @ w_in[:, D:2 * D + RET_D_V]
    k_c = heads(p_c[..., :D], m, RET_QK_DIM) * scale
    v_c = heads(p_c[..., D:], m, RET_V_DIM)
    s_f = retention_final_state(k_c, v_c, ld[0])
    s_b = retention_final_state(flip(k_c), flip(v_c), ld[1])
    y_l = retention_output(bidir(q_l, k_l, v_l, s_f, s_b), g_l, gn_g, w_out)
    y_c = None
    if need_ctx:
        q_c = heads(a_c @ w_in[:, :D], m, RET_QK_DIM)
        g_c = a_c @ w_in[:, 2 * D + RET_D_V:]
        zero = jnp.zeros_like(s_f)
        y_c = retention_output(bidir(q_c, k_c, v_c, zero, zero), g_c, gn_g, w_out)
    return y_l, y_c


def ec_moe(h, w_router, w_gate, w_up, w_down):
    b, n, d = h.shape
    cap = EC_CAPACITY * n // N_EXPERTS
    aff = jax.nn.softmax(jnp.einsum('bnd,de->bne', h, w_router).astype(F32), axis=-1)
    gate, idx = lax.top_k(jnp.swapaxes(aff, 1, 2), cap)
    xe = jax.vmap(lambda hb, ib: hb[ib])(h, idx)
    hid = jax.nn.silu(jnp.einsum('becd,edf->becf', xe, w_gate)) * jnp.einsum('becd,edf->becf', xe, w_up)
    ye = jnp.einsum('becf,efd->becd', hid, w_down) * gate[..., None].astype(h.dtype)
    return jax.vmap(lambda yb, ib: jnp.zeros((n, d), h.dtype).at[ib.reshape(-1)].add(yb.reshape(-1, d)))(ye, idx)


def setup_inputs(seed: int = 0) -> dict:
    key = jax.random.key(seed)
    ks = jax.random.split(key, 26)
    D = D_MODEL

    def nrm(k, shape, fan):
        return jax.random.normal(k, shape, F32) * fan ** -0.5

    def gain(k, shape):
        return 1.0 + 0.01 * jax.random.normal(k, shape, F32)

    def small(k, shape):
        return 0.01 * jax.random.normal(k, shape, F32)

    x = jax.random.normal(ks[0], (BATCH, SEQ, D), F32)
    c = jax.random.normal(ks[1], (BATCH, D), F32)
    ctx = jax.random.normal(ks[2], (BATCH, CTX_LEN, D), F32)
    c_ctx = jax.random.normal(ks[3], (D,), F32)
    ada_w = nrm(ks[4], (DEPTH, D, N_MOD * D), D)
    ada_b = small(ks[5], (DEPTH, N_MOD * D))
    norm_mix_g = gain(ks[6], (DEPTH, D))
    norm_ffn_g = gain(ks[7], (DEPTH, D))
    ssd_w_in = nrm(ks[8], (N_SSD_LAYERS, D, SSD_IN), D)
    ssd_conv_w = nrm(ks[9], (N_SSD_LAYERS, SSD_CONV_W, SSD_XBC), SSD_CONV_W)
    ssd_conv_b = small(ks[10], (N_SSD_LAYERS, SSD_XBC))
    dt0 = jnp.exp(jax.random.uniform(ks[11], (N_SSD_LAYERS, 2, SSD_HEADS), F32, np.log(1e-3), np.log(1e-1)))
    ssd_dt_bias = dt0 + jnp.log(-jnp.expm1(-dt0))
    ssd_a_log = jnp.log(jax.random.uniform(ks[12], (N_SSD_LAYERS, 2, SSD_HEADS), F32, 1.0, 16.0))
    ssd_d = gain(ks[13], (N_SSD_LAYERS, SSD_HEADS))
    ssd_norm_g = gain(ks[14], (N_SSD_LAYERS, SSD_D_INNER))
    ssd_w_out = nrm(ks[15], (N_SSD_LAYERS, SSD_D_INNER, D), SSD_D_INNER)
    ret_w_in = nrm(ks[16], (N_RET_LAYERS, D, RET_IN), D)
    base = np.log(-np.log(1.0 - 2.0 ** (-5.0 - np.arange(RET_HEADS)))).astype(np.float32)
    ret_decay = jnp.asarray(base) + 0.05 * jax.random.normal(ks[17], (N_RET_LAYERS, 2, RET_HEADS), F32)
    ret_gn_g = gain(ks[18], (N_RET_LAYERS, RET_D_V))
    ret_w_out = nrm(ks[19], (N_RET_LAYERS, RET_D_V, D), RET_D_V)
    moe_w_router = nrm(ks[20], (DEPTH, D, N_EXPERTS), D)
    moe_w_gate = nrm(ks[21], (DEPTH, N_EXPERTS, D, D_EXPERT), D)
    moe_w_up = nrm(ks[22], (DEPTH, N_EXPERTS, D, D_EXPERT), D)
    moe_w_down = nrm(ks[23], (DEPTH, N_EXPERTS, D_EXPERT, D), D_EXPERT)
    final_norm_g = gain(ks[24], (D,))
    return {"x": x, "c": c, "ctx": ctx, "c_ctx": c_ctx, "ada_w": ada_w, "ada_b": ada_b,
            "norm_mix_g": norm_mix_g, "norm_ffn_g": norm_ffn_g,
            "ssd_w_in": ssd_w_in, "ssd_conv_w": ssd_conv_w, "ssd_conv_b": ssd_conv_b,
            "ssd_dt_bias": ssd_dt_bias, "ssd_a_log": ssd_a_log, "ssd_d": ssd_d,
            "ssd_norm_g": ssd_norm_g, "ssd_w_out": ssd_w_out,
            "ret_w_in": ret_w_in, "ret_decay": ret_decay, "ret_gn_g": ret_gn_g, "ret_w_out": ret_w_out,
            "moe_w_router": moe_w_router, "moe_w_gate": moe_w_gate, "moe_w_up": moe_w_up,
            "moe_w_down": moe_w_down, "final_norm_g": final_norm_g}


def reference(x, c, ctx, c_ctx, ada_w, ada_b, norm_mix_g, norm_ffn_g,
              ssd_w_in, ssd_conv_w, ssd_conv_b, ssd_dt_bias, ssd_a_log, ssd_d, ssd_norm_g, ssd_w_out,
              ret_w_in, ret_decay, ret_gn_g, ret_w_out,
              moe_w_router, moe_w_gate, moe_w_up, moe_w_down, final_norm_g):
    n_lat = x.shape[1]
    ROWS = n_lat // GRID_W
    row_ids = jnp.repeat(jnp.arange(ROWS), GRID_W)
    col_ids = jnp.tile(jnp.arange(GRID_W), ROWS)
    silu_c = jax.nn.silu(c)
    silu_cc = jax.nn.silu(c_ctx)
    h_lat, h_ctx = x, ctx
    for i in range(DEPTH):
        need_ctx = i < DEPTH - 1
        j = i // N_MIXERS
        sh1, sc1, g1, sh2, sc2, g2 = jnp.split((silu_c @ ada_w[i] + ada_b[i])[:, None, :], N_MOD, axis=-1)
        csh1, csc1, cg1, csh2, csc2, cg2 = jnp.split(silu_cc @ ada_w[i] + ada_b[i], N_MOD, axis=-1)
        a_l = rmsnorm(h_lat, norm_mix_g[i]) * (1.0 + sc1) + sh1
        a_c = rmsnorm(h_ctx, norm_mix_g[i]) * (1.0 + csc1) + csh1
        if i % N_MIXERS == 0:
            y_l, y_c = ssd_mixer(a_l, a_c, ssd_w_in[j], ssd_conv_w[j], ssd_conv_b[j], ssd_dt_bias[j],
                                 ssd_a_log[j], ssd_d[j], ssd_norm_g[j], ssd_w_out[j], need_ctx)
        else:
            y_l, y_c = retention_mixer(a_l, a_c, row_ids, col_ids, ret_w_in[j], ret_decay[j],
                                       ret_gn_g[j], ret_w_out[j], need_ctx)
        h_lat = h_lat + g1 * y_l
        f_l = rmsnorm(h_lat, norm_ffn_g[i]) * (1.0 + sc2) + sh2
        h_lat = h_lat + g2 * ec_moe(f_l, moe_w_router[i], moe_w_gate[i], moe_w_up[i], moe_w_down[i])
        if need_ctx:
            h_ctx = h_ctx + cg1 * y_c
            f_c = rmsnorm(h_ctx, norm_ffn_g[i]) * (1.0 + csc2) + csh2
            h_ctx = h_ctx + cg2 * ec_moe(f_c, moe_w_router[i], moe_w_gate[i], moe_w_up[i], moe_w_down[i])
    return rmsnorm(h_lat, final_norm_g)
```

```python
from contextlib import ExitStack
import numpy as np
import concourse.bass as bass
import concourse.mybir as mybir
from concourse.bass_utils import run_bass_kernel_spmd

F32 = mybir.dt.float32
BF16 = mybir.dt.bfloat16
AF = mybir.ActivationFunctionType
ALU = mybir.AluOpType
AX = mybir.AxisListType

D = 2048
L = 2048
M = 256
T = L + M
NT = T // 128
NM = 6 * D
EPS = 1e-6
ENG = ("sp", "act", "pool", "dve", "pe")
SAME_ENG_SYNC = True


class Prog:
    def __init__(self, nc, es, n_dma_sems=6):
        self.nc = nc
        self.streams = {e: [] for e in ENG}
        self.sem = {e: es.enter_context(nc.semaphore("s_" + e)) for e in ENG}
        self.cnt = {e: 0 for e in ENG}
        self.dsem = {e: [es.enter_context(nc.semaphore(f"d_{e}{i}")) for i in range(n_dma_sems)]
                     for e in ("sp", "act", "pool")}
        self.dcnt = {e: [0] * n_dma_sems for e in ("sp", "act", "pool")}
        self.drr = {e: 0 for e in ("sp", "act", "pool")}
        self.waited = {}
        self.lastw = {}
        self.readers = {}
        self.ninstr = 0
        self.psn = 0
        self.nrot = 8

    def _wait(self, eng, tok):
        if tok is None:
            return
        if tok[0] == "e":
            _, src, val = tok
            if src == eng and (eng == "pe" or not SAME_ENG_SYNC):
                return
            k = (eng, src)
            sem = self.sem[src]
        else:
            _, src, idx, val = tok
            k = (eng, src, idx)
            sem = self.dsem[src][idx]
        if self.waited.get(k, 0) >= val:
            return
        self.waited[k] = val
        self.streams[eng].append(lambda h, sem=sem, val=val: h.wait_ge(sem, val))
        self.ninstr += 1

    def _deps(self, eng, r, w):
        for k in r:
            self._wait(eng, self.lastw.get(k))
        for k in w:
            self._wait(eng, self.lastw.get(k))
            for t in self.readers.get(k, ()):
                self._wait(eng, t)

    def _update(self, tok, r, w):
        for k in r:
            lst = self.readers.setdefault(k, [])
            lst.append(tok)
            if len(lst) > 16:
                d = {}
                for t in lst:
                    kk = t[:-1]
                    if kk not in d or d[kk][-1] < t[-1]:
                        d[kk] = t
                self.readers[k] = list(d.values())
        for k in w:
            self.lastw[k] = tok
            self.readers[k] = []

    def op(self, eng, fn, r=(), w=(), inc=True):
        pr = [x for x in r if isinstance(x, tuple) and x[0] == "ps"]
        if pr:
            r = [x for x in r if x not in pr]
            w = list(w) + pr
        self._deps(eng, r, w)
        if inc:
            self.cnt[eng] += 1
            sem = self.sem[eng]
            self.streams[eng].append(lambda h, fn=fn, sem=sem: fn(h).then_inc(sem, 1))
            tok = ("e", eng, self.cnt[eng])
        else:
            self.streams[eng].append(lambda h, fn=fn: fn(h))
            tok = ("e", eng, self.cnt[eng] + 1)
        self.ninstr += 1
        self._update(tok, r, w)
        return tok

    def dma(self, eng, out, in_, r=(), w=(), **kw):
        i = self.drr[eng]
        self.drr[eng] = (i + 1) % len(self.dsem[eng])
        prev = self.dcnt[eng][i]
        if prev:
            self._wait(eng, ("d", eng, i, prev))
        self._deps(eng, r, w)
        self.dcnt[eng][i] += 16
        sem = self.dsem[eng][i]
        self.streams[eng].append(
            lambda h, out=out, in_=in_, sem=sem, kw=kw: h.dma_start(out=out, in_=in_, **kw).then_inc(sem, 16))
        self.ninstr += 1
        tok = ("d", eng, i, self.dcnt[eng][i])
        self._update(tok, r, w)
        return tok

    def wait_all(self, eng):
        for e in ENG:
            if self.cnt[e]:
                self._wait(eng, ("e", e, self.cnt[e]))
        for e in ("sp", "act", "pool"):
            for i, v in enumerate(self.dcnt[e]):
                if v:
                    self._wait(eng, ("d", e, i, v))

    def barrier(self):
        for e in ENG:
            self.wait_all(e)

    def bank(self):
        i = self.psn % self.nrot
        self.psn = (i + 1) % self.nrot
        return i

    def run(self):
        with self.nc.Block() as block:
            for e, name in (("sp", "sync"), ("act", "scalar"), ("pool", "gpsimd"), ("dve", "vector"), ("pe", "tensor")):
                lst = self.streams[e]

                def body(h, lst=lst):
                    for f in lst:
                        f(h)
                getattr(block, name)(body)


class K:
    pass


_SBN = [0]


def sb(nc, es, name, shape, dt):
    _SBN[0] += 1
    return es.enter_context(nc.sbuf_tensor(f"{name}_{_SBN[0]}", shape, dt))


def phase_mod(k, layer):
    nc, P = k.nc, k.P
    with ExitStack() as es:
        cT = sb(nc, es, "cT", [128, 16, 2], F32)
        sT = sb(nc, es, "sT", [128, 16, 2], F32)
        wt = [sb(nc, es, f"mwt{i}", [128, 16, 512], F32) for i in range(2)]
        bt = [sb(nc, es, f"mbt{i}", [2, 512], F32) for i in range(2)]
        ot = [sb(nc, es, f"mot{i}", [2, 512], F32) for i in range(2)]
        P.dma("sp", cT[:, :, 0], k.c.rearrange("(k p) -> p k", p=128), w=["cT"], allow_slow_non_contiguous=True)
        P.dma("sp", cT[:, :, 1], k.c_ctx.rearrange("(k p) -> p k", p=128), w=["cT"], allow_slow_non_contiguous=True)
        P.op("act", lambda h: h.activation(out=sT[:], in_=cT[:], func=AF.Silu), r=["cT"], w=["sT"])
        wv = k.ada_w[layer].rearrange("(k p) n -> p k n", p=128)
        for j in range(NM // 512):
            i = j % 2
            pb = P.bank()
            ps = k.ps[pb]
            P.dma("sp" if j % 2 == 0 else "act", wt[i][:], wv[:, :, j * 512:(j + 1) * 512], w=[("mwt", i)])
            P.dma("sp", bt[i][:], k.ada_b[layer, j * 512:(j + 1) * 512].partition_broadcast(2), w=[("mbt", i)])
            for kk in range(16):
                P.op("pe", lambda h, kk=kk, i=i, ps=ps: h.matmul(ps[0:2, :], lhsT=sT[:, kk, :], rhs=wt[i][:, kk, :],
                                                                 start=(kk == 0), stop=(kk == 15)),
                     r=["sT", ("mwt", i)], w=[("ps", pb)], inc=(kk == 15))
            P.op("dve", lambda h, i=i, ps=ps: h.tensor_tensor(out=ot[i][:], in0=ps[0:2, :], in1=bt[i][:], op=ALU.add),
                 r=[("ps", pb), ("mbt", i)], w=[("mot", i)])
            P.dma("sp", k.mod[layer, :, j * 512:(j + 1) * 512], ot[i][:], r=[("mot", i)], w=[("mod", layer)])


def load_bc(k, es, name, src_ap, n, eng="sp", key=None):
    t = sb(k.nc, es, name, [128, n], F32)
    k.P.dma(eng, t[:], src_ap.partition_broadcast(128), r=[key] if key else [], w=[name])
    return t


def phase_norm_T(k, layer, src_lat, src_ctx, gvec, sc_i, sh_i, aT, f_tok=None, f32T=None):
    nc, P = k.nc, k.P
    with ExitStack() as es:
        hb = [sb(nc, es, f"hb{i}", [128, D], F32) for i in range(2)]
        tmp = [sb(nc, es, f"ntmp{i}", [128, D], F32) for i in range(2)]
        ab = [sb(nc, es, f"ab{i}", [128, D], BF16) for i in range(2)]
        st = sb(nc, es, "nst", [128, NT, 4], F32)
        gb = load_bc(k, es, "gb", gvec, D)
        for r, (src, t0, nt) in enumerate(((src_lat, 0, L // 128), (src_ctx, L // 128, M // 128))):
            if src is None:
                continue
            scb = load_bc(k, es, f"scb{r}", k.mod[layer, r, sc_i * D:(sc_i + 1) * D], D, key=("mod", layer))
            shb = load_bc(k, es, f"shb{r}", k.mod[layer, r, sh_i * D:(sh_i + 1) * D], D, key=("mod", layer))
            gm = sb(nc, es, f"gm{r}", [128, D], F32)
            P.op("dve", lambda h, gm=gm, scb=scb: h.scalar_tensor_tensor(out=gm[:], in0=scb[:], scalar=1.0, in1=gb[:],
                                                                          op0=ALU.add, op1=ALU.mult),
                 r=[f"scb{r}", "gb"], w=[f"gm{r}"])
            for tt in range(nt):
                tg = t0 + tt
                i = tg % 2
                P.dma("sp", hb[i][:], src[tt * 128:(tt + 1) * 128, :], w=[("hb", i)])
                P.op("act", lambda h, i=i: h.activation(out=tmp[i][:], in_=hb[i][:], func=AF.Square),
                     r=[("hb", i)], w=[("ntmp", i)])
                P.op("dve", lambda h, i=i, tg=tg: h.tensor_reduce(out=st[:, tg, 0:1], in_=tmp[i][:], axis=AX.X, op=ALU.add),
                     r=[("ntmp", i)], w=[("nst", tg)])
                P.op("dve", lambda h, tg=tg: h.tensor_scalar(out=st[:, tg, 1:2], in0=st[:, tg, 0:1], scalar1=1.0 / D, scalar2=EPS,
                                                             op0=ALU.mult, op1=ALU.add), r=[("nst", tg)], w=[("nst", tg)])
                P.op("act", lambda h, tg=tg: h.activation(out=st[:, tg, 2:3], in_=st[:, tg, 1:2], func=AF.Sqrt),
                     r=[("nst", tg)], w=[("nst", tg)])
                P.op("dve", lambda h, tg=tg: h.reciprocal(out=st[:, tg, 3:4], in_=st[:, tg, 2:3]), r=[("nst", tg)], w=[("nst", tg)])
                P.op("dve", lambda h, i=i, tg=tg, gm=gm: h.scalar_tensor_tensor(out=tmp[i][:], in0=hb[i][:], scalar=st[:, tg, 3:4],
                                                                               in1=gm[:], op0=ALU.mult, op1=ALU.mult),
                     r=[("hb", i), ("nst", tg), f"gm{r}"], w=[("ntmp", i)])
                if f32T is None:
                    P.op("pool", lambda h, i=i, shb=shb: h.tensor_tensor(out=ab[i][:], in0=tmp[i][:], in1=shb[:], op=ALU.add),
                         r=[("ntmp", i), f"shb{r}"], w=[("ab", i)])
                    for half in range(2):
                        pb = P.bank()
                        psb = k.ps[pb][:].bitcast(BF16)
                        for j in range(8):
                            kk = half * 8 + j
                            P.op("pe", lambda h, i=i, j=j, kk=kk, psb=psb: h.transpose(out=psb[:, j * 128:(j + 1) * 128],
                                                                                     in_=ab[i][:, kk * 128:(kk + 1) * 128],
                                                                                     identity=k.ident_bf[:]),
                                 r=[("ab", i), "ident"], w=[("ps", pb)], inc=(j == 7))
                        P.op("act", lambda h, half=half, tg=tg, psb=psb: h.activation(
                            out=aT[:, half * 8:(half + 1) * 8, tg * 128:(tg + 1) * 128],
                            in_=psb.rearrange("p (j t) -> p j t", j=8), func=AF.Copy),
                            r=[("ps", pb)], w=[("aT", tg)])
                    if f_tok is not None:
                        P.op("pool", lambda h, i=i, tg=tg: h.tensor_copy(out=f_tok[:, tg, :], in_=ab[i][:]),
                             r=[("ab", i)], w=[("ftok", tg)])


def build_consts(k, es):
    nc, P = k.nc, k.P
    k.ident_f = sb(nc, es, "ident_f", [128, 128], F32)
    k.ident_bf = sb(nc, es, "ident_bf", [128, 128], BF16)
    P.dma("sp", k.ident_f[:], k.c_ident, w=["ident_f"])
    P.op("dve", lambda h: h.tensor_copy(out=k.ident_bf[:], in_=k.ident_f[:]), r=["ident_f"], w=["ident"])


XB = 4096
UW = T + 8


def phase_ssd_inproj(k, es_out, aT):
    nc, P = k.nc, k.P
    if es_out is None:
        dt_tok, dtA_tok = k.dt_pre
    else:
        dt_tok = sb(nc, es_out, "dt_tok", [128, NT, 128], F32)
        dtA_tok = sb(nc, es_out, "dtA_tok", [128, NT, 128], F32)
    wv = k.ssd_w_in.rearrange("(k p) n -> p k n", p=128)
    with ExitStack() as es:
        cwT = sb(nc, es, "cwT", [128, 48, 6], F32)
        with ExitStack() as es2:
            cw6 = sb(nc, es2, "cw6", [6, 6144], F32)
            P.dma("sp", cw6[0:5, :], k.ssd_conv_w, w=["cw6"])
            P.dma("sp", cw6[5:6, :], k.ssd_conv_b.partition_broadcast(1), w=["cw6"])
            pb = P.bank()
            for j in range(48):
                P.op("pe", lambda h, j=j, pb=pb: h.transpose(out=k.ps[pb][:, j * 6:(j + 1) * 6], in_=cw6[:, j * 128:(j + 1) * 128],
                                                            identity=k.ident_f[0:6, 0:6]),
                     r=["cw6", "ident_f"], w=[("ps", pb)], inc=(j == 47))
            P.op("dve", lambda h, pb=pb: h.tensor_copy(out=cwT[:], in_=k.ps[pb][:, 0:288].rearrange("p (j s) -> p j s", s=6)),
                 r=[("ps", pb)], w=["cwT"])
            P.barrier()
        wt = [sb(nc, es, f"wt{i}", [128, 16, 512], BF16) for i in range(2)]
        U = [sb(nc, es, f"U{i}", [128, UW], F32) for i in range(2)]
        acc = [sb(nc, es, f"acc{i}", [128, T], F32) for i in range(2)]
        S = [sb(nc, es, f"S{i}", [128, T], BF16) for i in range(2)]
        stage = [sb(nc, es, f"stg{i}", [128, NT, 512], BF16) for i in range(2)]
        one = sb(nc, es, "onec", [128, 1], F32)
        P.op("pool", lambda h: h.memset(one[:], 1.0), w=["onec"])
        for i in range(2):
            P.op("pool", lambda h, i=i: h.memset(U[i][:], 0.0), w=[("U", i)])
        dtb = load_bc(k, es, "dtb", k.ssd_dt_bias.rearrange("a b -> (a b)"), 128)
        alog = load_bc(k, es, "alog", k.ssd_a_log.rearrange("a b -> (a b)"), 128)
        Abc = sb(nc, es, "Abc", [128, 128], F32)
        P.op("act", lambda h: h.activation(out=Abc[:], in_=alog[:], func=AF.Exp), r=["alog"], w=["Abc"])
        P.op("dve", lambda h: h.tensor_scalar(out=Abc[:], in0=Abc[:], scalar1=-1.0, scalar2=None, op0=ALU.mult), r=["Abc"], w=["Abc"])

        nw = [0]

        def load_w(c0, ncols=512):
            i = nw[0] % 2
            nw[0] += 1
            P.dma("pool", wt[i][:, :, 0:ncols], wv[:, :, c0:c0 + ncols], w=[("wt", i)])
            return i

        tok_blocks = [(tb * 512, 512) for tb in range(4)] + [(L, M)]
        u_off = lambda t0: (2 + t0) if t0 < L else (L + 6 + (t0 - L))
        nblk = 0
        for ti in range(12):
            wi = load_w(XB + ti * 512)
            si = ti % 2
            for jb in range(4):
                cb = ti * 4 + jb
                ui = nblk % 2
                nblk += 1
                for (t0, n) in tok_blocks:
                    pb = P.bank()
                    for kk in range(16):
                        P.op("pe", lambda h, kk=kk, wi=wi, jb=jb, t0=t0, n=n, pb=pb: h.matmul(
                            k.ps[pb][:, 0:n], lhsT=wt[wi][:, kk, jb * 128:(jb + 1) * 128], rhs=aT[:, kk, t0:t0 + n],
                            start=(kk == 0), stop=(kk == 15)),
                            r=[("wt", wi)] + [("aT", t0 // 128 + q) for q in range(n // 128)], w=[("ps", pb)], inc=(kk == 15))
                    P.op("act", lambda h, ui=ui, t0=t0, n=n, pb=pb: h.activation(out=U[ui][:, u_off(t0):u_off(t0) + n],
                                                                               in_=k.ps[pb][:, 0:n], func=AF.Copy),
                         r=[("ps", pb)], w=[("U", ui)])
                for (a0, u0, n) in ((0, 0, L), (L, L + 4, M)):
                    P.op("dve", lambda h, ui=ui, cb=cb, a0=a0, u0=u0, n=n: h.tensor_scalar(
                        out=acc[ui][:, a0:a0 + n], in0=U[ui][:, u0:u0 + n], scalar1=cwT[:, cb, 0:1], scalar2=cwT[:, cb, 5:6],
                        op0=ALU.mult, op1=ALU.add), r=[("U", ui), "cwT"], w=[("acc", ui)])
                    for tap in range(1, 5):
                        P.op("dve", lambda h, ui=ui, cb=cb, a0=a0, u0=u0, n=n, tap=tap: h.scalar_tensor_tensor(
                            out=acc[ui][:, a0:a0 + n], in0=U[ui][:, u0 + tap:u0 + tap + n], scalar=cwT[:, cb, tap:tap + 1],
                            in1=acc[ui][:, a0:a0 + n], op0=ALU.mult, op1=ALU.add), r=[("U", ui), "cwT", ("acc", ui)], w=[("acc", ui)])
                P.op("act", lambda h, ui=ui: h.activation(out=S[ui][:], in_=acc[ui][:], func=AF.Silu),
                     r=[("acc", ui)], w=[("S", ui)])
                if cb < 40:
                    for t8 in range(0, NT, 8):
                        nn = min(8, NT - t8)
                        pb = P.bank()
                        psb = k.ps[pb][:].bitcast(BF16)
                        for j in range(nn):
                            P.op("pe", lambda h, ui=ui, j=j, t8=t8, psb=psb: h.transpose(
                                out=psb[:, j * 128:(j + 1) * 128], in_=S[ui][:, (t8 + j) * 128:(t8 + j + 1) * 128],
                                identity=k.ident_bf[:]), r=[("S", ui), "ident"], w=[("ps", pb)], inc=(j == nn - 1))
                        P.op("dve", lambda h, si=si, jb=jb, t8=t8, nn=nn, psb=psb: h.tensor_copy(
                            out=stage[si][:, t8:t8 + nn, jb * 128:(jb + 1) * 128],
                            in_=psb[:, 0:nn * 128].rearrange("p (j c) -> p j c", c=128)),
                            r=[("ps", pb)], w=[("stg", si)])
                if 32 <= cb < 40:
                    P.dma("sp", k.bT_s[cb - 32], S[ui][:], r=[("S", ui)], w=["bT_s"])
                if cb >= 40:
                    P.dma("sp", k.cT_s[cb - 40], S[ui][:], r=[("S", ui)], w=["cT_s"])
            if ti < 8:
                P.dma("sp", k.xs_tok.rearrange("(g p) c -> p g c", p=128)[:, :, ti * 512:(ti + 1) * 512], stage[si][:],
                      r=[("stg", si)], w=["xs_tok"])
            elif ti < 10:
                P.dma("sp", k.b_tok.rearrange("(g p) c -> p g c", p=128)[:, :, (ti - 8) * 512:(ti - 7) * 512], stage[si][:],
                      r=[("stg", si)], w=["b_tok"])
        for ti in range(8):
            wi = load_w(ti * 512)
            si = ti % 2
            for tg in range(NT):
                pb = P.bank()
                for kk in range(16):
                    P.op("pe", lambda h, kk=kk, wi=wi, tg=tg, pb=pb: h.matmul(
                        k.ps[pb][:], lhsT=aT[:, kk, tg * 128:(tg + 1) * 128], rhs=wt[wi][:, kk, :],
                        start=(kk == 0), stop=(kk == 15)), r=[("wt", wi), ("aT", tg)], w=[("ps", pb)], inc=(kk == 15))
                P.op("act", lambda h, si=si, tg=tg, pb=pb: h.activation(out=stage[si][:, tg, :], in_=k.ps[pb][:], func=AF.Silu),
                     r=[("ps", pb)], w=[("stg", si)])
            P.dma("sp", k.sz_tok.rearrange("(g p) c -> p g c", p=128)[:, :, ti * 512:(ti + 1) * 512], stage[si][:],
                  r=[("stg", si)], w=["sz_tok"])
        wi = load_w(XB + 6144, 128)
        etmp = sb(nc, es, "etmp", [128, 128], F32)
        for tg in range(NT):
            pb = P.bank()
            for kk in range(16):
                P.op("pe", lambda h, kk=kk, wi=wi, tg=tg, pb=pb: h.matmul(
                    k.ps[pb][:, 0:128], lhsT=aT[:, kk, tg * 128:(tg + 1) * 128], rhs=wt[wi][:, kk, 0:128],
                    start=(kk == 0), stop=(kk == 15)), r=[("wt", wi), ("aT", tg)], w=[("ps", pb)], inc=(kk == 15))
            P.op("dve", lambda h, pb=pb: h.tensor_tensor(out=etmp[:], in0=k.ps[pb][:, 0:128], in1=dtb[:], op=ALU.add),
                 r=[("ps", pb), "dtb"], w=["etmp"])
            P.op("act", lambda h: h.activation(out=etmp[:], in_=etmp[:], func=AF.Exp), r=["etmp"], w=["etmp"])
            P.op("act", lambda h, tg=tg: h.activation(out=dt_tok[:, tg, :], in_=etmp[:], func=AF.Ln, bias=one[:]),
                 r=["etmp", "onec"], w=[("dt", tg)])
            P.op("dve", lambda h, tg=tg: h.tensor_tensor(out=dtA_tok[:, tg, :], in0=dt_tok[:, tg, :], in1=Abc[:], op=ALU.mult),
                 r=[("dt", tg), "Abc"], w=[("dtA", tg)])
        P.barrier()
    return dt_tok, dtA_tok


def host_tri():
    i = np.arange(128)
    kk, ll = i[:, None], i[None, :]
    return np.stack([(kk <= ll), (kk >= ll), (kk > ll), (kk < ll), np.ones((128, 128), bool)]).astype(np.float32)


def fillreg(k, h):
    if getattr(k, "_fillreg", None) is None:
        k._fillreg = h.to_reg(-30000.0)
    return k._fillreg


def bc3(ap2, n):
    return ap2.unsqueeze(2).to_broadcast([ap2.shape[0], ap2.shape[1], n])


def phase_ssd_scan(k, dt_tok, dtA_tok, groups=range(8), stage=99, dirs=(0, 1), maxchunks=99):
    nc, P = k.nc, k.P
    with ExitStack() as es:
        tri = sb(nc, es, "tri", [128, 5, 128], F32)
        P.dma("sp", tri[:], k.c_tri.rearrange("a p l -> p a l"), w=["tri"])
        cs_tok = sb(nc, es, "cs_tok", [128, NT, 128], F32)
        E3 = sb(nc, es, "E3", [128, NT, 384], F32)
        for tg in range(NT):
            pb = P.bank()
            ps = k.ps[pb]
            for d in range(2):
                P.op("pe", lambda h, d=d, tg=tg, ps=ps: h.matmul(ps[:, d * 64:(d + 1) * 64], lhsT=tri[:, d, :],
                                                                rhs=dtA_tok[:, tg, d * 64:(d + 1) * 64], start=True, stop=True),
                     r=["tri", ("dtA", tg)], w=[("ps", pb)], inc=False)
                P.op("pe", lambda h, d=d, tg=tg, ps=ps: h.matmul(ps[:, 128 + d * 64:128 + (d + 1) * 64], lhsT=tri[:, 2 + d, :],
                                                                rhs=dtA_tok[:, tg, d * 64:(d + 1) * 64], start=True, stop=True),
                     r=["tri", ("dtA", tg)], w=[("ps", pb)], inc=False)
            P.op("pe", lambda h, tg=tg, ps=ps: h.matmul(ps[:, 256:384], lhsT=tri[:, 4, :], rhs=dtA_tok[:, tg, :], start=True, stop=True),
                 r=["tri", ("dtA", tg)], w=[("ps", pb)])
            P.op("dve", lambda h, tg=tg, ps=ps: h.tensor_copy(out=cs_tok[:, tg, :], in_=ps[:, 0:128]), r=[("ps", pb)], w=[("cs", tg)])
            P.op("act", lambda h, tg=tg, ps=ps: h.activation(out=E3[:, tg, :], in_=ps[:, 0:384], func=AF.Exp),
                 r=[("ps", pb)], w=[("E3", tg)])
        BT = [sb(nc, es, f"BT{i}", [128, T], BF16) for i in range(2)]
        CT = [sb(nc, es, f"CT{i}", [128, T], BF16) for i in range(2)]
        Bt = [sb(nc, es, f"Bt{i}", [128, NT, 128], BF16) for i in range(2)]
        Xs = [sb(nc, es, f"Xs{i}", [128, NT, 512], BF16) for i in range(2)]
        xdt = [sb(nc, es, f"xdt{i}", [128, 512], BF16) for i in range(2)]
        xw = [sb(nc, es, f"xw{i}", [128, 512], BF16) for i in range(2)]
        Gs = [sb(nc, es, f"Gs{i}", [128, 128], BF16) for i in range(2)]
        arg = [sb(nc, es, f"arg{i}", [128, 512], F32) for i in range(2)]
        Em = [sb(nc, es, f"Em{i}", [128, 512], BF16) for i in range(2)]
        Mt = [sb(nc, es, f"Mt{i}", [128, 512], BF16) for i in range(2)]
        yo = [sb(nc, es, f"yo{i}", [128, 512], F32) for i in range(2)]
        Dg = [sb(nc, es, f"Dg{i}", [128, 512], F32) for i in range(2)]
        hst = sb(nc, es, "hst", [128, 512], F32)
        hbf = sb(nc, es, "hbf", [128, 512], BF16)
        nh = [0]
        P.nrot = 6
        for gi, g in enumerate(groups):
            r = gi % 2
            P.dma("sp", BT[r][:], k.bT_s[g], r=["bT_s"], w=[("BT", r)])
            P.dma("act", CT[r][:], k.cT_s[g], r=["cT_s"], w=[("CT", r)])
            P.dma("sp", Bt[r][:], k.b_tok.rearrange("(t p) c -> p t c", p=128)[:, :, g * 128:(g + 1) * 128], r=["b_tok"], w=[("Bt", r)])
            P.dma("act", Xs[r][:], k.xs_tok.rearrange("(t p) c -> p t c", p=128)[:, :, g * 512:(g + 1) * 512], r=["xs_tok"], w=[("Xs", r)])
            for d in dirs:
                P.op("pool", lambda h: h.memset(hst[:], 0.0), w=["hst"])
                P.op("pool", lambda h: h.memset(hbf[:], 0.0), w=["hbf"])
                order = [16, 17] + list(range(16)) if d == 0 else [17, 16] + list(range(15, -1, -1))
                sgn = 1 if d == 0 else -1
                dh0 = d * 64 + g * 8
                def gen_pre(tg, q):
                    tk = slice(tg * 128, (tg + 1) * 128)
                    P.op("dve", lambda h, q=q, r=r, tg=tg, dh0=dh0: h.tensor_tensor(
                        out=xdt[q][:].rearrange("p (e c) -> p e c", c=64), in0=Xs[r][:, tg, :].rearrange("p (e c) -> p e c", c=64),
                        in1=bc3(dt_tok[:, tg, dh0:dh0 + 8], 64), op=ALU.mult), r=[("Xs", r), ("dt", tg)], w=[("xdt", q)])
                    yield
                    P.op("dve", lambda h, q=q, tg=tg, dh0=dh0: h.tensor_tensor(
                        out=xw[q][:].rearrange("p (e c) -> p e c", c=64), in0=xdt[q][:].rearrange("p (e c) -> p e c", c=64),
                        in1=bc3(E3[:, tg, 128 + dh0:128 + dh0 + 8], 64), op=ALU.mult), r=[("xdt", q), ("E3", tg)], w=[("xw", q)])
                    yield
                    pg = P.bank()
                    P.op("pe", lambda h, r=r, tk=tk, pg=pg: h.matmul(k.ps[pg][:, 0:128], lhsT=BT[r][:, tk], rhs=CT[r][:, tk],
                                                                    start=True, stop=True), r=[("BT", r), ("CT", r)], w=[("ps", pg)])
                    yield
                    P.op("act", lambda h, q=q, pg=pg: h.activation(out=Gs[q][:], in_=k.ps[pg][:, 0:128], func=AF.Copy),
                         r=[("ps", pg)], w=[("Gs", q)])
                    yield

                def gen_half(tg, q, half, py):
                    hq = half
                    pr = P.bank()
                    c0 = dh0 + half * 4
                    P.op("pool", lambda h, hq=hq, tg=tg, c0=c0: h.tensor_tensor(
                        out=Dg[hq][:].rearrange("p (e c) -> p e c", c=128), in0=k.ident_f[:].unsqueeze(1).to_broadcast([128, 4, 128]),
                        in1=bc3(cs_tok[:, tg, c0:c0 + 4], 128), op=ALU.mult), r=[("cs", tg), "ident_f"], w=[("Dg", hq)])
                    yield
                    P.op("pe", lambda h, hq=hq, pr=pr: h.matmul(k.ps[pr][:], lhsT=tri[:, 4, :], rhs=Dg[hq][:], start=True, stop=True),
                         r=[("Dg", hq), "tri"], w=[("ps", pr)])
                    yield
                    P.op("dve", lambda h, hq=hq, pr=pr, tg=tg, c0=c0: h.tensor_tensor(
                        out=arg[hq][:].rearrange("p (e c) -> p e c", c=128), in0=k.ps[pr][:].rearrange("p (e c) -> p e c", c=128),
                        in1=bc3(cs_tok[:, tg, c0:c0 + 4], 128), op=ALU.subtract), r=[("ps", pr), ("cs", tg)], w=[("arg", hq)])
                    yield
                    P.op("pool", lambda h, hq=hq, sgn=sgn: h.affine_select(
                        out=arg[hq][:].rearrange("p (e c) -> p e c", c=128), in_=arg[hq][:].rearrange("p (e c) -> p e c", c=128),
                        pattern=[[0, 4], [sgn, 128]], compare_op=ALU.is_ge, fill=fillreg(k, h), base=0, channel_multiplier=-sgn),
                        r=[("arg", hq)], w=[("arg", hq)])
                    yield
                    P.op("act", lambda h, hq=hq: h.activation(out=Em[hq][:], in_=arg[hq][:], func=AF.Exp),
                         r=[("arg", hq)], w=[("Em", hq)])
                    yield
                    P.op("dve", lambda h, hq=hq, q=q: h.tensor_tensor(
                        out=Mt[hq][:].rearrange("p (e c) -> p e c", c=128), in0=Em[hq][:].rearrange("p (e c) -> p e c", c=128),
                        in1=Gs[q][:].unsqueeze(1).to_broadcast([128, 4, 128]), op=ALU.mult),
                        r=[("Em", hq), ("Gs", q)], w=[("Mt", hq)])
                    yield
                    for j in range(4):
                        e = half * 4 + j
                        P.op("pe", lambda h, hq=hq, j=j, e=e, q=q, py=py: h.matmul(
                            k.ps[py][:, e * 64:(e + 1) * 64], lhsT=Mt[hq][:, j * 128:(j + 1) * 128], rhs=xdt[q][:, e * 64:(e + 1) * 64],
                            start=True, stop=True), r=[("Mt", hq), ("xdt", q)], w=[("ps", py)], inc=(j == 3))
                    yield

                def gen_b(tg, q, py):
                    tk = slice(tg * 128, (tg + 1) * 128)
                    pys = P.bank()
                    P.op("pe", lambda h, r=r, tk=tk, pys=pys: h.matmul(k.ps[pys][:], lhsT=CT[r][:, tk], rhs=hbf[:], start=True, stop=True),
                         r=[("CT", r), "hbf"], w=[("ps", pys)])
                    yield
                    pd = P.bank()
                    P.op("pe", lambda h, r=r, tg=tg, q=q, pd=pd: h.matmul(k.ps[pd][:], lhsT=Bt[r][:, tg, :], rhs=xw[q][:],
                                                                        start=True, stop=True), r=[("Bt", r), ("xw", q)], w=[("ps", pd)])
                    yield
                    P.op("dve", lambda h, tg=tg, dh0=dh0: h.tensor_tensor(
                        out=hst[:].rearrange("p (e c) -> p e c", c=64), in0=hst[:].rearrange("p (e c) -> p e c", c=64),
                        in1=bc3(E3[:, tg, 256 + dh0:256 + dh0 + 8], 64), op=ALU.mult), r=["hst", ("E3", tg)], w=["hst"])
                    yield
                    P.op("dve", lambda h, pd=pd: h.tensor_tensor(out=hst[:], in0=k.ps[pd][:], in1=hst[:], op=ALU.add),
                         r=[("ps", pd), "hst"], w=["hst"])
                    yield
                    P.op("act", lambda h: h.activation(out=hbf[:], in_=hst[:], func=AF.Copy), r=["hst"], w=["hbf"])
                    yield
                    P.op("dve", lambda h, q=q, pys=pys, tg=tg, dh0=dh0: h.tensor_tensor(
                        out=yo[q][:].rearrange("p (e c) -> p e c", c=64), in0=k.ps[pys][:].rearrange("p (e c) -> p e c", c=64),
                        in1=bc3(E3[:, tg, dh0:dh0 + 8], 64), op=ALU.mult), r=[("ps", pys), ("E3", tg)], w=[("yo", q)])
                    yield
                    P.op("dve", lambda h, q=q, py=py: h.tensor_tensor(out=yo[q][:], in0=k.ps[py][:], in1=yo[q][:], op=ALU.add),
                         r=[("ps", py), ("yo", q)], w=[("yo", q)])
                    yield
                    P.dma("sp", k.y_s[d, tk, g * 512:(g + 1) * 512], yo[q][:], r=[("yo", q)], w=["y_s"])
                    yield

                def merge(gens):
                    gens = list(gens)
                    while gens:
                        for gnr in list(gens):
                            try:
                                next(gnr)
                            except StopIteration:
                                gens.remove(gnr)

                order = order[:maxchunks]
                pend = None
                for ci, tg in enumerate(order):
                    q = ci % 2
                    py = 6 + q
                    gens = [gen_pre(tg, q), gen_half(tg, q, 0, py), gen_half(tg, q, 1, py)]
                    if pend is not None:
                        gens.append(gen_b(*pend))
                    merge(gens)
                    pend = (tg, q, py)
                merge([gen_b(*pend)])
        P.barrier()
        P.nrot = 8


def phase_ssd_out(k, layer, src_lat, src_ctx):
    nc, P = k.nc, k.P
    with ExitStack() as es:
        W = sb(nc, es, "Wout", [128, 32, D], BF16)
        ngT = sb(nc, es, "ngT", [128, 32], F32)
        P.dma("sp", ngT[:], k.ssd_norm_g.rearrange("(k p) -> p k", p=128), w=["ngT"], allow_slow_non_contiguous=True)
        wv = k.ssd_w_out.rearrange("(k p) n -> p k n", p=128)
        yf = sb(nc, es, "yf", [128, 2048], F32)
        yb = sb(nc, es, "yb", [128, 2048], F32)
        for kk in range(32):
            stg = yf if kk % 2 == 0 else yb
            key = "yf" if kk % 2 == 0 else "yb"
            P.dma("sp" if kk % 2 == 0 else "act", stg[:], wv[:, kk, :], w=[key])
            P.op("dve" if kk % 2 == 0 else "pool", lambda h, kk=kk, stg=stg: h.tensor_scalar(
                out=W[:, kk, :], in0=stg[:], scalar1=ngT[:, kk:kk + 1], scalar2=None, op0=ALU.mult), r=[key, "ngT"], w=["Wout"])
        xs = sb(nc, es, "xs4", [128, 4096], BF16)
        sz = sb(nc, es, "sz4", [128, 4096], BF16)
        yzb = sb(nc, es, "yzb", [128, 4096], BF16)
        ynT = sb(nc, es, "ynT", [128, 32, 128], BF16)
        hb = sb(nc, es, "hb4", [128, D], F32)
        ho = sb(nc, es, "ho4", [128, D], F32)
        st = sb(nc, es, "st4", [128, 8], F32)
        Dbc = load_bc(k, es, "Dbc", k.ssd_d, 64)
        g1b = sb(nc, es, "g1bc", [128, D], F32)
        for tg in range(NT):
            lat = tg < L // 128
            src = src_lat[tg * 128:(tg + 1) * 128, :] if lat else src_ctx[(tg - 16) * 128:(tg - 15) * 128, :]
            if tg in (0, L // 128):
                P.dma("sp", g1b[:], k.mod[layer, 0 if lat else 1, 2 * D:3 * D].partition_broadcast(128), r=[("mod", layer)], w=["g1bc"])
            tk = slice(tg * 128, (tg + 1) * 128)
            P.dma("sp", hb[:], src, w=["hb4"])
            P.dma("act", xs[:], k.xs_tok[tk, :], r=["xs_tok"], w=["xs4"])
            P.dma("act", sz[:], k.sz_tok[tk, :], r=["sz_tok"], w=["sz4"])
            for half in range(2):
                cs_ = slice(half * 2048, (half + 1) * 2048)
                P.dma("sp", yf[:], k.y_s[0, tk, cs_], r=["y_s"], w=["yf"])
                P.dma("sp", yb[:], k.y_s[1, tk, cs_], r=["y_s"], w=["yb"])
                P.op("dve", lambda h: h.tensor_tensor(out=yf[:], in0=yf[:], in1=yb[:], op=ALU.add), r=["yf", "yb"], w=["yf"])
                P.op("pool", lambda h, half=half, cs_=cs_: h.tensor_tensor(
                    out=yb[:].rearrange("p (e c) -> p e c", c=64), in0=xs[:, cs_].rearrange("p (e c) -> p e c", c=64),
                    in1=bc3(Dbc[:, half * 32:(half + 1) * 32], 64), op=ALU.mult), r=["xs4", "Dbc", "yf"], w=["yb"])
                P.op("dve", lambda h: h.tensor_tensor(out=yf[:], in0=yf[:], in1=yb[:], op=ALU.add), r=["yf", "yb"], w=["yf"])
                P.op("dve", lambda h, cs_=cs_: h.tensor_tensor(out=yf[:], in0=yf[:], in1=sz[:, cs_], op=ALU.mult), r=["yf", "sz4"], w=["yf"])
                P.op("act", lambda h: h.activation(out=yb[:], in_=yf[:], func=AF.Square), r=["yf"], w=["yb"])
                P.op("dve", lambda h, half=half: h.tensor_reduce(out=st[:, half:half + 1], in_=yb[:], axis=AX.X, op=ALU.add),
                     r=["yb"], w=["st4"])
                P.op("pool", lambda h, cs_=cs_: h.tensor_copy(out=yzb[:, cs_], in_=yf[:]), r=["yf"], w=["yzb"])
            P.op("dve", lambda h: h.tensor_tensor(out=st[:, 2:3], in0=st[:, 0:1], in1=st[:, 1:2], op=ALU.add), r=["st4"], w=["st4"])
            P.op("dve", lambda h: h.tensor_scalar(out=st[:, 3:4], in0=st[:, 2:3], scalar1=1.0 / XB, scalar2=EPS, op0=ALU.mult, op1=ALU.add),
                 r=["st4"], w=["st4"])
            P.op("act", lambda h: h.activation(out=st[:, 4:5], in_=st[:, 3:4], func=AF.Sqrt), r=["st4"], w=["st4"])
            P.op("dve", lambda h: h.reciprocal(out=st[:, 5:6], in_=st[:, 4:5]), r=["st4"], w=["st4"])
            for k8 in range(4):
                pb = P.bank()
                psb = k.ps[pb][:].bitcast(BF16)
                for j in range(8):
                    kk = k8 * 8 + j
                    P.op("pe", lambda h, j=j, kk=kk, psb=psb: h.transpose(out=psb[:, j * 128:(j + 1) * 128],
                                                                       in_=yzb[:, kk * 128:(kk + 1) * 128], identity=k.ident_bf[:]),
                         r=["yzb", "ident"], w=[("ps", pb)], inc=(j == 7))
                P.op("act" if k8 % 2 == 0 else "dve", (lambda h, k8=k8, psb=psb: h.activation(
                    out=ynT[:, k8 * 8:(k8 + 1) * 8, :], in_=psb.rearrange("p (j t) -> p j t", j=8), func=AF.Copy)) if k8 % 2 == 0 else
                    (lambda h, k8=k8, psb=psb: h.tensor_copy(out=ynT[:, k8 * 8:(k8 + 1) * 8, :], in_=psb.rearrange("p (j t) -> p j t", j=8))),
                    r=[("ps", pb)], w=["ynT"])
            for db in range(4):
                pb = P.bank()
                dsl = slice(db * 512, (db + 1) * 512)
                for kk in range(32):
                    P.op("pe", lambda h, kk=kk, dsl=dsl, pb=pb: h.matmul(k.ps[pb][:], lhsT=ynT[:, kk, :], rhs=W[:, kk, dsl],
                                                                        start=(kk == 0), stop=(kk == 31)),
                         r=["ynT", "Wout"], w=[("ps", pb)], inc=(kk == 31))
                P.op("dve", lambda h, dsl=dsl, pb=pb: h.scalar_tensor_tensor(
                    out=ho[:, dsl], in0=k.ps[pb][:], scalar=st[:, 5:6], in1=g1b[:, dsl], op0=ALU.mult, op1=ALU.mult),
                    r=[("ps", pb), "st4", "g1bc"], w=["ho4"])
                P.op("pool", lambda h, dsl=dsl: h.tensor_tensor(out=ho[:, dsl], in0=ho[:, dsl], in1=hb[:, dsl], op=ALU.add),
                     r=["ho4", "hb4"], w=["ho4"])
            P.dma("sp", k.hres[tk, :], ho[:], r=["ho4"], w=["hres"])
        P.barrier()


NE = 16
CAPL = 256
CAPC = 32


def host_moe_consts():
    iota = np.tile(np.arange(288, dtype=np.float32)[None, :], (128, 1))
    jidx = (np.arange(128, dtype=np.float32)[:, None] + 128.0 * np.arange(3, dtype=np.float32)[None, :])
    sel = np.zeros((16, 16, 128), np.float32)
    for e in range(16):
        sel[e, e, :] = 1.0
    return iota, np.ascontiguousarray(jidx), sel


def phase_moe(k, layer, with_ctx):
    nc, P = k.nc, k.P
    ntl = L // 128
    nt = NT if with_ctx else ntl
    ntok = nt * 128
    ns = 288 if with_ctx else 256
    sets = [(0, ntl, CAPL, 0)] + ([(ntl, 2, CAPC, 256)] if with_ctx else [])
    jts = [(0, 128), (128, 128)] + ([(256, 32)] if with_ctx else [])
    with ExitStack() as es:
        gate_all = sb(nc, es, "gate_all", [128, NE, 3], F32)
        slotT = sb(nc, es, "slotT", [16, T], F32)
        with ExitStack() as esA:
            f_tok = sb(nc, esA, "f_tok", [128, NT, D], BF16)
            aff_tok = sb(nc, esA, "aff_tok", [128, NT, NE], F32)
            affhl = sb(nc, esA, "affhl", [128, NT, NE, 2], BF16)
            slot_tok = sb(nc, esA, "slot_tok", [128, NT, NE], F32)
            iota = sb(nc, esA, "iota", [128, 288], F32)
            P.dma("sp", iota[:], k.c_iota, w=["iota"])
            with ExitStack() as esN:
                affT = sb(nc, esN, "affT", [16, T], F32)
                work = sb(nc, esN, "work", [16, T], F32)
                mask = sb(nc, esN, "mask", [16, T], F32)
                pos = sb(nc, esN, "pos", [16, T], F32)
                hb = [sb(nc, esN, f"mhb{i}", [128, D], F32) for i in range(2)]
                tmp = sb(nc, esN, "mtmp", [128, D], F32)
                a32 = sb(nc, esN, "ma32", [128, D], F32)
                fT32 = sb(nc, esN, "fT32", [128, 16, 128], F32)
                st = sb(nc, esN, "mst", [128, NT, 8], F32)
                e16 = sb(nc, esN, "e16", [128, NE], F32)
                lo32 = sb(nc, esN, "lo32", [128, NE], F32)
                m8 = sb(nc, esN, "m8", [16, 8], F32)
                wr = sb(nc, esN, "wr", [128, 16, NE], F32)
                P.dma("sp", wr[:], k.moe_w_router[layer].rearrange("(k p) e -> p k e", p=128), w=["wr"])
                gb = load_bc(k, esN, "mgb", k.norm_ffn_g[layer], D)
                scb = sb(nc, esN, "mscb", [128, D], F32)
                shb = sb(nc, esN, "mshb", [128, D], F32)
                gm = sb(nc, esN, "mgm", [128, D], F32)
                for (t0, ntile, cap, soff) in sets:
                    r = 0 if t0 == 0 else 1
                    P.dma("sp", scb[:], k.mod[layer, r, 4 * D:5 * D].partition_broadcast(128), r=[("mod", layer)], w=["mscb"])
                    P.dma("sp", shb[:], k.mod[layer, r, 3 * D:4 * D].partition_broadcast(128), r=[("mod", layer)], w=["mshb"])
                    P.op("dve", lambda h: h.scalar_tensor_tensor(out=gm[:], in0=scb[:], scalar=1.0, in1=gb[:], op0=ALU.add, op1=ALU.mult),
                         r=["mscb", "mgb"], w=["mgm"])
                    for tg in range(t0, t0 + ntile):
                        i = tg % 2
                        P.dma("sp", hb[i][:], k.hres[tg * 128:(tg + 1) * 128, :], r=["hres"], w=[("mhb", i)])
                        P.op("act", lambda h, i=i: h.activation(out=tmp[:], in_=hb[i][:], func=AF.Square), r=[("mhb", i)], w=["mtmp"])
                        P.op("dve", lambda h, tg=tg: h.tensor_reduce(out=st[:, tg, 0:1], in_=tmp[:], axis=AX.X, op=ALU.add),
                             r=["mtmp"], w=[("mst", tg)])
                        P.op("dve", lambda h, tg=tg: h.tensor_scalar(out=st[:, tg, 1:2], in0=st[:, tg, 0:1], scalar1=1.0 / D, scalar2=EPS,
                                                                     op0=ALU.mult, op1=ALU.add), r=[("mst", tg)], w=[("mst", tg)])
                        P.op("act", lambda h, tg=tg: h.activation(out=st[:, tg, 2:3], in_=st[:, tg, 1:2], func=AF.Sqrt),
                             r=[("mst", tg)], w=[("mst", tg)])
                        P.op("dve", lambda h, tg=tg: h.reciprocal(out=st[:, tg, 3:4], in_=st[:, tg, 2:3]), r=[("mst", tg)], w=[("mst", tg)])
                        P.op("dve", lambda h, i=i, tg=tg: h.scalar_tensor_tensor(out=tmp[:], in0=hb[i][:], scalar=st[:, tg, 3:4], in1=gm[:],
                                                                               op0=ALU.mult, op1=ALU.mult),
                             r=[("mhb", i), ("mst", tg), "mgm"], w=["mtmp"])
                        P.op("pool", lambda h: h.tensor_tensor(out=a32[:], in0=tmp[:], in1=shb[:], op=ALU.add),
                             r=["mtmp", "mshb"], w=["ma32"])
                        P.op("act", lambda h, tg=tg: h.activation(out=f_tok[:, tg, :], in_=a32[:], func=AF.Copy), r=["ma32"], w=[("ftok", tg)])
                        for k4 in range(4):
                            pb = P.bank()
                            for j in range(4):
                                kk = k4 * 4 + j
                                P.op("pe", lambda h, j=j, kk=kk, pb=pb: h.transpose(out=k.ps[pb][:, j * 128:(j + 1) * 128],
                                                                                 in_=a32[:, kk * 128:(kk + 1) * 128], identity=k.ident_f[:]),
                                     r=["ma32", "ident_f"], w=[("ps", pb)], inc=(j == 3))
                            P.op("dve", lambda h, k4=k4, pb=pb: h.tensor_copy(out=fT32[:, k4 * 4:(k4 + 1) * 4, :],
                                                                             in_=k.ps[pb][:].rearrange("p (j t) -> p j t", j=4)),
                                 r=[("ps", pb)], w=["fT32"])
                        pb = P.bank()
                        for kk in range(16):
                            P.op("pe", lambda h, kk=kk, pb=pb: h.matmul(k.ps[pb][:, 0:NE], lhsT=fT32[:, kk, :], rhs=wr[:, kk, :],
                                                                      start=(kk == 0), stop=(kk == 15)), r=["fT32", "wr"], w=[("ps", pb)], inc=(kk == 15))
                        P.op("dve", lambda h, tg=tg, pb=pb: h.tensor_reduce(out=st[:, tg, 4:5], in_=k.ps[pb][:, 0:NE], axis=AX.X, op=ALU.max),
                             r=[("ps", pb)], w=[("mst", tg)])
                        P.op("dve", lambda h, tg=tg: h.tensor_scalar(out=st[:, tg, 5:6], in0=st[:, tg, 4:5], scalar1=-1.0, scalar2=None, op0=ALU.mult),
                             r=[("mst", tg)], w=[("mst", tg)])
                        P.op("act", lambda h, tg=tg, pb=pb: h.activation(out=e16[:], in_=k.ps[pb][:, 0:NE], func=AF.Exp, bias=st[:, tg, 5:6]),
                             r=[("ps", pb), ("mst", tg)], w=["e16"])
                        P.op("dve", lambda h, tg=tg: h.tensor_reduce(out=st[:, tg, 6:7], in_=e16[:], axis=AX.X, op=ALU.add), r=["e16"], w=[("mst", tg)])
                        P.op("dve", lambda h, tg=tg: h.reciprocal(out=st[:, tg, 7:8], in_=st[:, tg, 6:7]), r=[("mst", tg)], w=[("mst", tg)])
                        P.op("dve", lambda h, tg=tg: h.tensor_scalar(out=aff_tok[:, tg, :], in0=e16[:], scalar1=st[:, tg, 7:8], scalar2=None, op0=ALU.mult),
                             r=["e16", ("mst", tg)], w=[("aff", tg)])
                        P.op("pool", lambda h, tg=tg: h.tensor_copy(out=affhl[:, tg, :, 0], in_=aff_tok[:, tg, :]), r=[("aff", tg)], w=[("affhl", tg)])
                        P.op("dve", lambda h, tg=tg: h.tensor_tensor(out=lo32[:], in0=aff_tok[:, tg, :], in1=affhl[:, tg, :, 0], op=ALU.subtract),
                             r=[("aff", tg), ("affhl", tg)], w=["lo32"])
                        P.op("pool", lambda h, tg=tg: h.tensor_copy(out=affhl[:, tg, :, 1], in_=lo32[:]), r=["lo32"], w=[("affhl", tg)])
                        pb = P.bank()
                        P.op("pe", lambda h, tg=tg, pb=pb: h.transpose(out=k.ps[pb][0:16, 0:128], in_=aff_tok[:, tg, :], identity=k.ident_f[:]),
                             r=[("aff", tg), "ident_f"], w=[("ps", pb)])
                        P.op("dve", lambda h, tg=tg, pb=pb: h.tensor_copy(out=affT[:, tg * 128:(tg + 1) * 128], in_=k.ps[pb][0:16, 0:128]),
                             r=[("ps", pb)], w=["affT"])
                    c0, c1 = t0 * 128, (t0 + ntile) * 128
                    P.op("dve", lambda h, c0=c0, c1=c1: h.tensor_copy(out=work[:, c0:c1], in_=affT[:, c0:c1]), r=["affT"], w=["work"])
                    for rnd in range(cap // 8):
                        P.op("dve", lambda h, c0=c0, c1=c1: h.max(out=m8[:], in_=work[:, c0:c1]), r=["work"], w=["m8"])
                        if rnd < cap // 8 - 1:
                            P.op("dve", lambda h, c0=c0, c1=c1: h.match_replace(out=work[:, c0:c1], in_to_replace=m8[:], in_values=work[:, c0:c1],
                                                                              imm_value=-1.0), r=["work", "m8"], w=["work"])
                    P.op("dve", lambda h, c0=c0, c1=c1: h.tensor_scalar(out=mask[:, c0:c1], in0=affT[:, c0:c1], scalar1=m8[:, 7:8], scalar2=None,
                                                                       op0=ALU.is_ge), r=["affT", "m8"], w=["mask"])
                    P.op("dve", lambda h, c0=c0, c1=c1: h.tensor_tensor_scan(out=pos[:, c0:c1], data0=mask[:, c0:c1], data1=mask[:, c0:c1],
                                                                            initial=0.0, op0=ALU.add, op1=ALU.max), r=["mask"], w=["pos"])
                    P.op("dve", lambda h, c0=c0, c1=c1, soff=soff: h.scalar_tensor_tensor(out=slotT[:, c0:c1], in0=pos[:, c0:c1], scalar=float(soff),
                                                                                         in1=mask[:, c0:c1], op0=ALU.add, op1=ALU.mult),
                         r=["pos", "mask"], w=["slotT"])
                    P.op("dve", lambda h, c0=c0, c1=c1: h.tensor_scalar(out=slotT[:, c0:c1], in0=slotT[:, c0:c1], scalar1=-1.0, scalar2=None, op0=ALU.add),
                         r=["slotT"], w=["slotT"])
                pb = P.bank()
                for tg in range(nt):
                    P.op("pe", lambda h, tg=tg, pb=pb: h.transpose(out=k.ps[pb][:, tg * 16:(tg + 1) * 16], in_=slotT[:, tg * 128:(tg + 1) * 128],
                                                                  identity=k.ident_f[0:16, 0:16]), r=["slotT", "ident_f"], w=[("ps", pb)], inc=(tg == nt - 1))
                P.op("dve", lambda h, pb=pb: h.tensor_copy(out=slot_tok[:, 0:nt, :], in_=k.ps[pb][:, 0:nt * 16].rearrange("p (t e) -> p t e", e=16)),
                     r=[("ps", pb)], w=["slot_tok"])
                P.barrier()
            PeL = [sb(nc, esA, f"PeL{i}", [128, ntl, CAPL], BF16) for i in range(2)]
            PeC = [sb(nc, esA, f"PeC{i}", [128, 2, CAPC], BF16) for i in range(2)]
            xe = [sb(nc, esA, f"xeg{i}", [128, 16, ns], BF16) for i in range(2)]
            gtmp = sb(nc, esA, "gtmp", [128, 2], F32)
            for e in range(NE):
                q = e % 2
                for tg in range(nt):
                    if tg < ntl:
                        P.op("dve" if tg % 2 == 0 else "pool", lambda h, q=q, tg=tg, e=e: h.tensor_scalar(
                            out=PeL[q][:, tg, :], in0=iota[:, 0:CAPL], scalar1=slot_tok[:, tg, e:e + 1], scalar2=None, op0=ALU.is_equal),
                            r=["iota", "slot_tok"], w=[("PeL", q)])
                    else:
                        P.op("dve", lambda h, q=q, tg=tg, e=e: h.tensor_scalar(
                            out=PeC[q][:, tg - ntl, :], in0=iota[:, 256:288], scalar1=slot_tok[:, tg, e:e + 1], scalar2=None, op0=ALU.is_equal),
                            r=["iota", "slot_tok"], w=[("PeC", q)])
                for kk in range(16):
                    pb = P.bank()
                    for tg in range(ntl):
                        P.op("pe", lambda h, q=q, tg=tg, kk=kk, pb=pb: h.matmul(k.ps[pb][:, 0:CAPL], lhsT=f_tok[:, tg, kk * 128:(kk + 1) * 128],
                                                                              rhs=PeL[q][:, tg, :], start=(tg == 0), stop=(tg == ntl - 1)),
                             r=[("ftok", tg), ("PeL", q)], w=[("ps", pb)], inc=(tg == ntl - 1))
                    if with_ctx:
                        for tc in range(2):
                            P.op("pe", lambda h, q=q, tc=tc, kk=kk, pb=pb: h.matmul(k.ps[pb][:, 256:288], lhsT=f_tok[:, ntl + tc, kk * 128:(kk + 1) * 128],
                                                                                  rhs=PeC[q][:, tc, :], start=(tc == 0), stop=(tc == 1)),
                                 r=[("ftok", ntl + tc), ("PeC", q)], w=[("ps", pb)], inc=(tc == 1))
                    P.op("act" if kk % 2 == 0 else "dve", (lambda h, q=q, kk=kk, pb=pb: h.activation(out=xe[q][:, kk, :], in_=k.ps[pb][:, 0:ns], func=AF.Copy))
                         if kk % 2 == 0 else (lambda h, q=q, kk=kk, pb=pb: h.tensor_copy(out=xe[q][:, kk, :], in_=k.ps[pb][:, 0:ns])),
                         r=[("ps", pb)], w=[("xeg", q)])
                P.dma("sp", k.xeT_d[e, :, :, 0:ns], xe[q][:], r=[("xeg", q)], w=["xeT_d"])
                for jt, (j0, rows) in enumerate(jts):
                    pb = P.bank()
                    if jt < 2:
                        for tg in range(ntl):
                            P.op("pe", lambda h, q=q, tg=tg, e=e, j0=j0, pb=pb: h.matmul(k.ps[pb][:, 0:2], lhsT=PeL[q][:, tg, j0:j0 + 128],
                                                                                       rhs=affhl[:, tg, e, :], start=(tg == 0), stop=(tg == ntl - 1)),
                                 r=[("PeL", q)] + [("affhl", tg)], w=[("ps", pb)], inc=(tg == ntl - 1))
                    else:
                        for tc in range(2):
                            P.op("pe", lambda h, q=q, tc=tc, e=e, pb=pb: h.matmul(k.ps[pb][0:32, 0:2], lhsT=PeC[q][:, tc, :],
                                                                                rhs=affhl[:, ntl + tc, e, :], start=(tc == 0), stop=(tc == 1)),
                                 r=[("PeC", q), ("affhl", ntl + tc)], w=[("ps", pb)], inc=(tc == 1))
                    P.op("dve", lambda h, rows=rows, pb=pb: h.tensor_copy(out=gtmp[0:rows, :], in_=k.ps[pb][0:rows, 0:2]), r=[("ps", pb)], w=["gtmp"])
                    P.op("dve", lambda h, rows=rows, e=e, jt=jt: h.tensor_tensor(out=gate_all[0:rows, e, jt:jt + 1], in0=gtmp[0:rows, 0:1],
                                                                               in1=gtmp[0:rows, 1:2], op=ALU.add), r=["gtmp"], w=["gate_all"])
            P.barrier()
        with ExitStack() as esB:
            wt = [sb(nc, esB, f"ewt{i}", [128, 16, 512], BF16) for i in range(8)]
            xe2 = [sb(nc, esB, f"xe2{i}", [128, 16, ns], BF16) for i in range(2)]
            hT = sb(nc, esB, "hT", [128, 16, ns], BF16)
            sg = [sb(nc, esB, f"sg{i}", [128, ns], F32) for i in range(2)]
            ye = [sb(nc, esB, f"ye{i}", [128, 3, D], BF16) for i in range(2)]
            nw = [0]

            def load_w(src):
                i = nw[0] % 8
                nw[0] += 1
                P.dma("pool", wt[i][:], src, w=[("ewt", i)])
                return i
            nsg = 0
            for e in range(NE):
                q = e % 2
                P.dma("sp", xe2[q][:], k.xeT_d[e, :, :, 0:ns], r=["xeT_d"], w=[("xe2", q)])
                wg = k.moe_w_gate[layer, e].rearrange("(k p) n -> p k n", p=128)
                wu = k.moe_w_up[layer, e].rearrange("(k p) n -> p k n", p=128)
                wd = k.moe_w_down[layer, e].rearrange("(k p) n -> p k n", p=128)
                for ft in range(4):
                    ig = load_w(wg[:, :, ft * 512:(ft + 1) * 512])
                    iu = load_w(wu[:, :, ft * 512:(ft + 1) * 512])
                    for fc in range(4):
                        fi = ft * 4 + fc
                        pg = P.bank()
                        for kk in range(16):
                            P.op("pe", lambda h, kk=kk, ig=ig, fc=fc, q=q, pg=pg: h.matmul(k.ps[pg][:, 0:ns], lhsT=wt[ig][:, kk, fc * 128:(fc + 1) * 128],
                                                                                         rhs=xe2[q][:, kk, :], start=(kk == 0), stop=(kk == 15)),
                                 r=[("ewt", ig), ("xe2", q)], w=[("ps", pg)], inc=(kk == 15))
                        pu = P.bank()
                        for kk in range(16):
                            P.op("pe", lambda h, kk=kk, iu=iu, fc=fc, q=q, pu=pu: h.matmul(k.ps[pu][:, 0:ns], lhsT=wt[iu][:, kk, fc * 128:(fc + 1) * 128],
                                                                                         rhs=xe2[q][:, kk, :], start=(kk == 0), stop=(kk == 15)),
                                 r=[("ewt", iu), ("xe2", q)], w=[("ps", pu)], inc=(kk == 15))
                        s_ = nsg % 2
                        nsg += 1
                        P.op("act", lambda h, s_=s_, pg=pg: h.activation(out=sg[s_][:], in_=k.ps[pg][:, 0:ns], func=AF.Silu), r=[("ps", pg)], w=[("sg", s_)])
                        P.op("dve", lambda h, s_=s_, pu=pu, fi=fi: h.tensor_tensor(out=hT[:, fi, :], in0=k.ps[pu][:, 0:ns], in1=sg[s_][:], op=ALU.mult),
                             r=[("ps", pu), ("sg", s_)], w=["hT"])
                for db in range(4):
                    iw = load_w(wd[:, :, db * 512:(db + 1) * 512])
                    for jt, (j0, rows) in enumerate(jts):
                        pb = P.bank()
                        for fi in range(16):
                            P.op("pe", lambda h, fi=fi, iw=iw, j0=j0, rows=rows, pb=pb: h.matmul(k.ps[pb][0:rows, :], lhsT=hT[:, fi, j0:j0 + rows],
                                                                                                rhs=wt[iw][:, fi, :], start=(fi == 0), stop=(fi == 15)),
                                 r=["hT", ("ewt", iw)], w=[("ps", pb)], inc=(fi == 15))
                        P.op("dve", lambda h, q=q, jt=jt, db=db, rows=rows, e=e, pb=pb: h.tensor_scalar(
                            out=ye[q][0:rows, jt, db * 512:(db + 1) * 512], in0=k.ps[pb][0:rows, :], scalar1=gate_all[0:rows, e, jt:jt + 1],
                            scalar2=None, op0=ALU.mult), r=[("ps", pb), "gate_all"], w=[("ye", q)])
                for jt, (j0, rows) in enumerate(jts):
                    P.dma("sp", k.ye_d[e, 0:rows, jt, :], ye[q][0:rows, jt, :], r=[("ye", q)], w=["ye_d"])
            P.barrier()
        with ExitStack() as esC:
            sel = sb(nc, esC, "sel", [16, NE, 128], F32)
            jidx = sb(nc, esC, "jidx", [128, 3], F32)
            P.dma("sp", sel[:], k.c_sel, w=["sel"])
            P.dma("sp", jidx[:], k.c_jidx, w=["jidx"])
            PT = sb(nc, esC, "PT", [128, NE, 2, 512], BF16)
            yeb = sb(nc, esC, "yeb", [128, NE, 3, 512], BF16)
            hb3 = sb(nc, esC, "chb", [128, 4, D], F32)
            t5 = [sb(nc, esC, f"ct5{i}", [128, 512], F32) for i in range(2)]
            g2b = sb(nc, esC, "g2b", [128, D], F32)
            blocks = [(tb * 512, 512, False) for tb in range(4)] + ([(L, M, True)] if with_ctx else [])
            yev = k.ye_d.rearrange("e p j d -> p e j d")
            nt5 = 0
            for (b0, bn, isctx) in blocks:
                ntb = bn // 128
                if b0 == 0 or isctx:
                    P.dma("sp", g2b[:], k.mod[layer, 1 if isctx else 0, 5 * D:6 * D].partition_broadcast(128), r=[("mod", layer)], w=["g2b"])
                P.dma("sp", hb3[:, 0:ntb, :], k.hres[b0:b0 + bn, :].rearrange("(t p) d -> p t d", p=128), r=["hres"], w=["chb"])
                for e in range(NE):
                    pb = P.bank()
                    P.op("pe", lambda h, e=e, b0=b0, bn=bn, pb=pb: h.matmul(k.ps[pb][:, 0:bn], lhsT=sel[:, e, :], rhs=slotT[:, b0:b0 + bn],
                                                                          start=True, stop=True), r=["sel", "slotT"], w=[("ps", pb)])
                    if not isctx:
                        for jt in range(2):
                            P.op("dve", lambda h, e=e, jt=jt, bn=bn, pb=pb: h.tensor_scalar(
                                out=PT[:, e, jt, 0:bn], in0=k.ps[pb][:, 0:bn], scalar1=jidx[:, jt:jt + 1], scalar2=None, op0=ALU.is_equal),
                                r=[("ps", pb), "jidx"], w=["PT"])
                    else:
                        P.op("dve", lambda h, e=e, bn=bn, pb=pb: h.tensor_scalar(
                            out=PT[0:32, e, 0, 0:bn], in0=k.ps[pb][0:32, 0:bn], scalar1=jidx[0:32, 2:3], scalar2=None, op0=ALU.is_equal),
                            r=[("ps", pb), "jidx"], w=["PT"])
                for db in range(4):
                    dsl = slice(db * 512, (db + 1) * 512)
                    if not isctx:
                        for jt in range(2):
                            P.dma("act" if jt == 0 else "sp", yeb[:, :, jt, :], yev[:, :, jt, dsl], r=["ye_d"], w=["yeb"])
                    else:
                        P.dma("act", yeb[0:32, :, 2, :], yev[0:32, :, 2, dsl], r=["ye_d"], w=["yeb"])
                    for ti in range(ntb):
                        pb = P.bank()
                        if not isctx:
                            i_mm = 0
                            for e in range(NE):
                                for jt in range(2):
                                    P.op("pe", lambda h, e=e, jt=jt, ti=ti, pb=pb, i_mm=i_mm: h.matmul(
                                        k.ps[pb][:], lhsT=PT[:, e, jt, ti * 128:(ti + 1) * 128], rhs=yeb[:, e, jt, :],
                                        start=(i_mm == 0), stop=(i_mm == 2 * NE - 1)), r=["PT", "yeb"], w=[("ps", pb)], inc=(i_mm == 2 * NE - 1))
                                    i_mm += 1
                        else:
                            for e in range(NE):
                                P.op("pe", lambda h, e=e, ti=ti, pb=pb: h.matmul(
                                    k.ps[pb][:], lhsT=PT[0:32, e, 0, ti * 128:(ti + 1) * 128], rhs=yeb[0:32, e, 2, :],
                                    start=(e == 0), stop=(e == NE - 1)), r=["PT", "yeb"], w=[("ps", pb)], inc=(e == NE - 1))
                        q5 = nt5 % 2
                        nt5 += 1
                        P.op("dve", lambda h, q5=q5, dsl=dsl, pb=pb: h.tensor_tensor(out=t5[q5][:], in0=k.ps[pb][:], in1=g2b[:, dsl], op=ALU.mult),
                             r=[("ps", pb), "g2b"], w=[("ct5", q5)])
                        P.op("pool", lambda h, q5=q5, ti=ti, dsl=dsl: h.tensor_tensor(out=hb3[:, ti, dsl], in0=hb3[:, ti, dsl], in1=t5[q5][:], op=ALU.add),
                             r=[("ct5", q5), "chb"], w=["chb"])
                P.dma("sp", k.hres[b0:b0 + bn, :].rearrange("(t p) d -> p t d", p=128), hb3[:, 0:ntb, :], r=["chb"], w=["hres"])
            P.barrier()


RH = 8
NTL = L // 128


def host_ret_consts():
    half = 64
    freqs = 10000.0 ** (-np.arange(half, dtype=np.float64) / half)
    t = np.arange(L)
    ang = np.concatenate([(t // 64)[:, None] * freqs[None, :], (t % 64)[:, None] * freqs[None, :]], axis=1)
    rope = np.stack([np.cos(ang), np.sin(ang)]).astype(np.float32)
    i = np.arange(128)
    rel = np.stack([i[None, :] - i[:, None], i[:, None] - i[None, :]]).astype(np.float32)
    cnt = np.stack([i + 1.0, 128.0 - i], axis=1).astype(np.float32)
    return rope, rel, cnt


def phase_ret_inproj(k, aT):
    nc, P = k.nc, k.P
    wv = k.ret_w_in.rearrange("(k p) n -> p k n", p=128)
    with ExitStack() as es:
        wt = [sb(nc, es, f"rwt{i}", [128, 16, 512], BF16) for i in range(2)]
        cosT = sb(nc, es, "cosT", [128, NTL, 128], F32)
        sinT = sb(nc, es, "sinT", [128, NTL, 128], F32)
        P.dma("sp", cosT[:], k.c_rope[0].rearrange("(t p) c -> p t c", p=128), w=["cosT"])
        P.dma("sp", sinT[:], k.c_rope[1].rearrange("(t p) c -> p t c", p=128), w=["sinT"])
        stage = [sb(nc, es, f"rstg{i}", [128, NT, 512], BF16) for i in range(2)]
        stT = [sb(nc, es, f"rstT{i}", [128, 4, L], BF16) for i in range(2)]
        t1 = [sb(nc, es, f"rt1{i}", [128, 512], F32) for i in range(2)]
        t2 = [sb(nc, es, f"rt2{i}", [128, 512], F32) for i in range(2)]
        rtok = [sb(nc, es, f"rtok{i}", [128, 512], BF16) for i in range(2)]
        nw = [0]

        def load_w(c0):
            i = nw[0] % 2
            nw[0] += 1
            P.dma("pool", wt[i][:], wv[:, :, c0:c0 + 512], w=[("rwt", i)])
            return i

        def v5(ap):
            return ap.rearrange("p (h a f c) -> p h a f c", h=2, a=2, f=2)
        nq = 0
        for which in range(2):
            for ti in range(4):
                wi = load_w(which * 2048 + ti * 512)
                si = ti % 2
                tiles = range(NTL) if which == 0 else range(NT)
                for tg in tiles:
                    pb = P.bank()
                    for kk in range(16):
                        P.op("pe", lambda h, kk=kk, wi=wi, tg=tg, pb=pb: h.matmul(k.ps[pb][:], lhsT=aT[:, kk, tg * 128:(tg + 1) * 128],
                                                                                rhs=wt[wi][:, kk, :], start=(kk == 0), stop=(kk == 15)),
                             r=[("rwt", wi), ("aT", tg)], w=[("ps", pb)], inc=(kk == 15))
                    q = nq % 2
                    nq += 1
                    if tg < NTL:
                        u = v5(k.ps[pb][:])
                        cb = cosT[:, tg, :].rearrange("p (a c) -> p a c", a=2).unsqueeze(1).to_broadcast([128, 2, 2, 64])
                        sbb = sinT[:, tg, :].rearrange("p (a c) -> p a c", a=2).unsqueeze(1).to_broadcast([128, 2, 2, 64])
                        a1, a2, ro = v5(t1[q][:]), v5(t2[q][:]), v5(rtok[q][:])
                        for f in range(2):
                            P.op("dve", lambda h, u=u, a1=a1, cb=cb, f=f: h.tensor_tensor(out=a1[:, :, :, f, :], in0=u[:, :, :, f, :], in1=cb, op=ALU.mult),
                                 r=[("ps", pb), "cosT"], w=[("rt1", q)])
                            P.op("dve", lambda h, u=u, a2=a2, sbb=sbb, f=f: h.tensor_tensor(out=a2[:, :, :, f, :], in0=u[:, :, :, 1 - f, :], in1=sbb, op=ALU.mult),
                                 r=[("ps", pb), "sinT"], w=[("rt2", q)])
                        P.op("pool", lambda h, a1=a1, a2=a2, ro=ro: h.tensor_tensor(out=ro[:, :, :, 0, :], in0=a1[:, :, :, 0, :], in1=a2[:, :, :, 0, :], op=ALU.subtract),
                             r=[("rt1", q), ("rt2", q)], w=[("rtok", q)])
                        P.op("pool", lambda h, a1=a1, a2=a2, ro=ro: h.tensor_tensor(out=ro[:, :, :, 1, :], in0=a1[:, :, :, 1, :], in1=a2[:, :, :, 1, :], op=ALU.add),
                             r=[("rt1", q), ("rt2", q)], w=[("rtok", q)])
                        if which == 1:
                            P.op("act", lambda h, q=q, si=si, tg=tg: h.activation(out=stage[si][:, tg, :], in_=rtok[q][:], func=AF.Copy, scale=1.0 / 16.0),
                                 r=[("rtok", q)], w=[("rstg", si)])
                        pt = P.bank()
                        psb = k.ps[pt][:].bitcast(BF16)
                        for j in range(4):
                            P.op("pe", lambda h, q=q, j=j, psb=psb: h.transpose(out=psb[:, j * 128:(j + 1) * 128], in_=rtok[q][:, j * 128:(j + 1) * 128],
                                                                               identity=k.ident_bf[:]), r=[("rtok", q), "ident"], w=[("ps", pt)], inc=(j == 3))
                        P.op("act", lambda h, si=si, tg=tg, psb=psb, which=which: h.activation(
                            out=stT[si][:, :, tg * 128:(tg + 1) * 128], in_=psb[:, 0:512].rearrange("p (j t) -> p j t", j=4), func=AF.Copy,
                            scale=(1.0 if which == 0 else 1.0 / 16.0)), r=[("ps", pt)], w=[("rstT", si)])
                    else:
                        P.op("act", lambda h, si=si, tg=tg, pb=pb: h.activation(out=stage[si][:, tg, :], in_=k.ps[pb][:], func=AF.Copy, scale=1.0 / 16.0),
                             r=[("ps", pb)], w=[("rstg", si)])
                dstT = k.qT_s if which == 0 else k.kT_s
                P.dma("sp", dstT[:, 2 * ti:2 * ti + 2].rearrange("p h c t -> p (h c) t"), stT[si][:], r=[("rstT", si)], w=["qkT_s"])
                if which == 1:
                    P.dma("sp", k.k_tok.rearrange("(g p) c -> p g c", p=128)[:, :, ti * 512:(ti + 1) * 512], stage[si][:], r=[("rstg", si)], w=["k_tok"])
        for which in range(2):
            for ti in range(8):
                wi = load_w((4096 if which == 0 else 8192) + ti * 512)
                si = ti % 2
                tiles = range(NT) if which == 0 else range(NTL)
                for tg in tiles:
                    pb = P.bank()
                    for kk in range(16):
                        P.op("pe", lambda h, kk=kk, wi=wi, tg=tg, pb=pb: h.matmul(k.ps[pb][:], lhsT=aT[:, kk, tg * 128:(tg + 1) * 128],
                                                                                rhs=wt[wi][:, kk, :], start=(kk == 0), stop=(kk == 15)),
                             r=[("rwt", wi), ("aT", tg)], w=[("ps", pb)], inc=(kk == 15))
                    P.op("act", lambda h, si=si, tg=tg, pb=pb, which=which: h.activation(out=stage[si][:, tg, :], in_=k.ps[pb][:],
                                                                                       func=(AF.Copy if which == 0 else AF.Silu)),
                         r=[("ps", pb)], w=[("rstg", si)])
                if which == 0:
                    P.dma("sp", k.v_tok.rearrange("(g p) c -> p g c", p=128)[:, :, ti * 512:(ti + 1) * 512], stage[si][:], r=[("rstg", si)], w=["v_tok"])
                else:
                    P.dma("sp", k.sg_tok.rearrange("(g p) c -> p g c", p=128)[:, :, ti * 512:(ti + 1) * 512], stage[si][:, 0:NTL, :],
                          r=[("rstg", si)], w=["sg_tok"])
        P.barrier()


def phase_ret_scan(k, heads=range(RH)):
    nc, P = k.nc, k.P
    with ExitStack() as es:
        ldb = load_bc(k, es, "ldb", k.ret_decay.rearrange("a b -> (a b)"), 16)
        rel = sb(nc, es, "rel", [128, 2, 128], F32)
        cnt = sb(nc, es, "cnt", [128, 2], F32)
        P.dma("sp", rel[:], k.c_rel.rearrange("d s l -> s d l"), w=["rel"])
        P.dma("sp", cnt[:], k.c_cnt, w=["cnt"])
        P.op("act", lambda h: h.activation(out=ldb[:], in_=ldb[:], func=AF.Exp), r=["ldb"], w=["ldb"])
        P.op("dve", lambda h: h.tensor_scalar(out=ldb[:], in0=ldb[:], scalar1=-1.0, scalar2=None, op0=ALU.mult), r=["ldb"], w=["ldb"])
        decT = sb(nc, es, "decT", [128, 16, 128], F32)
        dfs = sb(nc, es, "dfs", [128, 16], F32)
        dte = sb(nc, es, "dte", [128, 16], F32)
        dch = sb(nc, es, "dch", [128, 16], F32)
        c2 = sb(nc, es, "c2", [128, 2], F32)
        P.op("dve", lambda h: h.tensor_scalar(out=c2[:], in0=cnt[:], scalar1=-1.0, scalar2=128.0, op0=ALU.mult, op1=ALU.add), r=["cnt"], w=["c2"])
        for d in range(2):
            for hh in range(RH):
                dh = d * RH + hh
                P.op("dve", lambda h, d=d, dh=dh: h.tensor_scalar(out=decT[:, dh, :], in0=rel[:, d, :], scalar1=ldb[:, dh:dh + 1], scalar2=None, op0=ALU.mult),
                     r=["rel", "ldb"], w=["decT"])
            P.op("pool", lambda h, d=d: h.affine_select(out=decT[:, d * RH:(d + 1) * RH, :], in_=decT[:, d * RH:(d + 1) * RH, :],
                                                        pattern=[[0, RH], [1 if d == 0 else -1, 128]], compare_op=ALU.is_ge, fill=fillreg(k, h),
                                                        base=0, channel_multiplier=(-1 if d == 0 else 1)), r=["decT"], w=["decT"])
            P.op("dve", lambda h, d=d: h.tensor_scalar(out=dfs[:, d * RH:(d + 1) * RH], in0=ldb[:, d * RH:(d + 1) * RH], scalar1=cnt[:, d:d + 1], scalar2=None,
                                                       op0=ALU.mult), r=["ldb", "cnt"], w=["dfs"])
            P.op("dve", lambda h, d=d: h.tensor_scalar(out=dte[:, d * RH:(d + 1) * RH], in0=ldb[:, d * RH:(d + 1) * RH], scalar1=c2[:, d:d + 1], scalar2=None,
                                                       op0=ALU.mult), r=["ldb", "c2"], w=["dte"])
        P.op("dve", lambda h: h.tensor_scalar(out=dch[:], in0=ldb[:], scalar1=128.0, scalar2=None, op0=ALU.mult), r=["ldb"], w=["dch"])
        P.op("act", lambda h: h.activation(out=decT[:], in_=decT[:], func=AF.Exp), r=["decT"], w=["decT"])
        P.op("act", lambda h: h.activation(out=dfs[:], in_=dfs[:], func=AF.Exp), r=["dfs"], w=["dfs"])
        P.op("act", lambda h: h.activation(out=dte[:], in_=dte[:], func=AF.Exp), r=["dte"], w=["dte"])
        P.op("act", lambda h: h.activation(out=dch[:], in_=dch[:], func=AF.Exp), r=["dch"], w=["dch"])
        qT = [sb(nc, es, f"qTh{i}", [128, 2, L], BF16) for i in range(2)]
        kT = [sb(nc, es, f"kTh{i}", [128, 2, L], BF16) for i in range(2)]
        kt = [sb(nc, es, f"kth{i}", [128, NT, 256], BF16) for i in range(2)]
        vt = [sb(nc, es, f"vth{i}", [128, NT, 512], BF16) for i in range(2)]
        scD = [sb(nc, es, f"scD{i}", [128, 128], BF16) for i in range(2)]
        kd = [sb(nc, es, f"kd{i}", [128, 256], BF16) for i in range(2)]
        oo = [sb(nc, es, f"oo{i}", [128, 512], F32) for i in range(2)]
        Sp = [sb(nc, es, f"S{i}", [128, 2, 512], F32) for i in range(2)]
        Sbf = sb(nc, es, "Sbf", [128, 2, 512], BF16)
        n_it = 0
        n_s = [0]
        P.nrot = 4
        for hi, hh in enumerate(heads):
            r = hi % 2
            P.dma("sp", qT[r][:], k.qT_s[:, hh], r=["qkT_s"], w=[("qTh", r)])
            P.dma("act", kT[r][:], k.kT_s[:, hh], r=["qkT_s"], w=[("kTh", r)])
            P.dma("sp", kt[r][:], k.k_tok.rearrange("(t p) c -> p t c", p=128)[:, :, hh * 256:(hh + 1) * 256], r=["k_tok"], w=[("kth", r)])
            P.dma("act", vt[r][:], k.v_tok.rearrange("(t p) c -> p t c", p=128)[:, :, hh * 512:(hh + 1) * 512], r=["v_tok"], w=[("vth", r)])
            for d in range(2):
                dh = d * RH + hh
                s0 = Sp[n_s[0] % 2]
                P.op("pool", lambda h, s0=s0: h.memset(s0[:], 0.0), w=[("S", n_s[0] % 2)])
                P.op("pool", lambda h: h.memset(Sbf[:], 0.0), w=["Sbf"])
                order = [16, 17] + list(range(16)) if d == 0 else [17, 16] + list(range(15, -1, -1))
                def merge(gens):
                    gens = list(gens)
                    while gens:
                        for gnr in list(gens):
                            try:
                                next(gnr)
                            except StopIteration:
                                gens.remove(gnr)

                def g_state(tg, q):
                    P.op("pool", lambda h, q=q, r=r, tg=tg, dh=dh: h.tensor_scalar(out=kd[q][:], in0=kt[r][:, tg, :], scalar1=dte[:, dh:dh + 1], scalar2=None,
                                                                                 op0=ALU.mult), r=[("kth", r), "dte"], w=[("kd", q)])
                    yield
                    for c in range(2):
                        pd = 6 + c
                        P.op("pe", lambda h, q=q, r=r, c=c, tg=tg, pd=pd: h.matmul(k.ps[pd][:], lhsT=kd[q][:, c * 128:(c + 1) * 128], rhs=vt[r][:, tg, :],
                                                                                 start=True, stop=True), r=[("kd", q), ("vth", r)], w=[("ps", pd)])
                        yield
                    so, sn = Sp[n_s[0] % 2], Sp[(n_s[0] + 1) % 2]
                    ko, kn = ("S", n_s[0] % 2), ("S", (n_s[0] + 1) % 2)
                    n_s[0] += 1
                    for c in range(2):
                        pd = 6 + c
                        P.op("dve", lambda h, c=c, dh=dh, pd=pd, so=so, sn=sn: h.scalar_tensor_tensor(out=sn[:, c, :], in0=so[:, c, :], scalar=dch[:, dh:dh + 1],
                                                                                                    in1=k.ps[pd][:], op0=ALU.mult, op1=ALU.add),
                             r=[ko, "dch", ("ps", pd)], w=[kn])
                        yield
                    P.op("act", lambda h, sn=sn: h.activation(out=Sbf[:], in_=sn[:], func=AF.Copy), r=[kn], w=["Sbf"])
                    yield

                def g_intra(tg, q, p1):
                    tk = slice(tg * 128, (tg + 1) * 128)
                    ps_ = P.bank()
                    for c in range(2):
                        P.op("pe", lambda h, r=r, c=c, tk=tk, ps_=ps_: h.matmul(k.ps[ps_][:, 0:128], lhsT=kT[r][:, c, tk], rhs=qT[r][:, c, tk],
                                                                              start=(c == 0), stop=(c == 1)), r=[("kTh", r), ("qTh", r)], w=[("ps", ps_)], inc=(c == 1))
                    yield
                    P.op("dve", lambda h, q=q, dh=dh, ps_=ps_: h.tensor_tensor(out=scD[q][:], in0=k.ps[ps_][:, 0:128], in1=decT[:, dh, :], op=ALU.mult),
                         r=[("ps", ps_), "decT"], w=[("scD", q)])
                    yield
                    P.op("pe", lambda h, q=q, r=r, tg=tg, p1=p1: h.matmul(k.ps[p1][:], lhsT=scD[q][:], rhs=vt[r][:, tg, :], start=True, stop=True),
                         r=[("scD", q), ("vth", r)], w=[("ps", p1)])
                    yield

                def g_cross(tg, q, p1):
                    tk = slice(tg * 128, (tg + 1) * 128)
                    p2 = P.bank()
                    for c in range(2):
                        P.op("pe", lambda h, r=r, c=c, tk=tk, p2=p2: h.matmul(k.ps[p2][:], lhsT=qT[r][:, c, tk], rhs=Sbf[:, c, :],
                                                                            start=(c == 0), stop=(c == 1)), r=[("qTh", r), "Sbf"], w=[("ps", p2)], inc=(c == 1))
                    yield
                    P.op("dve", lambda h, q=q, dh=dh, p2=p2: h.tensor_scalar(out=oo[q][:], in0=k.ps[p2][:], scalar1=dfs[:, dh:dh + 1], scalar2=None, op0=ALU.mult),
                         r=[("ps", p2), "dfs"], w=[("oo", q)])
                    yield
                    P.op("dve", lambda h, q=q, p1=p1: h.tensor_tensor(out=oo[q][:], in0=k.ps[p1][:], in1=oo[q][:], op=ALU.add),
                         r=[("ps", p1), ("oo", q)], w=[("oo", q)])
                    yield
                    P.dma("sp", k.o_s[d, tk, hh * 512:(hh + 1) * 512], oo[q][:], r=[("oo", q)], w=["o_s"])
                    yield

                for tg in order:
                    q = n_it % 2
                    n_it += 1
                    if tg < NTL:
                        p1 = 4 + q
                        merge([g_intra(tg, q, p1), g_cross(tg, q, p1), g_state(tg, q)])
                    else:
                        merge([g_state(tg, q)])
        P.barrier()
        P.nrot = 8


def phase_ret_out(k, layer):
    nc, P = k.nc, k.P
    with ExitStack() as es:
        W = sb(nc, es, "rWout", [128, 32, D], BF16)
        ngT = sb(nc, es, "rngT", [128, 32], F32)
        P.dma("sp", ngT[:], k.ret_gn_g.rearrange("(k p) -> p k", p=128), w=["rngT"], allow_slow_non_contiguous=True)
        wv = k.ret_w_out.rearrange("(k p) n -> p k n", p=128)
        yf = sb(nc, es, "ryf", [128, 2048], F32)
        yb = sb(nc, es, "ryb", [128, 2048], F32)
        for kk in range(32):
            stg = yf if kk % 2 == 0 else yb
            key = "ryf" if kk % 2 == 0 else "ryb"
            P.dma("sp" if kk % 2 == 0 else "act", stg[:], wv[:, kk, :], w=[key])
            P.op("dve" if kk % 2 == 0 else "pool", lambda h, kk=kk, stg=stg: h.tensor_scalar(
                out=W[:, kk, :], in0=stg[:], scalar1=ngT[:, kk:kk + 1], scalar2=None, op0=ALU.mult), r=[key, "rngT"], w=["rWout"])
        sg = sb(nc, es, "rsg", [128, 4096], BF16)
        yzb = sb(nc, es, "ryzb", [128, 4096], BF16)
        ynT = sb(nc, es, "rynT", [128, 32, 128], BF16)
        hb = sb(nc, es, "rhb", [128, D], F32)
        ho = sb(nc, es, "rho", [128, D], F32)
        st = sb(nc, es, "rst", [128, 4, 4], F32)
        g1b = load_bc(k, es, "rg1b", k.mod[layer, 0, 2 * D:3 * D], D, key=("mod", layer))
        for tg in range(NTL):
            tk = slice(tg * 128, (tg + 1) * 128)
            P.dma("sp", hb[:], k.hres[tk, :], r=["hres"], w=["rhb"])
            P.dma("act", sg[:], k.sg_tok[tk, :], r=["sg_tok"], w=["rsg"])
            for half in range(2):
                cs_ = slice(half * 2048, (half + 1) * 2048)
                P.dma("sp", yf[:], k.o_s[0, tk, cs_], r=["o_s"], w=["ryf"])
                P.dma("sp", yb[:], k.o_s[1, tk, cs_], r=["o_s"], w=["ryb"])
                P.op("dve", lambda h: h.tensor_tensor(out=yf[:], in0=yf[:], in1=yb[:], op=ALU.add), r=["ryf", "ryb"], w=["ryf"])
                y3 = yf[:].rearrange("p (e c) -> p e c", c=512)
                b3 = yb[:].rearrange("p (e c) -> p e c", c=512)
                P.op("dve", lambda h, y3=y3: h.tensor_reduce(out=st[:, :, 0], in_=y3, axis=AX.X, op=ALU.add), r=["ryf"], w=["rst"])
                P.op("dve", lambda h: h.tensor_scalar(out=st[:, :, 0], in0=st[:, :, 0], scalar1=1.0 / 512, scalar2=None, op0=ALU.mult), r=["rst"], w=["rst"])
                P.op("dve", lambda h, y3=y3: h.tensor_tensor(out=y3, in0=y3, in1=bc3(st[:, :, 0], 512), op=ALU.subtract), r=["ryf", "rst"], w=["ryf"])
                P.op("act", lambda h: h.activation(out=yb[:], in_=yf[:], func=AF.Square), r=["ryf"], w=["ryb"])
                P.op("dve", lambda h, b3=b3: h.tensor_reduce(out=st[:, :, 1], in_=b3, axis=AX.X, op=ALU.add), r=["ryb"], w=["rst"])
                P.op("dve", lambda h: h.tensor_scalar(out=st[:, :, 1], in0=st[:, :, 1], scalar1=1.0 / 512, scalar2=EPS, op0=ALU.mult, op1=ALU.add), r=["rst"], w=["rst"])
                P.op("act", lambda h: h.activation(out=st[:, :, 2], in_=st[:, :, 1], func=AF.Sqrt), r=["rst"], w=["rst"])
                P.op("dve", lambda h: h.reciprocal(out=st[:, :, 3], in_=st[:, :, 2]), r=["rst"], w=["rst"])
                P.op("dve", lambda h, y3=y3: h.tensor_tensor(out=y3, in0=y3, in1=bc3(st[:, :, 3], 512), op=ALU.mult), r=["ryf", "rst"], w=["ryf"])
                P.op("pool", lambda h, cs_=cs_: h.tensor_tensor(out=yzb[:, cs_], in0=yf[:], in1=sg[:, cs_], op=ALU.mult), r=["ryf", "rsg"], w=["ryzb"])
            for k8 in range(4):
                pb = P.bank()
                psb = k.ps[pb][:].bitcast(BF16)
                for j in range(8):
                    kk = k8 * 8 + j
                    P.op("pe", lambda h, j=j, kk=kk, psb=psb: h.transpose(out=psb[:, j * 128:(j + 1) * 128],
                                                                       in_=yzb[:, kk * 128:(kk + 1) * 128], identity=k.ident_bf[:]),
                         r=["ryzb", "ident"], w=[("ps", pb)], inc=(j == 7))
                P.op("act", lambda h, k8=k8, psb=psb: h.activation(out=ynT[:, k8 * 8:(k8 + 1) * 8, :], in_=psb.rearrange("p (j t) -> p j t", j=8), func=AF.Copy),
                     r=[("ps", pb)], w=["rynT"])
            for db in range(4):
                pb = P.bank()
                dsl = slice(db * 512, (db + 1) * 512)
                for kk in range(32):
                    P.op("pe", lambda h, kk=kk, dsl=dsl, pb=pb: h.matmul(k.ps[pb][:], lhsT=ynT[:, kk, :], rhs=W[:, kk, dsl],
                                                                        start=(kk == 0), stop=(kk == 31)),
                         r=["rynT", "rWout"], w=[("ps", pb)], inc=(kk == 31))
                P.op("dve", lambda h, dsl=dsl, pb=pb: h.tensor_tensor(out=ho[:, dsl], in0=k.ps[pb][:], in1=g1b[:, dsl], op=ALU.mult),
                     r=[("ps", pb), "rg1b"], w=["rho"])
                P.op("pool", lambda h, dsl=dsl: h.tensor_tensor(out=ho[:, dsl], in0=ho[:, dsl], in1=hb[:, dsl], op=ALU.add),
                     r=["rho", "rhb"], w=["rho"])
            P.dma("sp", k.hres[tk, :], ho[:], r=["rho"], w=["hres"])
        P.barrier()


def phase_final_norm(k, src, dst):
    nc, P = k.nc, k.P
    with ExitStack() as es:
        hb = [sb(nc, es, f"fhb{i}", [128, D], F32) for i in range(2)]
        tmp = [sb(nc, es, f"ftmp{i}", [128, D], F32) for i in range(2)]
        st = sb(nc, es, "fst", [128, L // 128, 4], F32)
        gb = load_bc(k, es, "fgb", k.final_norm_g, D)
        for tg in range(L // 128):
            i = tg % 2
            P.dma("sp", hb[i][:], src[tg * 128:(tg + 1) * 128, :], r=["hres"], w=[("fhb", i)])
            P.op("act", lambda h, i=i: h.activation(out=tmp[i][:], in_=hb[i][:], func=AF.Square), r=[("fhb", i)], w=[("ftmp", i)])
            P.op("dve", lambda h, i=i, tg=tg: h.tensor_reduce(out=st[:, tg, 0:1], in_=tmp[i][:], axis=AX.X, op=ALU.add),
                 r=[("ftmp", i)], w=[("fst", tg)])
            P.op("dve", lambda h, tg=tg: h.tensor_scalar(out=st[:, tg, 1:2], in0=st[:, tg, 0:1], scalar1=1.0 / D, scalar2=EPS,
                                                         op0=ALU.mult, op1=ALU.add), r=[("fst", tg)], w=[("fst", tg)])
            P.op("act", lambda h, tg=tg: h.activation(out=st[:, tg, 2:3], in_=st[:, tg, 1:2], func=AF.Sqrt), r=[("fst", tg)], w=[("fst", tg)])
            P.op("dve", lambda h, tg=tg: h.reciprocal(out=st[:, tg, 3:4], in_=st[:, tg, 2:3]), r=[("fst", tg)], w=[("fst", tg)])
            P.op("dve", lambda h, i=i, tg=tg: h.scalar_tensor_tensor(out=tmp[i][:], in0=hb[i][:], scalar=st[:, tg, 3:4], in1=gb[:],
                                                                   op0=ALU.mult, op1=ALU.mult),
                 r=[("fhb", i), ("fst", tg), "fgb"], w=[("ftmp", i)])
            P.dma("sp", dst[tg * 128:(tg + 1) * 128, :], tmp[i][:], r=[("ftmp", i)], w=["out"])
        P.barrier()


def build_program():
    nc = bass.Bass("TRN2", target_bir_lowering=False)
    k = K()
    k.nc = nc
    I = lambda name, shape, dt=F32: nc.dram_tensor(name, shape, dt, kind="ExternalInput").ap()
    S = lambda name, shape, dt=F32: nc.dram_tensor(name, shape, dt, kind="Internal").ap()
    k.x = I("x", [L, D])
    k.ctx = I("ctx", [M, D])
    k.c = I("c", [D])
    k.c_ctx = I("c_ctx", [D])
    k.ada_w = I("ada_w", [2, D, NM])
    k.ada_b = I("ada_b", [2, NM])
    k.norm_mix_g = I("norm_mix_g", [2, D])
    k.norm_ffn_g = I("norm_ffn_g", [2, D])
    k.ssd_w_in = I("ssd_w_in", [D, 10368])
    k.ssd_conv_w = I("ssd_conv_w", [5, 6144])
    k.ssd_conv_b = I("ssd_conv_b", [6144])
    k.ssd_dt_bias = I("ssd_dt_bias", [2, 64])
    k.ssd_a_log = I("ssd_a_log", [2, 64])
    k.ssd_d = I("ssd_d", [64])
    k.ssd_norm_g = I("ssd_norm_g", [4096])
    k.ssd_w_out = I("ssd_w_out", [4096, D])
    k.ret_w_in = I("ret_w_in", [D, 12288])
    k.ret_decay = I("ret_decay", [2, 8])
    k.ret_gn_g = I("ret_gn_g", [4096])
    k.ret_w_out = I("ret_w_out", [4096, D])
    k.moe_w_router = I("moe_w_router", [2, D, NE])
    k.moe_w_gate = I("moe_w_gate", [2, NE, D, D])
    k.moe_w_up = I("moe_w_up", [2, NE, D, D])
    k.moe_w_down = I("moe_w_down", [2, NE, D, D])
    k.final_norm_g = I("final_norm_g", [D])
    k.c_ident = I("c_ident", [128, 128])
    k.c_tri = I("c_tri", [5, 128, 128])
    k.c_iota = I("c_iota", [128, 288])
    k.c_jidx = I("c_jidx", [128, 3])
    k.c_sel = I("c_sel", [16, 16, 128])
    k.c_rope = I("c_rope", [2, L, 128])
    k.c_rel = I("c_rel", [2, 128, 128])
    k.c_cnt = I("c_cnt", [128, 2])
    k.out = nc.dram_tensor("out", [L, D], F32, kind="ExternalOutput").ap()
    k.mod = S("mod", [2, 2, NM])
    k.xs_tok = S("xs_tok", [T, 4096], BF16)
    k.b_tok = S("b_tok", [T, 1024], BF16)
    k.bT_s = S("bT_s", [8, 128, T], BF16)
    k.cT_s = S("cT_s", [8, 128, T], BF16)
    k.sz_tok = S("sz_tok", [T, 4096], BF16)
    k.y_s = S("y_s", [2, T, 4096])
    k.hres = S("hres", [T, D])
    k.xeT_d = S("xeT_d", [NE, 128, 16, 288], BF16)
    k.ye_d = S("ye_d", [NE, 128, 3, D], BF16)
    k.qT_s = S("qT_s", [128, 8, 2, L], BF16)
    k.kT_s = S("kT_s", [128, 8, 2, L], BF16)
    k.k_tok = S("k_tok", [T, 2048], BF16)
    k.v_tok = S("v_tok", [T, 4096], BF16)
    k.sg_tok = S("sg_tok", [L, 4096], BF16)
    k.o_s = S("o_s", [2, L, 4096])
    with ExitStack() as es:
        k.P = P = Prog(nc, es)
        k.ps = [es.enter_context(nc.psum_tensor(f"ps{i}", [128, 512], F32)) for i in range(8)]
        build_consts(k, es)
        phase_mod(k, 0)
        phase_mod(k, 1)
        P.barrier()
        with ExitStack() as es1:
            dt_tok = sb(nc, es1, "dt_tok", [128, NT, 128], F32)
            dtA_tok = sb(nc, es1, "dtA_tok", [128, NT, 128], F32)
            k.dt_pre = (dt_tok, dtA_tok)
            with ExitStack() as es0:
                aT = sb(nc, es0, "aT", [128, 16, T], BF16)
                phase_norm_T(k, 0, k.x, k.ctx, k.norm_mix_g[0], 1, 0, aT)
                P.barrier()
                phase_ssd_inproj(k, None, aT)
            phase_ssd_scan(k, dt_tok, dtA_tok)
        phase_ssd_out(k, 0, k.x, k.ctx)
        phase_moe(k, 0, True)
        with ExitStack() as es0:
            aT = sb(nc, es0, "aT", [128, 16, T], BF16)
            phase_norm_T(k, 1, k.hres[0:L], k.hres[L:T], k.norm_mix_g[1], 1, 0, aT)
            P.barrier()
            phase_ret_inproj(k, aT)
        phase_ret_scan(k)
        phase_ret_out(k, 1)
        phase_moe(k, 1, False)
        phase_final_norm(k, k.hres, k.out)
        P.wait_all("sp")
        P.run()
    return nc


def kernel(**inputs):
    f = lambda a: np.ascontiguousarray(np.asarray(a, dtype=np.float32))
    nc = build_program()
    iota, jidx, sel = host_moe_consts()
    rope, rel, cnt = host_ret_consts()
    shared = {
        "c_ctx": f(inputs["c_ctx"]), "ada_w": f(inputs["ada_w"]), "ada_b": f(inputs["ada_b"]),
        "norm_mix_g": f(inputs["norm_mix_g"]), "norm_ffn_g": f(inputs["norm_ffn_g"]),
        "ssd_w_in": f(inputs["ssd_w_in"][0]), "ssd_conv_w": f(inputs["ssd_conv_w"][0]),
        "ssd_conv_b": f(inputs["ssd_conv_b"][0]), "ssd_dt_bias": f(inputs["ssd_dt_bias"][0]), "ssd_a_log": f(inputs["ssd_a_log"][0]),
        "ssd_d": f(inputs["ssd_d"][0]), "ssd_norm_g": f(inputs["ssd_norm_g"][0]), "ssd_w_out": f(inputs["ssd_w_out"][0]),
        "ret_w_in": f(inputs["ret_w_in"][0]), "ret_decay": f(inputs["ret_decay"][0]), "ret_gn_g": f(inputs["ret_gn_g"][0]),
        "ret_w_out": f(inputs["ret_w_out"][0]),
        "moe_w_router": f(inputs["moe_w_router"]), "moe_w_gate": f(inputs["moe_w_gate"]), "moe_w_up": f(inputs["moe_w_up"]),
        "moe_w_down": f(inputs["moe_w_down"]),
        "final_norm_g": f(inputs["final_norm_g"]),
        "c_ident": np.eye(128, dtype=np.float32), "c_tri": host_tri(), "c_iota": iota, "c_jidx": jidx, "c_sel": sel,
        "c_rope": rope, "c_rel": rel, "c_cnt": cnt,
    }
    in_maps = []
    for b in range(8):
        m = dict(shared)
        m["x"] = f(inputs["x"][b])
        m["ctx"] = f(inputs["ctx"][b])
        m["c"] = f(inputs["c"][b])
        in_maps.append(m)
    res = run_bass_kernel_spmd(nc, in_maps, core_ids=list(range(8)))
    return np.stack([np.asarray(r["out"], dtype=np.float32) for r in res.results], axis=0)
```

```python
from contextlib import ExitStack
import numpy as np
import concourse.bass as bass
import concourse.mybir as mybir
from concourse.bass_utils import run_bass_kernel_spmd

F32 = mybir.dt.float32
BF16 = mybir.dt.bfloat16
AF = mybir.ActivationFunctionType
ALU = mybir.AluOpType
AX = mybir.AxisListType

D = 2048
L = 2048
M = 256
T = L + M
NT = T // 128
NM = 6 * D
EPS = 1e-6
ENG = ("sp", "act", "pool", "dve", "pe")
SAME_ENG_SYNC = True


class Prog:
    def __init__(self, nc, es, n_dma_sems=6):
        self.nc = nc
        self.streams = {e: [] for e in ENG}
        self.sem = {e: es.enter_context(nc.semaphore("s_" + e)) for e in ENG}
        self.cnt = {e: 0 for e in ENG}
        self.dsem = {e: [es.enter_context(nc.semaphore(f"d_{e}{i}")) for i in range(n_dma_sems)]
                     for e in ("sp", "act", "pool")}
        self.dcnt = {e: [0] * n_dma_sems for e in ("sp", "act", "pool")}
        self.drr = {e: 0 for e in ("sp", "act", "pool")}
        self.waited = {}
        self.lastw = {}
        self.readers = {}
        self.ninstr = 0
        self.psn = 0
        self.nrot = 8

    def _wait(self, eng, tok):
        if tok is None:
            return
        if tok[0] == "e":
            _, src, val = tok
            if src == eng and (eng == "pe" or not SAME_ENG_SYNC):
                return
            k = (eng, src)
            sem = self.sem[src]
        else:
            _, src, idx, val = tok
            k = (eng, src, idx)
            sem = self.dsem[src][idx]
        if self.waited.get(k, 0) >= val:
            return
        self.waited[k] = val
        self.streams[eng].append(lambda h, sem=sem, val=val: h.wait_ge(sem, val))
        self.ninstr += 1

    def _deps(self, eng, r, w):
        for k in r:
            self._wait(eng, self.lastw.get(k))
        for k in w:
            self._wait(eng, self.lastw.get(k))
            for t in self.readers.get(k, ()):
                self._wait(eng, t)

    def _update(self, tok, r, w):
        for k in r:
            lst = self.readers.setdefault(k, [])
            lst.append(tok)
            if len(lst) > 16:
                d = {}
                for t in lst:
                    kk = t[:-1]
                    if kk not in d or d[kk][-1] < t[-1]:
                        d[kk] = t
                self.readers[k] = list(d.values())
        for k in w:
            self.lastw[k] = tok
            self.readers[k] = []

    def op(self, eng, fn, r=(), w=(), inc=True):
        pr = [x for x in r if isinstance(x, tuple) and x[0] == "ps"]
        if pr:
            r = [x for x in r if x not in pr]
            w = list(w) + pr
        self._deps(eng, r, w)
        if inc:
            self.cnt[eng] += 1
            sem = self.sem[eng]
            self.streams[eng].append(lambda h, fn=fn, sem=sem: fn(h).then_inc(sem, 1))
            tok = ("e", eng, self.cnt[eng])
        else:
            self.streams[eng].append(lambda h, fn=fn: fn(h))
            tok = ("e", eng, self.cnt[eng] + 1)
        self.ninstr += 1
        self._update(tok, r, w)
        return tok

    def dma(self, eng, out, in_, r=(), w=(), **kw):
        i = self.drr[eng]
        self.drr[eng] = (i + 1) % len(self.dsem[eng])
        prev = self.dcnt[eng][i]
        if prev:
            self._wait(eng, ("d", eng, i, prev))
        self._deps(eng, r, w)
        self.dcnt[eng][i] += 16
        sem = self.dsem[eng][i]
        self.streams[eng].append(
            lambda h, out=out, in_=in_, sem=sem, kw=kw: h.dma_start(out=out, in_=in_, **kw).then_inc(sem, 16))
        self.ninstr += 1
        tok = ("d", eng, i, self.dcnt[eng][i])
        self._update(tok, r, w)
        return tok

    def wait_all(self, eng):
        for e in ENG:
            if self.cnt[e]:
                self._wait(eng, ("e", e, self.cnt[e]))
        for e in ("sp", "act", "pool"):
            for i, v in enumerate(self.dcnt[e]):
                if v:
                    self._wait(eng, ("d", e, i, v))

    def barrier(self):
        for e in ENG:
            self.wait_all(e)

    def bank(self):
        i = self.psn % self.nrot
        self.psn = (i + 1) % self.nrot
        return i

    def run(self):
        with self.nc.Block() as block:
            for e, name in (("sp", "sync"), ("act", "scalar"), ("pool", "gpsimd"), ("dve", "vector"), ("pe", "tensor")):
                lst = self.streams[e]

                def body(h, lst=lst):
                    for f in lst:
                        f(h)
                getattr(block, name)(body)


class K:
    pass


_SBN = [0]


def sb(nc, es, name, shape, dt):
    _SBN[0] += 1
    return es.enter_context(nc.sbuf_tensor(f"{name}_{_SBN[0]}", shape, dt))


def phase_mod(k, layer):
    nc, P = k.nc, k.P
    with ExitStack() as es:
        cT = sb(nc, es, "cT", [128, 16, 2], F32)
        sT = sb(nc, es, "sT", [128, 16, 2], F32)
        wt = [sb(nc, es, f"mwt{i}", [128, 16, 512], F32) for i in range(2)]
        bt = [sb(nc, es, f"mbt{i}", [2, 512], F32) for i in range(2)]
        ot = [sb(nc, es, f"mot{i}", [2, 512], F32) for i in range(2)]
        P.dma("sp", cT[:, :, 0], k.c.rearrange("(k p) -> p k", p=128), w=["cT"], allow_slow_non_contiguous=True)
        P.dma("sp", cT[:, :, 1], k.c_ctx.rearrange("(k p) -> p k", p=128), w=["cT"], allow_slow_non_contiguous=True)
        P.op("act", lambda h: h.activation(out=sT[:], in_=cT[:], func=AF.Silu), r=["cT"], w=["sT"])
        wv = k.ada_w[layer].rearrange("(k p) n -> p k n", p=128)
        for j in range(NM // 512):
            i = j % 2
            pb = P.bank()
            ps = k.ps[pb]
            P.dma("sp" if j % 2 == 0 else "act", wt[i][:], wv[:, :, j * 512:(j + 1) * 512], w=[("mwt", i)])
            P.dma("sp", bt[i][:], k.ada_b[layer, j * 512:(j + 1) * 512].partition_broadcast(2), w=[("mbt", i)])
            for kk in range(16):
                P.op("pe", lambda h, kk=kk, i=i, ps=ps: h.matmul(ps[0:2, :], lhsT=sT[:, kk, :], rhs=wt[i][:, kk, :],
                                                                 start=(kk == 0), stop=(kk == 15)),
                     r=["sT", ("mwt", i)], w=[("ps", pb)], inc=(kk == 15))
            P.op("dve", lambda h, i=i, ps=ps: h.tensor_tensor(out=ot[i][:], in0=ps[0:2, :], in1=bt[i][:], op=ALU.add),
                 r=[("ps", pb), ("mbt", i)], w=[("mot", i)])
            P.dma("sp", k.mod[layer, :, j * 512:(j + 1) * 512], ot[i][:], r=[("mot", i)], w=[("mod", layer)])


def load_bc(k, es, name, src_ap, n, eng="sp", key=None):
    t = sb(k.nc, es, name, [128, n], F32)
    k.P.dma(eng, t[:], src_ap.partition_broadcast(128), r=[key] if key else [], w=[name])
    return t


def phase_norm_T(k, layer, src_lat, src_ctx, gvec, sc_i, sh_i, aT, f_tok=None, f32T=None):
    nc, P = k.nc, k.P
    with ExitStack() as es:
        hb = [sb(nc, es, f"hb{i}", [128, D], F32) for i in range(2)]
        tmp = [sb(nc, es, f"ntmp{i}", [128, D], F32) for i in range(2)]
        ab = [sb(nc, es, f"ab{i}", [128, D], BF16) for i in range(2)]
        st = sb(nc, es, "nst", [128, NT, 4], F32)
        gb = load_bc(k, es, "gb", gvec, D)
        for r, (src, t0, nt) in enumerate(((src_lat, 0, L // 128), (src_ctx, L // 128, M // 128))):
            if src is None:
                continue
            scb = load_bc(k, es, f"scb{r}", k.mod[layer, r, sc_i * D:(sc_i + 1) * D], D, key=("mod", layer))
            shb = load_bc(k, es, f"shb{r}", k.mod[layer, r, sh_i * D:(sh_i + 1) * D], D, key=("mod", layer))
            gm = sb(nc, es, f"gm{r}", [128, D], F32)
            P.op("dve", lambda h, gm=gm, scb=scb: h.scalar_tensor_tensor(out=gm[:], in0=scb[:], scalar=1.0, in1=gb[:],
                                                                          op0=ALU.add, op1=ALU.mult),
                 r=[f"scb{r}", "gb"], w=[f"gm{r}"])
            for tt in range(nt):
                tg = t0 + tt
                i = tg % 2
                P.dma("sp", hb[i][:], src[tt * 128:(tt + 1) * 128, :], w=[("hb", i)])
                P.op("act", lambda h, i=i: h.activation(out=tmp[i][:], in_=hb[i][:], func=AF.Square),
                     r=[("hb", i)], w=[("ntmp", i)])
                P.op("dve", lambda h, i=i, tg=tg: h.tensor_reduce(out=st[:, tg, 0:1], in_=tmp[i][:], axis=AX.X, op=ALU.add),
                     r=[("ntmp", i)], w=[("nst", tg)])
                P.op("dve", lambda h, tg=tg: h.tensor_scalar(out=st[:, tg, 1:2], in0=st[:, tg, 0:1], scalar1=1.0 / D, scalar2=EPS,
                                                             op0=ALU.mult, op1=ALU.add), r=[("nst", tg)], w=[("nst", tg)])
                P.op("act", lambda h, tg=tg: h.activation(out=st[:, tg, 2:3], in_=st[:, tg, 1:2], func=AF.Sqrt),
                     r=[("nst", tg)], w=[("nst", tg)])
                P.op("dve", lambda h, tg=tg: h.reciprocal(out=st[:, tg, 3:4], in_=st[:, tg, 2:3]), r=[("nst", tg)], w=[("nst", tg)])
                P.op("dve", lambda h, i=i, tg=tg, gm=gm: h.scalar_tensor_tensor(out=tmp[i][:], in0=hb[i][:], scalar=st[:, tg, 3:4],
                                                                               in1=gm[:], op0=ALU.mult, op1=ALU.mult),
                     r=[("hb", i), ("nst", tg), f"gm{r}"], w=[("ntmp", i)])
                if f32T is None:
                    P.op("pool", lambda h, i=i, shb=shb: h.tensor_tensor(out=ab[i][:], in0=tmp[i][:], in1=shb[:], op=ALU.add),
                         r=[("ntmp", i), f"shb{r}"], w=[("ab", i)])
                    for half in range(2):
                        pb = P.bank()
                        psb = k.ps[pb][:].bitcast(BF16)
                        for j in range(8):
                            kk = half * 8 + j
                            P.op("pe", lambda h, i=i, j=j, kk=kk, psb=psb: h.transpose(out=psb[:, j * 128:(j + 1) * 128],
                                                                                     in_=ab[i][:, kk * 128:(kk + 1) * 128],
                                                                                     identity=k.ident_bf[:]),
                                 r=[("ab", i), "ident"], w=[("ps", pb)], inc=(j == 7))
                        P.op("act", lambda h, half=half, tg=tg, psb=psb: h.activation(
                            out=aT[:, half * 8:(half + 1) * 8, tg * 128:(tg + 1) * 128],
                            in_=psb.rearrange("p (j t) -> p j t", j=8), func=AF.Copy),
                            r=[("ps", pb)], w=[("aT", tg)])
                    if f_tok is not None:
                        P.op("pool", lambda h, i=i, tg=tg: h.tensor_copy(out=f_tok[:, tg, :], in_=ab[i][:]),
                             r=[("ab", i)], w=[("ftok", tg)])


def build_consts(k, es):
    nc, P = k.nc, k.P
    k.ident_f = sb(nc, es, "ident_f", [128, 128], F32)
    k.ident_bf = sb(nc, es, "ident_bf", [128, 128], BF16)
    P.dma("sp", k.ident_f[:], k.c_ident, w=["ident_f"])
    P.op("dve", lambda h: h.tensor_copy(out=k.ident_bf[:], in_=k.ident_f[:]), r=["ident_f"], w=["ident"])


XB = 4096
UW = T + 8


def phase_ssd_inproj(k, es_out, aT):
    nc, P = k.nc, k.P
    if es_out is None:
        dt_tok, dtA_tok = k.dt_pre
    else:
        dt_tok = sb(nc, es_out, "dt_tok", [128, NT, 128], F32)
        dtA_tok = sb(nc, es_out, "dtA_tok", [128, NT, 128], F32)
    wv = k.ssd_w_in.rearrange("(k p) n -> p k n", p=128)
    with ExitStack() as es:
        cwT = sb(nc, es, "cwT", [128, 48, 6], F32)
        with ExitStack() as es2:
            cw6 = sb(nc, es2, "cw6", [6, 6144], F32)
            P.dma("sp", cw6[0:5, :], k.ssd_conv_w, w=["cw6"])
            P.dma("sp", cw6[5:6, :], k.ssd_conv_b.partition_broadcast(1), w=["cw6"])
            pb = P.bank()
            for j in range(48):
                P.op("pe", lambda h, j=j, pb=pb: h.transpose(out=k.ps[pb][:, j * 6:(j + 1) * 6], in_=cw6[:, j * 128:(j + 1) * 128],
                                                            identity=k.ident_f[0:6, 0:6]),
                     r=["cw6", "ident_f"], w=[("ps", pb)], inc=(j == 47))
            P.op("dve", lambda h, pb=pb: h.tensor_copy(out=cwT[:], in_=k.ps[pb][:, 0:288].rearrange("p (j s) -> p j s", s=6)),
                 r=[("ps", pb)], w=["cwT"])
            P.barrier()
        wt = [sb(nc, es, f"wt{i}", [128, 16, 512], BF16) for i in range(2)]
        U = [sb(nc, es, f"U{i}", [128, UW], F32) for i in range(2)]
        acc = [sb(nc, es, f"acc{i}", [128, T], F32) for i in range(2)]
        S = [sb(nc, es, f"S{i}", [128, T], BF16) for i in range(2)]
        stage = [sb(nc, es, f"stg{i}", [128, NT, 512], BF16) for i in range(2)]
        one = sb(nc, es, "onec", [128, 1], F32)
        P.op("pool", lambda h: h.memset(one[:], 1.0), w=["onec"])
        for i in range(2):
            P.op("pool", lambda h, i=i: h.memset(U[i][:], 0.0), w=[("U", i)])
        dtb = load_bc(k, es, "dtb", k.ssd_dt_bias.rearrange("a b -> (a b)"), 128)
        alog = load_bc(k, es, "alog", k.ssd_a_log.rearrange("a b -> (a b)"), 128)
        Abc = sb(nc, es, "Abc", [128, 128], F32)
        P.op("act", lambda h: h.activation(out=Abc[:], in_=alog[:], func=AF.Exp), r=["alog"], w=["Abc"])
        P.op("dve", lambda h: h.tensor_scalar(out=Abc[:], in0=Abc[:], scalar1=-1.0, scalar2=None, op0=ALU.mult), r=["Abc"], w=["Abc"])

        nw = [0]

        def load_w(c0, ncols=512):
            i = nw[0] % 2
            nw[0] += 1
            P.dma("pool", wt[i][:, :, 0:ncols], wv[:, :, c0:c0 + ncols], w=[("wt", i)])
            return i

        tok_blocks = [(tb * 512, 512) for tb in range(4)] + [(L, M)]
        u_off = lambda t0: (2 + t0) if t0 < L else (L + 6 + (t0 - L))
        nblk = 0
        for ti in range(12):
            wi = load_w(XB + ti * 512)
            si = ti % 2
            for jb in range(4):
                cb = ti * 4 + jb
                ui = nblk % 2
                nblk += 1
                for (t0, n) in tok_blocks:
                    pb = P.bank()
                    for kk in range(16):
                        P.op("pe", lambda h, kk=kk, wi=wi, jb=jb, t0=t0, n=n, pb=pb: h.matmul(
                            k.ps[pb][:, 0:n], lhsT=wt[wi][:, kk, jb * 128:(jb + 1) * 128], rhs=aT[:, kk, t0:t0 + n],
                            start=(kk == 0), stop=(kk == 15)),
                            r=[("wt", wi)] + [("aT", t0 // 128 + q) for q in range(n // 128)], w=[("ps", pb)], inc=(kk == 15))
                    P.op("act", lambda h, ui=ui, t0=t0, n=n, pb=pb: h.activation(out=U[ui][:, u_off(t0):u_off(t0) + n],
                                                                               in_=k.ps[pb][:, 0:n], func=AF.Copy),
                         r=[("ps", pb)], w=[("U", ui)])
                for (a0, u0, n) in ((0, 0, L), (L, L + 4, M)):
                    P.op("dve", lambda h, ui=ui, cb=cb, a0=a0, u0=u0, n=n: h.tensor_scalar(
                        out=acc[ui][:, a0:a0 + n], in0=U[ui][:, u0:u0 + n], scalar1=cwT[:, cb, 0:1], scalar2=cwT[:, cb, 5:6],
                        op0=ALU.mult, op1=ALU.add), r=[("U", ui), "cwT"], w=[("acc", ui)])
                    for tap in range(1, 5):
                        P.op("dve", lambda h, ui=ui, cb=cb, a0=a0, u0=u0, n=n, tap=tap: h.scalar_tensor_tensor(
                            out=acc[ui][:, a0:a0 + n], in0=U[ui][:, u0 + tap:u0 + tap + n], scalar=cwT[:, cb, tap:tap + 1],
                            in1=acc[ui][:, a0:a0 + n], op0=ALU.mult, op1=ALU.add), r=[("U", ui), "cwT", ("acc", ui)], w=[("acc", ui)])
                P.op("act", lambda h, ui=ui: h.activation(out=S[ui][:], in_=acc[ui][:], func=AF.Silu),
                     r=[("acc", ui)], w=[("S", ui)])
                if cb < 40:
                    for t8 in range(0, NT, 8):
                        nn = min(8, NT - t8)
                        pb = P.bank()
                        psb = k.ps[pb][:].bitcast(BF16)
                        for j in range(nn):
                            P.op("pe", lambda h, ui=ui, j=j, t8=t8, psb=psb: h.transpose(
                                out=psb[:, j * 128:(j + 1) * 128], in_=S[ui][:, (t8 + j) * 128:(t8 + j + 1) * 128],
                                identity=k.ident_bf[:]), r=[("S", ui), "ident"], w=[("ps", pb)], inc=(j == nn - 1))
                        P.op("dve", lambda h, si=si, jb=jb, t8=t8, nn=nn, psb=psb: h.tensor_copy(
                            out=stage[si][:, t8:t8 + nn, jb * 128:(jb + 1) * 128],
                            in_=psb[:, 0:nn * 128].rearrange("p (j c) -> p j c", c=128)),
                            r=[("ps", pb)], w=[("stg", si)])
                if 32 <= cb < 40:
                    P.dma("sp", k.bT_s[cb - 32], S[ui][:], r=[("S", ui)], w=["bT_s"])
                if cb >= 40:
                    P.dma("sp", k.cT_s[cb - 40], S[ui][:], r=[("S", ui)], w=["cT_s"])
            if ti < 8:
                P.dma("sp", k.xs_tok.rearrange("(g p) c -> p g c", p=128)[:, :, ti * 512:(ti + 1) * 512], stage[si][:],
                      r=[("stg", si)], w=["xs_tok"])
            elif ti < 10:
                P.dma("sp", k.b_tok.rearrange("(g p) c -> p g c", p=128)[:, :, (ti - 8) * 512:(ti - 7) * 512], stage[si][:],
                      r=[("stg", si)], w=["b_tok"])
        for ti in range(8):
            wi = load_w(ti * 512)
            si = ti % 2
            for tg in range(NT):
                pb = P.bank()
                for kk in range(16):
                    P.op("pe", lambda h, kk=kk, wi=wi, tg=tg, pb=pb: h.matmul(
                        k.ps[pb][:], lhsT=aT[:, kk, tg * 128:(tg + 1) * 128], rhs=wt[wi][:, kk, :],
                        start=(kk == 0), stop=(kk == 15)), r=[("wt", wi), ("aT", tg)], w=[("ps", pb)], inc=(kk == 15))
                P.op("act", lambda h, si=si, tg=tg, pb=pb: h.activation(out=stage[si][:, tg, :], in_=k.ps[pb][:], func=AF.Silu),
                     r=[("ps", pb)], w=[("stg", si)])
            P.dma("sp", k.sz_tok.rearrange("(g p) c -> p g c", p=128)[:, :, ti * 512:(ti + 1) * 512], stage[si][:],
                  r=[("stg", si)], w=["sz_tok"])
        wi = load_w(XB + 6144, 128)
        etmp = sb(nc, es, "etmp", [128, 128], F32)
        for tg in range(NT):
            pb = P.bank()
            for kk in range(16):
                P.op("pe", lambda h, kk=kk, wi=wi, tg=tg, pb=pb: h.matmul(
                    k.ps[pb][:, 0:128], lhsT=aT[:, kk, tg * 128:(tg + 1) * 128], rhs=wt[wi][:, kk, 0:128],
                    start=(kk == 0), stop=(kk == 15)), r=[("wt", wi), ("aT", tg)], w=[("ps", pb)], inc=(kk == 15))
            P.op("dve", lambda h, pb=pb: h.tensor_tensor(out=etmp[:], in0=k.ps[pb][:, 0:128], in1=dtb[:], op=ALU.add),
                 r=[("ps", pb), "dtb"], w=["etmp"])
            P.op("act", lambda h: h.activation(out=etmp[:], in_=etmp[:], func=AF.Exp), r=["etmp"], w=["etmp"])
            P.op("act", lambda h, tg=tg: h.activation(out=dt_tok[:, tg, :], in_=etmp[:], func=AF.Ln, bias=one[:]),
                 r=["etmp", "onec"], w=[("dt", tg)])
            P.op("dve", lambda h, tg=tg: h.tensor_tensor(out=dtA_tok[:, tg, :], in0=dt_tok[:, tg, :], in1=Abc[:], op=ALU.mult),
                 r=[("dt", tg), "Abc"], w=[("dtA", tg)])
        P.barrier()
    return dt_tok, dtA_tok


def host_tri():
    i = np.arange(128)
    kk, ll = i[:, None], i[None, :]
    return np.stack([(kk <= ll), (kk >= ll), (kk > ll), (kk < ll), np.ones((128, 128), bool)]).astype(np.float32)


def fillreg(k, h):
    if getattr(k, "_fillreg", None) is None:
        k._fillreg = h.to_reg(-30000.0)
    return k._fillreg


def bc3(ap2, n):
    return ap2.unsqueeze(2).to_broadcast([ap2.shape[0], ap2.shape[1], n])


def phase_ssd_scan(k, dt_tok, dtA_tok, groups=range(8), stage=99, dirs=(0, 1), maxchunks=99):
    nc, P = k.nc, k.P
    with ExitStack() as es:
        tri = sb(nc, es, "tri", [128, 5, 128], F32)
        P.dma("sp", tri[:], k.c_tri.rearrange("a p l -> p a l"), w=["tri"])
        cs_tok = sb(nc, es, "cs_tok", [128, NT, 128], F32)
        E3 = sb(nc, es, "E3", [128, NT, 384], F32)
        for tg in range(NT):
            pb = P.bank()
            ps = k.ps[pb]
            for d in range(2):
                P.op("pe", lambda h, d=d, tg=tg, ps=ps: h.matmul(ps[:, d * 64:(d + 1) * 64], lhsT=tri[:, d, :],
                                                                rhs=dtA_tok[:, tg, d * 64:(d + 1) * 64], start=True, stop=True),
                     r=["tri", ("dtA", tg)], w=[("ps", pb)], inc=False)
                P.op("pe", lambda h, d=d, tg=tg, ps=ps: h.matmul(ps[:, 128 + d * 64:128 + (d + 1) * 64], lhsT=tri[:, 2 + d, :],
                                                                rhs=dtA_tok[:, tg, d * 64:(d + 1) * 64], start=True, stop=True),
                     r=["tri", ("dtA", tg)], w=[("ps", pb)], inc=False)
            P.op("pe", lambda h, tg=tg, ps=ps: h.matmul(ps[:, 256:384], lhsT=tri[:, 4, :], rhs=dtA_tok[:, tg, :], start=True, stop=True),
                 r=["tri", ("dtA", tg)], w=[("ps", pb)])
            P.op("dve", lambda h, tg=tg, ps=ps: h.tensor_copy(out=cs_tok[:, tg, :], in_=ps[:, 0:128]), r=[("ps", pb)], w=[("cs", tg)])
            P.op("act", lambda h, tg=tg, ps=ps: h.activation(out=E3[:, tg, :], in_=ps[:, 0:384], func=AF.Exp),
                 r=[("ps", pb)], w=[("E3", tg)])
        BT = [sb(nc, es, f"BT{i}", [128, T], BF16) for i in range(2)]
        CT = [sb(nc, es, f"CT{i}", [128, T], BF16) for i in range(2)]
        Bt = [sb(nc, es, f"Bt{i}", [128, NT, 128], BF16) for i in range(2)]
        Xs = [sb(nc, es, f"Xs{i}", [128, NT, 512], BF16) for i in range(2)]
        xdt = [sb(nc, es, f"xdt{i}", [128, 512], BF16) for i in range(2)]
        xw = [sb(nc, es, f"xw{i}", [128, 512], BF16) for i in range(2)]
        Gs = [sb(nc, es, f"Gs{i}", [128, 128], BF16) for i in range(2)]
        arg = [sb(nc, es, f"arg{i}", [128, 512], F32) for i in range(2)]
        Em = [sb(nc, es, f"Em{i}", [128, 512], BF16) for i in range(2)]
        Mt = [sb(nc, es, f"Mt{i}", [128, 512], BF16) for i in range(2)]
        yo = [sb(nc, es, f"yo{i}", [128, 512], F32) for i in range(2)]
        Dg = [sb(nc, es, f"Dg{i}", [128, 512], F32) for i in range(2)]
        hst = sb(nc, es, "hst", [128, 512], F32)
        hbf = sb(nc, es, "hbf", [128, 512], BF16)
        nh = [0]
        P.nrot = 6
        for gi, g in enumerate(groups):
            r = gi % 2
            P.dma("sp", BT[r][:], k.bT_s[g], r=["bT_s"], w=[("BT", r)])
            P.dma("act", CT[r][:], k.cT_s[g], r=["cT_s"], w=[("CT", r)])
            P.dma("sp", Bt[r][:], k.b_tok.rearrange("(t p) c -> p t c", p=128)[:, :, g * 128:(g + 1) * 128], r=["b_tok"], w=[("Bt", r)])
            P.dma("act", Xs[r][:], k.xs_tok.rearrange("(t p) c -> p t c", p=128)[:, :, g * 512:(g + 1) * 512], r=["xs_tok"], w=[("Xs", r)])
            for d in dirs:
                P.op("pool", lambda h: h.memset(hst[:], 0.0), w=["hst"])
                P.op("pool", lambda h: h.memset(hbf[:], 0.0), w=["hbf"])
                order = [16, 17] + list(range(16)) if d == 0 else [17, 16] + list(range(15, -1, -1))
                sgn = 1 if d == 0 else -1
                dh0 = d * 64 + g * 8
                def gen_pre(tg, q):
                    tk = slice(tg * 128, (tg + 1) * 128)
                    P.op("dve", lambda h, q=q, r=r, tg=tg, dh0=dh0: h.tensor_tensor(
                        out=xdt[q][:].rearrange("p (e c) -> p e c", c=64), in0=Xs[r][:, tg, :].rearrange("p (e c) -> p e c", c=64),
                        in1=bc3(dt_tok[:, tg, dh0:dh0 + 8], 64), op=ALU.mult), r=[("Xs", r), ("dt", tg)], w=[("xdt", q)])
                    yield
                    P.op("dve", lambda h, q=q, tg=tg, dh0=dh0: h.tensor_tensor(
                        out=xw[q][:].rearrange("p (e c) -> p e c", c=64), in0=xdt[q][:].rearrange("p (e c) -> p e c", c=64),
                        in1=bc3(E3[:, tg, 128 + dh0:128 + dh0 + 8], 64), op=ALU.mult), r=[("xdt", q), ("E3", tg)], w=[("xw", q)])
                    yield
                    pg = P.bank()
                    P.op("pe", lambda h, r=r, tk=tk, pg=pg: h.matmul(k.ps[pg][:, 0:128], lhsT=BT[r][:, tk], rhs=CT[r][:, tk],
                                                                    start=True, stop=True), r=[("BT", r), ("CT", r)], w=[("ps", pg)])
                    yield
                    P.op("act", lambda h, q=q, pg=pg: h.activation(out=Gs[q][:], in_=k.ps[pg][:, 0:128], func=AF.Copy),
                         r=[("ps", pg)], w=[("Gs", q)])
                    yield

                def gen_half(tg, q, half, py):
                    hq = half
                    pr = P.bank()
                    c0 = dh0 + half * 4
                    P.op("pool", lambda h, hq=hq, tg=tg, c0=c0: h.tensor_tensor(
                        out=Dg[hq][:].rearrange("p (e c) -> p e c", c=128), in0=k.ident_f[:].unsqueeze(1).to_broadcast([128, 4, 128]),
                        in1=bc3(cs_tok[:, tg, c0:c0 + 4], 128), op=ALU.mult), r=[("cs", tg), "ident_f"], w=[("Dg", hq)])
                    yield
                    P.op("pe", lambda h, hq=hq, pr=pr: h.matmul(k.ps[pr][:], lhsT=tri[:, 4, :], rhs=Dg[hq][:], start=True, stop=True),
                         r=[("Dg", hq), "tri"], w=[("ps", pr)])
                    yield
                    P.op("dve", lambda h, hq=hq, pr=pr, tg=tg, c0=c0: h.tensor_tensor(
                        out=arg[hq][:].rearrange("p (e c) -> p e c", c=128), in0=k.ps[pr][:].rearrange("p (e c) -> p e c", c=128),
                        in1=bc3(cs_tok[:, tg, c0:c0 + 4], 128), op=ALU.subtract), r=[("ps", pr), ("cs", tg)], w=[("arg", hq)])
                    yield
                    P.op("pool", lambda h, hq=hq, sgn=sgn: h.affine_select(
                        out=arg[hq][:].rearrange("p (e c) -> p e c", c=128), in_=arg[hq][:].rearrange("p (e c) -> p e c", c=128),
                        pattern=[[0, 4], [sgn, 128]], compare_op=ALU.is_ge, fill=fillreg(k, h), base=0, channel_multiplier=-sgn),
                        r=[("arg", hq)], w=[("arg", hq)])
                    yield
                    P.op("act", lambda h, hq=hq: h.activation(out=Em[hq][:], in_=arg[hq][:], func=AF.Exp),
                         r=[("arg", hq)], w=[("Em", hq)])
                    yield
                    P.op("dve", lambda h, hq=hq, q=q: h.tensor_tensor(
                        out=Mt[hq][:].rearrange("p (e c) -> p e c", c=128), in0=Em[hq][:].rearrange("p (e c) -> p e c", c=128),
                        in1=Gs[q][:].unsqueeze(1).to_broadcast([128, 4, 128]), op=ALU.mult),
                        r=[("Em", hq), ("Gs", q)], w=[("Mt", hq)])
                    yield
                    for j in range(4):
                        e = half * 4 + j
                        P.op("pe", lambda h, hq=hq, j=j, e=e, q=q, py=py: h.matmul(
                            k.ps[py][:, e * 64:(e + 1) * 64], lhsT=Mt[hq][:, j * 128:(j + 1) * 128], rhs=xdt[q][:, e * 64:(e + 1) * 64],
                            start=True, stop=True), r=[("Mt", hq), ("xdt", q)], w=[("ps", py)], inc=(j == 3))
                    yield

                def gen_b(tg, q, py):
                    tk = slice(tg * 128, (tg + 1) * 128)
                    pys = P.bank()
                    P.op("pe", lambda h, r=r, tk=tk, pys=pys: h.matmul(k.ps[pys][:], lhsT=CT[r][:, tk], rhs=hbf[:], start=True, stop=True),
                         r=[("CT", r), "hbf"], w=[("ps", pys)])
                    yield
                    pd = P.bank()
                    P.op("pe", lambda h, r=r, tg=tg, q=q, pd=pd: h.matmul(k.ps[pd][:], lhsT=Bt[r][:, tg, :], rhs=xw[q][:],
                                                                        start=True, stop=True), r=[("Bt", r), ("xw", q)], w=[("ps", pd)])
                    yield
                    P.op("dve", lambda h, tg=tg, dh0=dh0: h.tensor_tensor(
                        out=hst[:].rearrange("p (e c) -> p e c", c=64), in0=hst[:].rearrange("p (e c) -> p e c", c=64),
                        in1=bc3(E3[:, tg, 256 + dh0:256 + dh0 + 8], 64), op=ALU.mult), r=["hst", ("E3", tg)], w=["hst"])
                    yield
                    P.op("dve", lambda h, pd=pd: h.tensor_tensor(out=hst[:], in0=k.ps[pd][:], in1=hst[:], op=ALU.add),
                         r=[("ps", pd), "hst"], w=["hst"])
                    yield
                    P.op("act", lambda h: h.activation(out=hbf[:], in_=hst[:], func=AF.Copy), r=["hst"], w=["hbf"])
                    yield
                    P.op("dve", lambda h, q=q, pys=pys, tg=tg, dh0=dh0: h.tensor_tensor(
                        out=yo[q][:].rearrange("p (e c) -> p e c", c=64), in0=k.ps[pys][:].rearrange("p (e c) -> p e c", c=64),
                        in1=bc3(E3[:, tg, dh0:dh0 + 8], 64), op=ALU.mult), r=[("ps", pys), ("E3", tg)], w=[("yo", q)])
                    yield
                    P.op("dve", lambda h, q=q, py=py: h.tensor_tensor(out=yo[q][:], in0=k.ps[py][:], in1=yo[q][:], op=ALU.add),
                         r=[("ps", py), ("yo", q)], w=[("yo", q)])
                    yield
                    P.dma("sp", k.y_s[d, tk, g * 512:(g + 1) * 512], yo[q][:], r=[("yo", q)], w=["y_s"])
                    yield

                def merge(gens):
                    gens = list(gens)
                    while gens:
                        for gnr in list(gens):
                            try:
                                next(gnr)
                            except StopIteration:
                                gens.remove(gnr)

                order = order[:maxchunks]
                pend = None
                for ci, tg in enumerate(order):
                    q = ci % 2
                    py = 6 + q
                    gens = [gen_pre(tg, q), gen_half(tg, q, 0, py), gen_half(tg, q, 1, py)]
                    if pend is not None:
                        gens.append(gen_b(*pend))
                    merge(gens)
                    pend = (tg, q, py)
                merge([gen_b(*pend)])
        P.barrier()
        P.nrot = 8


def phase_ssd_out(k, layer, src_lat, src_ctx):
    nc, P = k.nc, k.P
    with ExitStack() as es:
        W = sb(nc, es, "Wout", [128, 32, D], BF16)
        ngT = sb(nc, es, "ngT", [128, 32], F32)
        P.dma("sp", ngT[:], k.ssd_norm_g.rearrange("(k p) -> p k", p=128), w=["ngT"], allow_slow_non_contiguous=True)
        wv = k.ssd_w_out.rearrange("(k p) n -> p k n", p=128)
        yf = sb(nc, es, "yf", [128, 2048], F32)
        yb = sb(nc, es, "yb", [128, 2048], F32)
        for kk in range(32):
            stg = yf if kk % 2 == 0 else yb
            key = "yf" if kk % 2 == 0 else "yb"
            P.dma("sp" if kk % 2 == 0 else "act", stg[:], wv[:, kk, :], w=[key])
            P.op("dve" if kk % 2 == 0 else "pool", lambda h, kk=kk, stg=stg: h.tensor_scalar(
                out=W[:, kk, :], in0=stg[:], scalar1=ngT[:, kk:kk + 1], scalar2=None, op0=ALU.mult), r=[key, "ngT"], w=["Wout"])
        xs = sb(nc, es, "xs4", [128, 4096], BF16)
        sz = sb(nc, es, "sz4", [128, 4096], BF16)
        yzb = sb(nc, es, "yzb", [128, 4096], BF16)
        ynT = sb(nc, es, "ynT", [128, 32, 128], BF16)
        hb = sb(nc, es, "hb4", [128, D], F32)
        ho = sb(nc, es, "ho4", [128, D], F32)
        st = sb(nc, es, "st4", [128, 8], F32)
        Dbc = load_bc(k, es, "Dbc", k.ssd_d, 64)
        g1b = sb(nc, es, "g1bc", [128, D], F32)
        for tg in range(NT):
            lat = tg < L // 128
            src = src_lat[tg * 128:(tg + 1) * 128, :] if lat else src_ctx[(tg - 16) * 128:(tg - 15) * 128, :]
            if tg in (0, L // 128):
                P.dma("sp", g1b[:], k.mod[layer, 0 if lat else 1, 2 * D:3 * D].partition_broadcast(128), r=[("mod", layer)], w=["g1bc"])
            tk = slice(tg * 128, (tg + 1) * 128)
            P.dma("act", xs[:], k.xs_tok[tk, :], r=["xs_tok"], w=["xs4"])
            P.dma("act", sz[:], k.sz_tok[tk, :], r=["sz_tok"], w=["sz4"])

            def g_half(half, A, B, ka, kb):
                cs_ = slice(half * 2048, (half + 1) * 2048)
                P.dma("sp", A[:], k.y_s[0, tk, cs_], r=["y_s"], w=[ka])
                P.dma("sp", B[:], k.y_s[1, tk, cs_], r=["y_s"], w=[kb])
                yield
                P.op("dve", lambda h: h.tensor_tensor(out=A[:], in0=A[:], in1=B[:], op=ALU.add), r=[ka, kb], w=[ka])
                yield
                P.op("pool", lambda h: h.tensor_tensor(
                    out=B[:].rearrange("p (e c) -> p e c", c=64), in0=xs[:, cs_].rearrange("p (e c) -> p e c", c=64),
                    in1=bc3(Dbc[:, half * 32:(half + 1) * 32], 64), op=ALU.mult), r=["xs4", "Dbc", ka], w=[kb])
                yield
                P.op("dve", lambda h: h.tensor_tensor(out=A[:], in0=A[:], in1=B[:], op=ALU.add), r=[ka, kb], w=[ka])
                yield
                P.op("dve", lambda h: h.tensor_tensor(out=A[:], in0=A[:], in1=sz[:, cs_], op=ALU.mult), r=[ka, "sz4"], w=[ka])
                yield
                P.op("act", lambda h: h.activation(out=B[:], in_=A[:], func=AF.Square), r=[ka], w=[kb])
                yield
                P.op("dve", lambda h: h.tensor_reduce(out=st[:, half:half + 1], in_=B[:], axis=AX.X, op=ALU.add), r=[kb], w=["st4"])
                yield
                P.op("pool", lambda h: h.tensor_copy(out=yzb[:, cs_], in_=A[:]), r=[ka], w=["yzb"])
                yield

            gens = [g_half(0, yf, yb, "yf", "yb"), g_half(1, ho, hb, "ho4", "hb4")]
            while gens:
                for gnr in list(gens):
                    try:
                        next(gnr)
                    except StopIteration:
                        gens.remove(gnr)
            P.dma("sp", hb[:], src, w=["hb4"])
            P.op("dve", lambda h: h.tensor_tensor(out=st[:, 2:3], in0=st[:, 0:1], in1=st[:, 1:2], op=ALU.add), r=["st4"], w=["st4"])
            P.op("dve", lambda h: h.tensor_scalar(out=st[:, 3:4], in0=st[:, 2:3], scalar1=1.0 / XB, scalar2=EPS, op0=ALU.mult, op1=ALU.add),
                 r=["st4"], w=["st4"])
            P.op("act", lambda h: h.activation(out=st[:, 4:5], in_=st[:, 3:4], func=AF.Sqrt), r=["st4"], w=["st4"])
            P.op("dve", lambda h: h.reciprocal(out=st[:, 5:6], in_=st[:, 4:5]), r=["st4"], w=["st4"])
            for k8 in range(4):
                pb = P.bank()
                psb = k.ps[pb][:].bitcast(BF16)
                for j in range(8):
                    kk = k8 * 8 + j
                    P.op("pe", lambda h, j=j, kk=kk, psb=psb: h.transpose(out=psb[:, j * 128:(j + 1) * 128],
                                                                       in_=yzb[:, kk * 128:(kk + 1) * 128], identity=k.ident_bf[:]),
                         r=["yzb", "ident"], w=[("ps", pb)], inc=(j == 7))
                P.op("act" if k8 % 2 == 0 else "dve", (lambda h, k8=k8, psb=psb: h.activation(
                    out=ynT[:, k8 * 8:(k8 + 1) * 8, :], in_=psb.rearrange("p (j t) -> p j t", j=8), func=AF.Copy)) if k8 % 2 == 0 else
                    (lambda h, k8=k8, psb=psb: h.tensor_copy(out=ynT[:, k8 * 8:(k8 + 1) * 8, :], in_=psb.rearrange("p (j t) -> p j t", j=8))),
                    r=[("ps", pb)], w=["ynT"])
            for db in range(4):
                pb = P.bank()
                dsl = slice(db * 512, (db + 1) * 512)
                for kk in range(32):
                    P.op("pe", lambda h, kk=kk, dsl=dsl, pb=pb: h.matmul(k.ps[pb][:], lhsT=ynT[:, kk, :], rhs=W[:, kk, dsl],
                                                                        start=(kk == 0), stop=(kk == 31)),
                         r=["ynT", "Wout"], w=[("ps", pb)], inc=(kk == 31))
                P.op("dve", lambda h, dsl=dsl, pb=pb: h.scalar_tensor_tensor(
                    out=ho[:, dsl], in0=k.ps[pb][:], scalar=st[:, 5:6], in1=g1b[:, dsl], op0=ALU.mult, op1=ALU.mult),
                    r=[("ps", pb), "st4", "g1bc"], w=["ho4"])
                P.op("pool", lambda h, dsl=dsl: h.tensor_tensor(out=ho[:, dsl], in0=ho[:, dsl], in1=hb[:, dsl], op=ALU.add),
                     r=["ho4", "hb4"], w=["ho4"])
            P.dma("sp", k.hres[tk, :], ho[:], r=["ho4"], w=["hres"])
        P.barrier()


NE = 16
CAPL = 256
CAPC = 32


def host_moe_consts():
    iota = np.tile(np.arange(288, dtype=np.float32)[None, :], (128, 1))
    jidx = (np.arange(128, dtype=np.float32)[:, None] + 128.0 * np.arange(3, dtype=np.float32)[None, :])
    sel = np.zeros((16, 16, 128), np.float32)
    for e in range(16):
        sel[e, e, :] = 1.0
    return iota, np.ascontiguousarray(jidx), sel


def phase_moe(k, layer, with_ctx):
    nc, P = k.nc, k.P
    ntl = L // 128
    nt = NT if with_ctx else ntl
    ntok = nt * 128
    ns = 288 if with_ctx else 256
    sets = [(0, ntl, CAPL, 0)] + ([(ntl, 2, CAPC, 256)] if with_ctx else [])
    jts = [(0, 128), (128, 128)] + ([(256, 32)] if with_ctx else [])
    with ExitStack() as es:
        gate_all = sb(nc, es, "gate_all", [128, NE, 3], F32)
        slotT = sb(nc, es, "slotT", [16, T], F32)
        with ExitStack() as esA:
            f_tok = sb(nc, esA, "f_tok", [128, NT, D], BF16)
            aff_tok = sb(nc, esA, "aff_tok", [128, NT, NE], F32)
            affhl = sb(nc, esA, "affhl", [128, NT, NE, 2], BF16)
            slot_tok = sb(nc, esA, "slot_tok", [128, NT, NE], F32)
            iota = sb(nc, esA, "iota", [128, 288], F32)
            P.dma("sp", iota[:], k.c_iota, w=["iota"])
            with ExitStack() as esN:
                affT = sb(nc, esN, "affT", [16, T], F32)
                work = sb(nc, esN, "work", [16, T], F32)
                mask = sb(nc, esN, "mask", [16, T], F32)
                pos = sb(nc, esN, "pos", [16, T], F32)
                hb = [sb(nc, esN, f"mhb{i}", [128, D], F32) for i in range(2)]
                tmp = sb(nc, esN, "mtmp", [128, D], F32)
                a32 = sb(nc, esN, "ma32", [128, D], F32)
                fT32 = sb(nc, esN, "fT32", [128, 16, 128], F32)
                st = sb(nc, esN, "mst", [128, NT, 8], F32)
                e16 = sb(nc, esN, "e16", [128, NE], F32)
                lo32 = sb(nc, esN, "lo32", [128, NE], F32)
                m8 = sb(nc, esN, "m8", [16, 8], F32)
                wr = sb(nc, esN, "wr", [128, 16, NE], F32)
                P.dma("sp", wr[:], k.moe_w_router[layer].rearrange("(k p) e -> p k e", p=128), w=["wr"])
                gb = load_bc(k, esN, "mgb", k.norm_ffn_g[layer], D)
                scb = sb(nc, esN, "mscb", [128, D], F32)
                shb = sb(nc, esN, "mshb", [128, D], F32)
                gm = sb(nc, esN, "mgm", [128, D], F32)
                for (t0, ntile, cap, soff) in sets:
                    r = 0 if t0 == 0 else 1
                    P.dma("sp", scb[:], k.mod[layer, r, 4 * D:5 * D].partition_broadcast(128), r=[("mod", layer)], w=["mscb"])
                    P.dma("sp", shb[:], k.mod[layer, r, 3 * D:4 * D].partition_broadcast(128), r=[("mod", layer)], w=["mshb"])
                    P.op("dve", lambda h: h.scalar_tensor_tensor(out=gm[:], in0=scb[:], scalar=1.0, in1=gb[:], op0=ALU.add, op1=ALU.mult),
                         r=["mscb", "mgb"], w=["mgm"])
                    for tg in range(t0, t0 + ntile):
                        i = tg % 2
                        P.dma("sp", hb[i][:], k.hres[tg * 128:(tg + 1) * 128, :], r=["hres"], w=[("mhb", i)])
                        P.op("act", lambda h, i=i: h.activation(out=tmp[:], in_=hb[i][:], func=AF.Square), r=[("mhb", i)], w=["mtmp"])
                        P.op("dve", lambda h, tg=tg: h.tensor_reduce(out=st[:, tg, 0:1], in_=tmp[:], axis=AX.X, op=ALU.add),
                             r=["mtmp"], w=[("mst", tg)])
                        P.op("dve", lambda h, tg=tg: h.tensor_scalar(out=st[:, tg, 1:2], in0=st[:, tg, 0:1], scalar1=1.0 / D, scalar2=EPS,
                                                                     op0=ALU.mult, op1=ALU.add), r=[("mst", tg)], w=[("mst", tg)])
                        P.op("act", lambda h, tg=tg: h.activation(out=st[:, tg, 2:3], in_=st[:, tg, 1:2], func=AF.Sqrt),
                             r=[("mst", tg)], w=[("mst", tg)])
                        P.op("dve", lambda h, tg=tg: h.reciprocal(out=st[:, tg, 3:4], in_=st[:, tg, 2:3]), r=[("mst", tg)], w=[("mst", tg)])
                        P.op("dve", lambda h, i=i, tg=tg: h.scalar_tensor_tensor(out=tmp[:], in0=hb[i][:], scalar=st[:, tg, 3:4], in1=gm[:],
                                                                               op0=ALU.mult, op1=ALU.mult),
                             r=[("mhb", i), ("mst", tg), "mgm"], w=["mtmp"])
                        P.op("pool", lambda h: h.tensor_tensor(out=a32[:], in0=tmp[:], in1=shb[:], op=ALU.add),
                             r=["mtmp", "mshb"], w=["ma32"])
                        P.op("act", lambda h, tg=tg: h.activation(out=f_tok[:, tg, :], in_=a32[:], func=AF.Copy), r=["ma32"], w=[("ftok", tg)])
                        for k4 in range(4):
                            pb = P.bank()
                            for j in range(4):
                                kk = k4 * 4 + j
                                P.op("pe", lambda h, j=j, kk=kk, pb=pb: h.transpose(out=k.ps[pb][:, j * 128:(j + 1) * 128],
                                                                                 in_=a32[:, kk * 128:(kk + 1) * 128], identity=k.ident_f[:]),
                                     r=["ma32", "ident_f"], w=[("ps", pb)], inc=(j == 3))
                            P.op("dve", lambda h, k4=k4, pb=pb: h.tensor_copy(out=fT32[:, k4 * 4:(k4 + 1) * 4, :],
                                                                             in_=k.ps[pb][:].rearrange("p (j t) -> p j t", j=4)),
                                 r=[("ps", pb)], w=["fT32"])
                        pb = P.bank()
                        for kk in range(16):
                            P.op("pe", lambda h, kk=kk, pb=pb: h.matmul(k.ps[pb][:, 0:NE], lhsT=fT32[:, kk, :], rhs=wr[:, kk, :],
                                                                      start=(kk == 0), stop=(kk == 15)), r=["fT32", "wr"], w=[("ps", pb)], inc=(kk == 15))
                        P.op("dve", lambda h, tg=tg, pb=pb: h.tensor_reduce(out=st[:, tg, 4:5], in_=k.ps[pb][:, 0:NE], axis=AX.X, op=ALU.max),
                             r=[("ps", pb)], w=[("mst", tg)])
                        P.op("dve", lambda h, tg=tg: h.tensor_scalar(out=st[:, tg, 5:6], in0=st[:, tg, 4:5], scalar1=-1.0, scalar2=None, op0=ALU.mult),
                             r=[("mst", tg)], w=[("mst", tg)])
                        P.op("act", lambda h, tg=tg, pb=pb: h.activation(out=e16[:], in_=k.ps[pb][:, 0:NE], func=AF.Exp, bias=st[:, tg, 5:6]),
                             r=[("ps", pb), ("mst", tg)], w=["e16"])
                        P.op("dve", lambda h, tg=tg: h.tensor_reduce(out=st[:, tg, 6:7], in_=e16[:], axis=AX.X, op=ALU.add), r=["e16"], w=[("mst", tg)])
                        P.op("dve", lambda h, tg=tg: h.reciprocal(out=st[:, tg, 7:8], in_=st[:, tg, 6:7]), r=[("mst", tg)], w=[("mst", tg)])
                        P.op("dve", lambda h, tg=tg: h.tensor_scalar(out=aff_tok[:, tg, :], in0=e16[:], scalar1=st[:, tg, 7:8], scalar2=None, op0=ALU.mult),
                             r=["e16", ("mst", tg)], w=[("aff", tg)])
                        P.op("pool", lambda h, tg=tg: h.tensor_copy(out=affhl[:, tg, :, 0], in_=aff_tok[:, tg, :]), r=[("aff", tg)], w=[("affhl", tg)])
                        P.op("dve", lambda h, tg=tg: h.tensor_tensor(out=lo32[:], in0=aff_tok[:, tg, :], in1=affhl[:, tg, :, 0], op=ALU.subtract),
                             r=[("aff", tg), ("affhl", tg)], w=["lo32"])
                        P.op("pool", lambda h, tg=tg: h.tensor_copy(out=affhl[:, tg, :, 1], in_=lo32[:]), r=["lo32"], w=[("affhl", tg)])
                        pb = P.bank()
                        P.op("pe", lambda h, tg=tg, pb=pb: h.transpose(out=k.ps[pb][0:16, 0:128], in_=aff_tok[:, tg, :], identity=k.ident_f[:]),
                             r=[("aff", tg), "ident_f"], w=[("ps", pb)])
                        P.op("dve", lambda h, tg=tg, pb=pb: h.tensor_copy(out=affT[:, tg * 128:(tg + 1) * 128], in_=k.ps[pb][0:16, 0:128]),
                             r=[("ps", pb)], w=["affT"])
                    c0, c1 = t0 * 128, (t0 + ntile) * 128
                    P.op("dve", lambda h, c0=c0, c1=c1: h.tensor_copy(out=work[:, c0:c1], in_=affT[:, c0:c1]), r=["affT"], w=["work"])
                    for rnd in range(cap // 8):
                        P.op("dve", lambda h, c0=c0, c1=c1: h.max(out=m8[:], in_=work[:, c0:c1]), r=["work"], w=["m8"])
                        if rnd < cap // 8 - 1:
                            P.op("dve", lambda h, c0=c0, c1=c1: h.match_replace(out=work[:, c0:c1], in_to_replace=m8[:], in_values=work[:, c0:c1],
                                                                              imm_value=-1.0), r=["work", "m8"], w=["work"])
                    P.op("dve", lambda h, c0=c0, c1=c1: h.tensor_scalar(out=mask[:, c0:c1], in0=affT[:, c0:c1], scalar1=m8[:, 7:8], scalar2=None,
                                                                       op0=ALU.is_ge), r=["affT", "m8"], w=["mask"])
                    P.op("dve", lambda h, c0=c0, c1=c1: h.tensor_tensor_scan(out=pos[:, c0:c1], data0=mask[:, c0:c1], data1=mask[:, c0:c1],
                                                                            initial=0.0, op0=ALU.add, op1=ALU.max), r=["mask"], w=["pos"])
                    P.op("dve", lambda h, c0=c0, c1=c1, soff=soff: h.scalar_tensor_tensor(out=slotT[:, c0:c1], in0=pos[:, c0:c1], scalar=float(soff),
                                                                                         in1=mask[:, c0:c1], op0=ALU.add, op1=ALU.mult),
                         r=["pos", "mask"], w=["slotT"])
                    P.op("dve", lambda h, c0=c0, c1=c1: h.tensor_scalar(out=slotT[:, c0:c1], in0=slotT[:, c0:c1], scalar1=-1.0, scalar2=None, op0=ALU.add),
                         r=["slotT"], w=["slotT"])
                pb = P.bank()
                for tg in range(nt):
                    P.op("pe", lambda h, tg=tg, pb=pb: h.transpose(out=k.ps[pb][:, tg * 16:(tg + 1) * 16], in_=slotT[:, tg * 128:(tg + 1) * 128],
                                                                  identity=k.ident_f[0:16, 0:16]), r=["slotT", "ident_f"], w=[("ps", pb)], inc=(tg == nt - 1))
                P.op("dve", lambda h, pb=pb: h.tensor_copy(out=slot_tok[:, 0:nt, :], in_=k.ps[pb][:, 0:nt * 16].rearrange("p (t e) -> p t e", e=16)),
                     r=[("ps", pb)], w=["slot_tok"])
                P.barrier()
            PeL = [sb(nc, esA, f"PeL{i}", [128, ntl, CAPL], BF16) for i in range(2)]
            PeC = [sb(nc, esA, f"PeC{i}", [128, 2, CAPC], BF16) for i in range(2)]
            xe = [sb(nc, esA, f"xeg{i}", [128, 16, ns], BF16) for i in range(2)]
            gtmp = sb(nc, esA, "gtmp", [128, 2], F32)
            for e in range(NE):
                q = e % 2
                for tg in range(nt):
                    if tg < ntl:
                        P.op("dve" if tg % 2 == 0 else "pool", lambda h, q=q, tg=tg, e=e: h.tensor_scalar(
                            out=PeL[q][:, tg, :], in0=iota[:, 0:CAPL], scalar1=slot_tok[:, tg, e:e + 1], scalar2=None, op0=ALU.is_equal),
                            r=["iota", "slot_tok"], w=[("PeL", q)])
                    else:
                        P.op("dve", lambda h, q=q, tg=tg, e=e: h.tensor_scalar(
                            out=PeC[q][:, tg - ntl, :], in0=iota[:, 256:288], scalar1=slot_tok[:, tg, e:e + 1], scalar2=None, op0=ALU.is_equal),
                            r=["iota", "slot_tok"], w=[("PeC", q)])
                for kk in range(16):
                    pb = P.bank()
                    for tg in range(ntl):
                        P.op("pe", lambda h, q=q, tg=tg, kk=kk, pb=pb: h.matmul(k.ps[pb][:, 0:CAPL], lhsT=f_tok[:, tg, kk * 128:(kk + 1) * 128],
                                                                              rhs=PeL[q][:, tg, :], start=(tg == 0), stop=(tg == ntl - 1)),
                             r=[("ftok", tg), ("PeL", q)], w=[("ps", pb)], inc=(tg == ntl - 1))
                    if with_ctx:
                        for tc in range(2):
                            P.op("pe", lambda h, q=q, tc=tc, kk=kk, pb=pb: h.matmul(k.ps[pb][:, 256:288], lhsT=f_tok[:, ntl + tc, kk * 128:(kk + 1) * 128],
                                                                                  rhs=PeC[q][:, tc, :], start=(tc == 0), stop=(tc == 1)),
                                 r=[("ftok", ntl + tc), ("PeC", q)], w=[("ps", pb)], inc=(tc == 1))
                    P.op("act" if kk % 2 == 0 else "dve", (lambda h, q=q, kk=kk, pb=pb: h.activation(out=xe[q][:, kk, :], in_=k.ps[pb][:, 0:ns], func=AF.Copy))
                         if kk % 2 == 0 else (lambda h, q=q, kk=kk, pb=pb: h.tensor_copy(out=xe[q][:, kk, :], in_=k.ps[pb][:, 0:ns])),
                         r=[("ps", pb)], w=[("xeg", q)])
                P.dma("sp", k.xeT_d[e, :, :, 0:ns], xe[q][:], r=[("xeg", q)], w=["xeT_d"])
                for jt, (j0, rows) in enumerate(jts):
                    pb = P.bank()
                    if jt < 2:
                        for tg in range(ntl):
                            P.op("pe", lambda h, q=q, tg=tg, e=e, j0=j0, pb=pb: h.matmul(k.ps[pb][:, 0:2], lhsT=PeL[q][:, tg, j0:j0 + 128],
                                                                                       rhs=affhl[:, tg, e, :], start=(tg == 0), stop=(tg == ntl - 1)),
                                 r=[("PeL", q)] + [("affhl", tg)], w=[("ps", pb)], inc=(tg == ntl - 1))
                    else:
                        for tc in range(2):
                            P.op("pe", lambda h, q=q, tc=tc, e=e, pb=pb: h.matmul(k.ps[pb][0:32, 0:2], lhsT=PeC[q][:, tc, :],
                                                                                rhs=affhl[:, ntl + tc, e, :], start=(tc == 0), stop=(tc == 1)),
                                 r=[("PeC", q), ("affhl", ntl + tc)], w=[("ps", pb)], inc=(tc == 1))
                    P.op("dve", lambda h, rows=rows, pb=pb: h.tensor_copy(out=gtmp[0:rows, :], in_=k.ps[pb][0:rows, 0:2]), r=[("ps", pb)], w=["gtmp"])
                    P.op("dve", lambda h, rows=rows, e=e, jt=jt: h.tensor_tensor(out=gate_all[0:rows, e, jt:jt + 1], in0=gtmp[0:rows, 0:1],
                                                                               in1=gtmp[0:rows, 1:2], op=ALU.add), r=["gtmp"], w=["gate_all"])
            P.barrier()
        with ExitStack() as esB:
            wt = [sb(nc, esB, f"ewt{i}", [128, 16, 512], BF16) for i in range(8)]
            xe2 = [sb(nc, esB, f"xe2{i}", [128, 16, ns], BF16) for i in range(2)]
            hT = sb(nc, esB, "hT", [128, 16, ns], BF16)
            sg = [sb(nc, esB, f"sg{i}", [128, ns], F32) for i in range(2)]
            ye = [sb(nc, esB, f"ye{i}", [128, 3, D], BF16) for i in range(2)]
            nw = [0]

            def load_w(src):
                i = nw[0] % 8
                nw[0] += 1
                P.dma("pool", wt[i][:], src, w=[("ewt", i)])
                return i
            nsg = 0
            for e in range(NE):
                q = e % 2
                P.dma("sp", xe2[q][:], k.xeT_d[e, :, :, 0:ns], r=["xeT_d"], w=[("xe2", q)])
                wg = k.moe_w_gate[layer, e].rearrange("(k p) n -> p k n", p=128)
                wu = k.moe_w_up[layer, e].rearrange("(k p) n -> p k n", p=128)
                wd = k.moe_w_down[layer, e].rearrange("(k p) n -> p k n", p=128)
                for ft in range(4):
                    ig = load_w(wg[:, :, ft * 512:(ft + 1) * 512])
                    iu = load_w(wu[:, :, ft * 512:(ft + 1) * 512])
                    for fc in range(4):
                        fi = ft * 4 + fc
                        pg = P.bank()
                        for kk in range(16):
                            P.op("pe", lambda h, kk=kk, ig=ig, fc=fc, q=q, pg=pg: h.matmul(k.ps[pg][:, 0:ns], lhsT=wt[ig][:, kk, fc * 128:(fc + 1) * 128],
                                                                                         rhs=xe2[q][:, kk, :], start=(kk == 0), stop=(kk == 15)),
                                 r=[("ewt", ig), ("xe2", q)], w=[("ps", pg)], inc=(kk == 15))
                        pu = P.bank()
                        for kk in range(16):
                            P.op("pe", lambda h, kk=kk, iu=iu, fc=fc, q=q, pu=pu: h.matmul(k.ps[pu][:, 0:ns], lhsT=wt[iu][:, kk, fc * 128:(fc + 1) * 128],
                                                                                         rhs=xe2[q][:, kk, :], start=(kk == 0), stop=(kk == 15)),
                                 r=[("ewt", iu), ("xe2", q)], w=[("ps", pu)], inc=(kk == 15))
                        s_ = nsg % 2
                        nsg += 1
                        P.op("act", lambda h, s_=s_, pg=pg: h.activation(out=sg[s_][:], in_=k.ps[pg][:, 0:ns], func=AF.Silu), r=[("ps", pg)], w=[("sg", s_)])
                        P.op("dve", lambda h, s_=s_, pu=pu, fi=fi: h.tensor_tensor(out=hT[:, fi, :], in0=k.ps[pu][:, 0:ns], in1=sg[s_][:], op=ALU.mult),
                             r=[("ps", pu), ("sg", s_)], w=["hT"])
                for db in range(4):
                    iw = load_w(wd[:, :, db * 512:(db + 1) * 512])
                    for jt, (j0, rows) in enumerate(jts):
                        pb = P.bank()
                        for fi in range(16):
                            P.op("pe", lambda h, fi=fi, iw=iw, j0=j0, rows=rows, pb=pb: h.matmul(k.ps[pb][0:rows, :], lhsT=hT[:, fi, j0:j0 + rows],
                                                                                                rhs=wt[iw][:, fi, :], start=(fi == 0), stop=(fi == 15)),
                                 r=["hT", ("ewt", iw)], w=[("ps", pb)], inc=(fi == 15))
                        P.op("dve", lambda h, q=q, jt=jt, db=db, rows=rows, e=e, pb=pb: h.tensor_scalar(
                            out=ye[q][0:rows, jt, db * 512:(db + 1) * 512], in0=k.ps[pb][0:rows, :], scalar1=gate_all[0:rows, e, jt:jt + 1],
                            scalar2=None, op0=ALU.mult), r=[("ps", pb), "gate_all"], w=[("ye", q)])
                for jt, (j0, rows) in enumerate(jts):
                    P.dma("sp", k.ye_d[e, 0:rows, jt, :], ye[q][0:rows, jt, :], r=[("ye", q)], w=["ye_d"])
            P.barrier()
        with ExitStack() as esC:
            sel = sb(nc, esC, "sel", [16, NE, 128], F32)
            jidx = sb(nc, esC, "jidx", [128, 3], F32)
            P.dma("sp", sel[:], k.c_sel, w=["sel"])
            P.dma("sp", jidx[:], k.c_jidx, w=["jidx"])
            PT = sb(nc, esC, "PT", [128, NE, 2, 512], BF16)
            yeb = sb(nc, esC, "yeb", [128, NE, 3, 512], BF16)
            hb3 = sb(nc, esC, "chb", [128, 4, D], F32)
            t5 = [sb(nc, esC, f"ct5{i}", [128, 512], F32) for i in range(2)]
            g2b = sb(nc, esC, "g2b", [128, D], F32)
            blocks = [(tb * 512, 512, False) for tb in range(4)] + ([(L, M, True)] if with_ctx else [])
            yev = k.ye_d.rearrange("e p j d -> p e j d")
            nt5 = 0
            for (b0, bn, isctx) in blocks:
                ntb = bn // 128
                if b0 == 0 or isctx:
                    P.dma("sp", g2b[:], k.mod[layer, 1 if isctx else 0, 5 * D:6 * D].partition_broadcast(128), r=[("mod", layer)], w=["g2b"])
                P.dma("sp", hb3[:, 0:ntb, :], k.hres[b0:b0 + bn, :].rearrange("(t p) d -> p t d", p=128), r=["hres"], w=["chb"])
                for e in range(NE):
                    pb = P.bank()
                    P.op("pe", lambda h, e=e, b0=b0, bn=bn, pb=pb: h.matmul(k.ps[pb][:, 0:bn], lhsT=sel[:, e, :], rhs=slotT[:, b0:b0 + bn],
                                                                          start=True, stop=True), r=["sel", "slotT"], w=[("ps", pb)])
                    if not isctx:
                        for jt in range(2):
                            P.op("dve", lambda h, e=e, jt=jt, bn=bn, pb=pb: h.tensor_scalar(
                                out=PT[:, e, jt, 0:bn], in0=k.ps[pb][:, 0:bn], scalar1=jidx[:, jt:jt + 1], scalar2=None, op0=ALU.is_equal),
                                r=[("ps", pb), "jidx"], w=["PT"])
                    else:
                        P.op("dve", lambda h, e=e, bn=bn, pb=pb: h.tensor_scalar(
                            out=PT[0:32, e, 0, 0:bn], in0=k.ps[pb][0:32, 0:bn], scalar1=jidx[0:32, 2:3], scalar2=None, op0=ALU.is_equal),
                            r=[("ps", pb), "jidx"], w=["PT"])
                for db in range(4):
                    dsl = slice(db * 512, (db + 1) * 512)
                    if not isctx:
                        for jt in range(2):
                            P.dma("act" if jt == 0 else "sp", yeb[:, :, jt, :], yev[:, :, jt, dsl], r=["ye_d"], w=["yeb"])
                    else:
                        P.dma("act", yeb[0:32, :, 2, :], yev[0:32, :, 2, dsl], r=["ye_d"], w=["yeb"])
                    for ti in range(ntb):
                        pb = P.bank()
                        if not isctx:
                            i_mm = 0
                            for e in range(NE):
                                for jt in range(2):
                                    P.op("pe", lambda h, e=e, jt=jt, ti=ti, pb=pb, i_mm=i_mm: h.matmul(
                                        k.ps[pb][:], lhsT=PT[:, e, jt, ti * 128:(ti + 1) * 128], rhs=yeb[:, e, jt, :],
                                        start=(i_mm == 0), stop=(i_mm == 2 * NE - 1)), r=["PT", "yeb"], w=[("ps", pb)], inc=(i_mm == 2 * NE - 1))
                                    i_mm += 1
                        else:
                            for e in range(NE):
                                P.op("pe", lambda h, e=e, ti=ti, pb=pb: h.matmul(
                                    k.ps[pb][:], lhsT=PT[0:32, e, 0, ti * 128:(ti + 1) * 128], rhs=yeb[0:32, e, 2, :],
                                    start=(e == 0), stop=(e == NE - 1)), r=["PT", "yeb"], w=[("ps", pb)], inc=(e == NE - 1))
                        q5 = nt5 % 2
                        nt5 += 1
                        P.op("dve", lambda h, q5=q5, dsl=dsl, pb=pb: h.tensor_tensor(out=t5[q5][:], in0=k.ps[pb][:], in1=g2b[:, dsl], op=ALU.mult),
                             r=[("ps", pb), "g2b"], w=[("ct5", q5)])
                        P.op("pool", lambda h, q5=q5, ti=ti, dsl=dsl: h.tensor_tensor(out=hb3[:, ti, dsl], in0=hb3[:, ti, dsl], in1=t5[q5][:], op=ALU.add),
                             r=[("ct5", q5), "chb"], w=["chb"])
                P.dma("sp", k.hres[b0:b0 + bn, :].rearrange("(t p) d -> p t d", p=128), hb3[:, 0:ntb, :], r=["chb"], w=["hres"])
            P.barrier()


RH = 8
NTL = L // 128


def host_ret_consts():
    half = 64
    freqs = 10000.0 ** (-np.arange(half, dtype=np.float64) / half)
    t = np.arange(L)
    ang = np.concatenate([(t // 64)[:, None] * freqs[None, :], (t % 64)[:, None] * freqs[None, :]], axis=1)
    rope = np.stack([np.cos(ang), np.sin(ang)]).astype(np.float32)
    i = np.arange(128)
    rel = np.stack([i[None, :] - i[:, None], i[:, None] - i[None, :]]).astype(np.float32)
    cnt = np.stack([i + 1.0, 128.0 - i], axis=1).astype(np.float32)
    return rope, rel, cnt


def phase_ret_inproj(k, aT):
    nc, P = k.nc, k.P
    wv = k.ret_w_in.rearrange("(k p) n -> p k n", p=128)
    with ExitStack() as es:
        wt = [sb(nc, es, f"rwt{i}", [128, 16, 512], BF16) for i in range(2)]
        cosT = sb(nc, es, "cosT", [128, NTL, 128], F32)
        sinT = sb(nc, es, "sinT", [128, NTL, 128], F32)
        P.dma("sp", cosT[:], k.c_rope[0].rearrange("(t p) c -> p t c", p=128), w=["cosT"])
        P.dma("sp", sinT[:], k.c_rope[1].rearrange("(t p) c -> p t c", p=128), w=["sinT"])
        stage = [sb(nc, es, f"rstg{i}", [128, NT, 512], BF16) for i in range(2)]
        stT = [sb(nc, es, f"rstT{i}", [128, 4, L], BF16) for i in range(2)]
        t1 = [sb(nc, es, f"rt1{i}", [128, 512], F32) for i in range(2)]
        t2 = [sb(nc, es, f"rt2{i}", [128, 512], F32) for i in range(2)]
        rtok = [sb(nc, es, f"rtok{i}", [128, 512], BF16) for i in range(2)]
        nw = [0]

        def load_w(c0):
            i = nw[0] % 2
            nw[0] += 1
            P.dma("pool", wt[i][:], wv[:, :, c0:c0 + 512], w=[("rwt", i)])
            return i

        def v5(ap):
            return ap.rearrange("p (h a f c) -> p h a f c", h=2, a=2, f=2)
        nq = 0
        for which in range(2):
            for ti in range(4):
                wi = load_w(which * 2048 + ti * 512)
                si = ti % 2
                tiles = range(NTL) if which == 0 else range(NT)
                def g_tile(tg, q, which=which, wi=wi, si=si):
                    pb = P.bank()
                    for kk in range(16):
                        P.op("pe", lambda h, kk=kk, wi=wi, tg=tg, pb=pb: h.matmul(k.ps[pb][:], lhsT=aT[:, kk, tg * 128:(tg + 1) * 128],
                                                                                rhs=wt[wi][:, kk, :], start=(kk == 0), stop=(kk == 15)),
                             r=[("rwt", wi), ("aT", tg)], w=[("ps", pb)], inc=(kk == 15))
                    yield
                    if tg < NTL:
                        u = v5(k.ps[pb][:])
                        cb = cosT[:, tg, :].rearrange("p (a c) -> p a c", a=2).unsqueeze(1).to_broadcast([128, 2, 2, 64])
                        sbb = sinT[:, tg, :].rearrange("p (a c) -> p a c", a=2).unsqueeze(1).to_broadcast([128, 2, 2, 64])
                        a1, a2, ro = v5(t1[q][:]), v5(t2[q][:]), v5(rtok[q][:])
                        for f in range(2):
                            yield
                            P.op("dve", lambda h, u=u, a1=a1, cb=cb, f=f: h.tensor_tensor(out=a1[:, :, :, f, :], in0=u[:, :, :, f, :], in1=cb, op=ALU.mult),
                                 r=[("ps", pb), "cosT"], w=[("rt1", q)])
                            P.op("dve", lambda h, u=u, a2=a2, sbb=sbb, f=f: h.tensor_tensor(out=a2[:, :, :, f, :], in0=u[:, :, :, 1 - f, :], in1=sbb, op=ALU.mult),
                                 r=[("ps", pb), "sinT"], w=[("rt2", q)])
                        yield
                        P.op("pool", lambda h, a1=a1, a2=a2, ro=ro: h.tensor_tensor(out=ro[:, :, :, 0, :], in0=a1[:, :, :, 0, :], in1=a2[:, :, :, 0, :], op=ALU.subtract),
                             r=[("rt1", q), ("rt2", q)], w=[("rtok", q)])
                        P.op("pool", lambda h, a1=a1, a2=a2, ro=ro: h.tensor_tensor(out=ro[:, :, :, 1, :], in0=a1[:, :, :, 1, :], in1=a2[:, :, :, 1, :], op=ALU.add),
                             r=[("rt1", q), ("rt2", q)], w=[("rtok", q)])
                        yield
                        if which == 1:
                            P.op("act", lambda h, q=q, si=si, tg=tg: h.activation(out=stage[si][:, tg, :], in_=rtok[q][:], func=AF.Copy, scale=1.0 / 16.0),
                                 r=[("rtok", q)], w=[("rstg", si)])
                        yield
                        pt = P.bank()
                        psb = k.ps[pt][:].bitcast(BF16)
                        for j in range(4):
                            P.op("pe", lambda h, q=q, j=j, psb=psb: h.transpose(out=psb[:, j * 128:(j + 1) * 128], in_=rtok[q][:, j * 128:(j + 1) * 128],
                                                                               identity=k.ident_bf[:]), r=[("rtok", q), "ident"], w=[("ps", pt)], inc=(j == 3))
                        yield
                        P.op("act", lambda h, si=si, tg=tg, psb=psb, which=which: h.activation(
                            out=stT[si][:, :, tg * 128:(tg + 1) * 128], in_=psb[:, 0:512].rearrange("p (j t) -> p j t", j=4), func=AF.Copy,
                            scale=(1.0 if which == 0 else 1.0 / 16.0)), r=[("ps", pt)], w=[("rstT", si)])
                    else:
                        P.op("act", lambda h, si=si, tg=tg, pb=pb: h.activation(out=stage[si][:, tg, :], in_=k.ps[pb][:], func=AF.Copy, scale=1.0 / 16.0),
                             r=[("ps", pb)], w=[("rstg", si)])

                tl = list(tiles)
                for i0 in range(0, len(tl), 2):
                    gens = [g_tile(tg, tg % 2) for tg in tl[i0:i0 + 2]]
                    while gens:
                        for gnr in list(gens):
                            try:
                                next(gnr)
                            except StopIteration:
                                gens.remove(gnr)
                dstT = k.qT_s if which == 0 else k.kT_s
                P.dma("sp", dstT[:, 2 * ti:2 * ti + 2].rearrange("p h c t -> p (h c) t"), stT[si][:], r=[("rstT", si)], w=["qkT_s"])
                if which == 1:
                    P.dma("sp", k.k_tok.rearrange("(g p) c -> p g c", p=128)[:, :, ti * 512:(ti + 1) * 512], stage[si][:], r=[("rstg", si)], w=["k_tok"])
        for which in range(2):
            for ti in range(8):
                wi = load_w((4096 if which == 0 else 8192) + ti * 512)
                si = ti % 2
                tiles = range(NT) if which == 0 else range(NTL)
                for tg in tiles:
                    pb = P.bank()
                    for kk in range(16):
                        P.op("pe", lambda h, kk=kk, wi=wi, tg=tg, pb=pb: h.matmul(k.ps[pb][:], lhsT=aT[:, kk, tg * 128:(tg + 1) * 128],
                                                                                rhs=wt[wi][:, kk, :], start=(kk == 0), stop=(kk == 15)),
                             r=[("rwt", wi), ("aT", tg)], w=[("ps", pb)], inc=(kk == 15))
                    P.op("act", lambda h, si=si, tg=tg, pb=pb, which=which: h.activation(out=stage[si][:, tg, :], in_=k.ps[pb][:],
                                                                                       func=(AF.Copy if which == 0 else AF.Silu)),
                         r=[("ps", pb)], w=[("rstg", si)])
                if which == 0:
                    P.dma("sp", k.v_tok.rearrange("(g p) c -> p g c", p=128)[:, :, ti * 512:(ti + 1) * 512], stage[si][:], r=[("rstg", si)], w=["v_tok"])
                else:
                    P.dma("sp", k.sg_tok.rearrange("(g p) c -> p g c", p=128)[:, :, ti * 512:(ti + 1) * 512], stage[si][:, 0:NTL, :],
                          r=[("rstg", si)], w=["sg_tok"])
        P.barrier()


def phase_ret_scan(k, heads=range(RH)):
    nc, P = k.nc, k.P
    with ExitStack() as es:
        ldb = load_bc(k, es, "ldb", k.ret_decay.rearrange("a b -> (a b)"), 16)
        rel = sb(nc, es, "rel", [128, 2, 128], F32)
        cnt = sb(nc, es, "cnt", [128, 2], F32)
        P.dma("sp", rel[:], k.c_rel.rearrange("d s l -> s d l"), w=["rel"])
        P.dma("sp", cnt[:], k.c_cnt, w=["cnt"])
        P.op("act", lambda h: h.activation(out=ldb[:], in_=ldb[:], func=AF.Exp), r=["ldb"], w=["ldb"])
        P.op("dve", lambda h: h.tensor_scalar(out=ldb[:], in0=ldb[:], scalar1=-1.0, scalar2=None, op0=ALU.mult), r=["ldb"], w=["ldb"])
        decT = sb(nc, es, "decT", [128, 16, 128], F32)
        dfs = sb(nc, es, "dfs", [128, 16], F32)
        dte = sb(nc, es, "dte", [128, 16], F32)
        dch = sb(nc, es, "dch", [128, 16], F32)
        c2 = sb(nc, es, "c2", [128, 2], F32)
        P.op("dve", lambda h: h.tensor_scalar(out=c2[:], in0=cnt[:], scalar1=-1.0, scalar2=128.0, op0=ALU.mult, op1=ALU.add), r=["cnt"], w=["c2"])
        for d in range(2):
            for hh in range(RH):
                dh = d * RH + hh
                P.op("dve", lambda h, d=d, dh=dh: h.tensor_scalar(out=decT[:, dh, :], in0=rel[:, d, :], scalar1=ldb[:, dh:dh + 1], scalar2=None, op0=ALU.mult),
                     r=["rel", "ldb"], w=["decT"])
            P.op("pool", lambda h, d=d: h.affine_select(out=decT[:, d * RH:(d + 1) * RH, :], in_=decT[:, d * RH:(d + 1) * RH, :],
                                                        pattern=[[0, RH], [1 if d == 0 else -1, 128]], compare_op=ALU.is_ge, fill=fillreg(k, h),
                                                        base=0, channel_multiplier=(-1 if d == 0 else 1)), r=["decT"], w=["decT"])
            P.op("dve", lambda h, d=d: h.tensor_scalar(out=dfs[:, d * RH:(d + 1) * RH], in0=ldb[:, d * RH:(d + 1) * RH], scalar1=cnt[:, d:d + 1], scalar2=None,
                                                       op0=ALU.mult), r=["ldb", "cnt"], w=["dfs"])
            P.op("dve", lambda h, d=d: h.tensor_scalar(out=dte[:, d * RH:(d + 1) * RH], in0=ldb[:, d * RH:(d + 1) * RH], scalar1=c2[:, d:d + 1], scalar2=None,
                                                       op0=ALU.mult), r=["ldb", "c2"], w=["dte"])
        P.op("dve", lambda h: h.tensor_scalar(out=dch[:], in0=ldb[:], scalar1=128.0, scalar2=None, op0=ALU.mult), r=["ldb"], w=["dch"])
        P.op("act", lambda h: h.activation(out=decT[:], in_=decT[:], func=AF.Exp), r=["decT"], w=["decT"])
        P.op("act", lambda h: h.activation(out=dfs[:], in_=dfs[:], func=AF.Exp), r=["dfs"], w=["dfs"])
        P.op("act", lambda h: h.activation(out=dte[:], in_=dte[:], func=AF.Exp), r=["dte"], w=["dte"])
        P.op("act", lambda h: h.activation(out=dch[:], in_=dch[:], func=AF.Exp), r=["dch"], w=["dch"])
        qT = [sb(nc, es, f"qTh{i}", [128, 2, L], BF16) for i in range(2)]
        kT = [sb(nc, es, f"kTh{i}", [128, 2, L], BF16) for i in range(2)]
        kt = [sb(nc, es, f"kth{i}", [128, NT, 256], BF16) for i in range(2)]
        vt = [sb(nc, es, f"vth{i}", [128, NT, 512], BF16) for i in range(2)]
        scD = [sb(nc, es, f"scD{i}", [128, 128], BF16) for i in range(2)]
        kd = [sb(nc, es, f"kd{i}", [128, 256], BF16) for i in range(2)]
        oo = [sb(nc, es, f"oo{i}", [128, 512], F32) for i in range(2)]
        Sp = [sb(nc, es, f"S{i}", [128, 2, 512], F32) for i in range(2)]
        Sbf = sb(nc, es, "Sbf", [128, 2, 512], BF16)
        n_it = 0
        n_s = [0]
        P.nrot = 4
        for hi, hh in enumerate(heads):
            r = hi % 2
            P.dma("sp", qT[r][:], k.qT_s[:, hh], r=["qkT_s"], w=[("qTh", r)])
            P.dma("act", kT[r][:], k.kT_s[:, hh], r=["qkT_s"], w=[("kTh", r)])
            P.dma("sp", kt[r][:], k.k_tok.rearrange("(t p) c -> p t c", p=128)[:, :, hh * 256:(hh + 1) * 256], r=["k_tok"], w=[("kth", r)])
            P.dma("act", vt[r][:], k.v_tok.rearrange("(t p) c -> p t c", p=128)[:, :, hh * 512:(hh + 1) * 512], r=["v_tok"], w=[("vth", r)])
            for d in range(2):
                dh = d * RH + hh
                s0 = Sp[n_s[0] % 2]
                P.op("pool", lambda h, s0=s0: h.memset(s0[:], 0.0), w=[("S", n_s[0] % 2)])
                P.op("pool", lambda h: h.memset(Sbf[:], 0.0), w=["Sbf"])
                order = [16, 17] + list(range(16)) if d == 0 else [17, 16] + list(range(15, -1, -1))
                def merge(gens):
                    gens = list(gens)
                    while gens:
                        for gnr in list(gens):
                            try:
                                next(gnr)
                            except StopIteration:
                                gens.remove(gnr)

                def g_state(tg, q):
                    P.op("pool", lambda h, q=q, r=r, tg=tg, dh=dh: h.tensor_scalar(out=kd[q][:], in0=kt[r][:, tg, :], scalar1=dte[:, dh:dh + 1], scalar2=None,
                                                                                 op0=ALU.mult), r=[("kth", r), "dte"], w=[("kd", q)])
                    yield
                    for c in range(2):
                        pd = 6 + c
                        P.op("pe", lambda h, q=q, r=r, c=c, tg=tg, pd=pd: h.matmul(k.ps[pd][:], lhsT=kd[q][:, c * 128:(c + 1) * 128], rhs=vt[r][:, tg, :],
                                                                                 start=True, stop=True), r=[("kd", q), ("vth", r)], w=[("ps", pd)])
                        yield
                    so, sn = Sp[n_s[0] % 2], Sp[(n_s[0] + 1) % 2]
                    ko, kn = ("S", n_s[0] % 2), ("S", (n_s[0] + 1) % 2)
                    n_s[0] += 1
                    for c in range(2):
                        pd = 6 + c
                        P.op("dve", lambda h, c=c, dh=dh, pd=pd, so=so, sn=sn: h.scalar_tensor_tensor(out=sn[:, c, :], in0=so[:, c, :], scalar=dch[:, dh:dh + 1],
                                                                                                    in1=k.ps[pd][:], op0=ALU.mult, op1=ALU.add),
                             r=[ko, "dch", ("ps", pd)], w=[kn])
                        yield
                    P.op("act", lambda h, sn=sn: h.activation(out=Sbf[:], in_=sn[:], func=AF.Copy), r=[kn], w=["Sbf"])
                    yield

                def g_intra(tg, q, p1):
                    tk = slice(tg * 128, (tg + 1) * 128)
                    ps_ = P.bank()
                    for c in range(2):
                        P.op("pe", lambda h, r=r, c=c, tk=tk, ps_=ps_: h.matmul(k.ps[ps_][:, 0:128], lhsT=kT[r][:, c, tk], rhs=qT[r][:, c, tk],
                                                                              start=(c == 0), stop=(c == 1)), r=[("kTh", r), ("qTh", r)], w=[("ps", ps_)], inc=(c == 1))
                    yield
                    P.op("dve", lambda h, q=q, dh=dh, ps_=ps_: h.tensor_tensor(out=scD[q][:], in0=k.ps[ps_][:, 0:128], in1=decT[:, dh, :], op=ALU.mult),
                         r=[("ps", ps_), "decT"], w=[("scD", q)])
                    yield
                    P.op("pe", lambda h, q=q, r=r, tg=tg, p1=p1: h.matmul(k.ps[p1][:], lhsT=scD[q][:], rhs=vt[r][:, tg, :], start=True, stop=True),
                         r=[("scD", q), ("vth", r)], w=[("ps", p1)])
                    yield

                def g_cross(tg, q, p1):
                    tk = slice(tg * 128, (tg + 1) * 128)
                    p2 = P.bank()
                    for c in range(2):
                        P.op("pe", lambda h, r=r, c=c, tk=tk, p2=p2: h.matmul(k.ps[p2][:], lhsT=qT[r][:, c, tk], rhs=Sbf[:, c, :],
                                                                            start=(c == 0), stop=(c == 1)), r=[("qTh", r), "Sbf"], w=[("ps", p2)], inc=(c == 1))
                    yield
                    P.op("dve", lambda h, q=q, dh=dh, p2=p2: h.tensor_scalar(out=oo[q][:], in0=k.ps[p2][:], scalar1=dfs[:, dh:dh + 1], scalar2=None, op0=ALU.mult),
                         r=[("ps", p2), "dfs"], w=[("oo", q)])
                    yield
                    P.op("dve", lambda h, q=q, p1=p1: h.tensor_tensor(out=oo[q][:], in0=k.ps[p1][:], in1=oo[q][:], op=ALU.add),
                         r=[("ps", p1), ("oo", q)], w=[("oo", q)])
                    yield
                    P.dma("sp", k.o_s[d, tk, hh * 512:(hh + 1) * 512], oo[q][:], r=[("oo", q)], w=["o_s"])
                    yield

                for tg in order:
                    q = n_it % 2
                    n_it += 1
                    if tg < NTL:
                        p1 = 4 + q
                        merge([g_intra(tg, q, p1), g_cross(tg, q, p1), g_state(tg, q)])
                    else:
                        merge([g_state(tg, q)])
        P.barrier()
        P.nrot = 8


def phase_ret_out(k, layer):
    nc, P = k.nc, k.P
    with ExitStack() as es:
        W = sb(nc, es, "rWout", [128, 32, D], BF16)
        ngT = sb(nc, es, "rngT", [128, 32], F32)
        P.dma("sp", ngT[:], k.ret_gn_g.rearrange("(k p) -> p k", p=128), w=["rngT"], allow_slow_non_contiguous=True)
        wv = k.ret_w_out.rearrange("(k p) n -> p k n", p=128)
        yf = sb(nc, es, "ryf", [128, 2048], F32)
        yb = sb(nc, es, "ryb", [128, 2048], F32)
        for kk in range(32):
            stg = yf if kk % 2 == 0 else yb
            key = "ryf" if kk % 2 == 0 else "ryb"
            P.dma("sp" if kk % 2 == 0 else "act", stg[:], wv[:, kk, :], w=[key])
            P.op("dve" if kk % 2 == 0 else "pool", lambda h, kk=kk, stg=stg: h.tensor_scalar(
                out=W[:, kk, :], in0=stg[:], scalar1=ngT[:, kk:kk + 1], scalar2=None, op0=ALU.mult), r=[key, "rngT"], w=["rWout"])
        sg = sb(nc, es, "rsg", [128, 4096], BF16)
        yzb = sb(nc, es, "ryzb", [128, 4096], BF16)
        ynT = sb(nc, es, "rynT", [128, 32, 128], BF16)
        hb = sb(nc, es, "rhb", [128, D], F32)
        ho = sb(nc, es, "rho", [128, D], F32)
        st = sb(nc, es, "rst", [128, 4, 4], F32)
        g1b = load_bc(k, es, "rg1b", k.mod[layer, 0, 2 * D:3 * D], D, key=("mod", layer))
        for tg in range(NTL):
            tk = slice(tg * 128, (tg + 1) * 128)
            P.dma("sp", hb[:], k.hres[tk, :], r=["hres"], w=["rhb"])
            P.dma("act", sg[:], k.sg_tok[tk, :], r=["sg_tok"], w=["rsg"])
            for half in range(2):
                cs_ = slice(half * 2048, (half + 1) * 2048)
                P.dma("sp", yf[:], k.o_s[0, tk, cs_], r=["o_s"], w=["ryf"])
                P.dma("sp", yb[:], k.o_s[1, tk, cs_], r=["o_s"], w=["ryb"])
                P.op("dve", lambda h: h.tensor_tensor(out=yf[:], in0=yf[:], in1=yb[:], op=ALU.add), r=["ryf", "ryb"], w=["ryf"])
                y3 = yf[:].rearrange("p (e c) -> p e c", c=512)
                b3 = yb[:].rearrange("p (e c) -> p e c", c=512)
                P.op("dve", lambda h, y3=y3: h.tensor_reduce(out=st[:, :, 0], in_=y3, axis=AX.X, op=ALU.add), r=["ryf"], w=["rst"])
                P.op("dve", lambda h: h.tensor_scalar(out=st[:, :, 0], in0=st[:, :, 0], scalar1=1.0 / 512, scalar2=None, op0=ALU.mult), r=["rst"], w=["rst"])
                P.op("dve", lambda h, y3=y3: h.tensor_tensor(out=y3, in0=y3, in1=bc3(st[:, :, 0], 512), op=ALU.subtract), r=["ryf", "rst"], w=["ryf"])
                P.op("act", lambda h: h.activation(out=yb[:], in_=yf[:], func=AF.Square), r=["ryf"], w=["ryb"])
                P.op("dve", lambda h, b3=b3: h.tensor_reduce(out=st[:, :, 1], in_=b3, axis=AX.X, op=ALU.add), r=["ryb"], w=["rst"])
                P.op("dve", lambda h: h.tensor_scalar(out=st[:, :, 1], in0=st[:, :, 1], scalar1=1.0 / 512, scalar2=EPS, op0=ALU.mult, op1=ALU.add), r=["rst"], w=["rst"])
                P.op("act", lambda h: h.activation(out=st[:, :, 2], in_=st[:, :, 1], func=AF.Sqrt), r=["rst"], w=["rst"])
                P.op("dve", lambda h: h.reciprocal(out=st[:, :, 3], in_=st[:, :, 2]), r=["rst"], w=["rst"])
                P.op("dve", lambda h, y3=y3: h.tensor_tensor(out=y3, in0=y3, in1=bc3(st[:, :, 3], 512), op=ALU.mult), r=["ryf", "rst"], w=["ryf"])
                P.op("pool", lambda h, cs_=cs_: h.tensor_tensor(out=yzb[:, cs_], in0=yf[:], in1=sg[:, cs_], op=ALU.mult), r=["ryf", "rsg"], w=["ryzb"])
            for k8 in range(4):
                pb = P.bank()
                psb = k.ps[pb][:].bitcast(BF16)
                for j in range(8):
                    kk = k8 * 8 + j
                    P.op("pe", lambda h, j=j, kk=kk, psb=psb: h.transpose(out=psb[:, j * 128:(j + 1) * 128],
                                                                       in_=yzb[:, kk * 128:(kk + 1) * 128], identity=k.ident_bf[:]),
                         r=["ryzb", "ident"], w=[("ps", pb)], inc=(j == 7))
                P.op("act", lambda h, k8=k8, psb=psb: h.activation(out=ynT[:, k8 * 8:(k8 + 1) * 8, :], in_=psb.rearrange("p (j t) -> p j t", j=8), func=AF.Copy),
                     r=[("ps", pb)], w=["rynT"])
            for db in range(4):
                pb = P.bank()
                dsl = slice(db * 512, (db + 1) * 512)
                for kk in range(32):
                    P.op("pe", lambda h, kk=kk, dsl=dsl, pb=pb: h.matmul(k.ps[pb][:], lhsT=ynT[:, kk, :], rhs=W[:, kk, dsl],
                                                                        start=(kk == 0), stop=(kk == 31)),
                         r=["rynT", "rWout"], w=[("ps", pb)], inc=(kk == 31))
                P.op("dve", lambda h, dsl=dsl, pb=pb: h.tensor_tensor(out=ho[:, dsl], in0=k.ps[pb][:], in1=g1b[:, dsl], op=ALU.mult),
                     r=[("ps", pb), "rg1b"], w=["rho"])
                P.op("pool", lambda h, dsl=dsl: h.tensor_tensor(out=ho[:, dsl], in0=ho[:, dsl], in1=hb[:, dsl], op=ALU.add),
                     r=["rho", "rhb"], w=["rho"])
            P.dma("sp", k.hres[tk, :], ho[:], r=["rho"], w=["hres"])
        P.barrier()


def phase_final_norm(k, src, dst):
    nc, P = k.nc, k.P
    with ExitStack() as es:
        hb = [sb(nc, es, f"fhb{i}", [128, D], F32) for i in range(2)]
        tmp = [sb(nc, es, f"ftmp{i}", [128, D], F32) for i in range(2)]
        st = sb(nc, es, "fst", [128, L // 128, 4], F32)
        gb = load_bc(k, es, "fgb", k.final_norm_g, D)
        for tg in range(L // 128):
            i = tg % 2
            P.dma("sp", hb[i][:], src[tg * 128:(tg + 1) * 128, :], r=["hres"], w=[("fhb", i)])
            P.op("act", lambda h, i=i: h.activation(out=tmp[i][:], in_=hb[i][:], func=AF.Square), r=[("fhb", i)], w=[("ftmp", i)])
            P.op("dve", lambda h, i=i, tg=tg: h.tensor_reduce(out=st[:, tg, 0:1], in_=tmp[i][:], axis=AX.X, op=ALU.add),
                 r=[("ftmp", i)], w=[("fst", tg)])
            P.op("dve", lambda h, tg=tg: h.tensor_scalar(out=st[:, tg, 1:2], in0=st[:, tg, 0:1], scalar1=1.0 / D, scalar2=EPS,
                                                         op0=ALU.mult, op1=ALU.add), r=[("fst", tg)], w=[("fst", tg)])
            P.op("act", lambda h, tg=tg: h.activation(out=st[:, tg, 2:3], in_=st[:, tg, 1:2], func=AF.Sqrt), r=[("fst", tg)], w=[("fst", tg)])
            P.op("dve", lambda h, tg=tg: h.reciprocal(out=st[:, tg, 3:4], in_=st[:, tg, 2:3]), r=[("fst", tg)], w=[("fst", tg)])
            P.op("dve", lambda h, i=i, tg=tg: h.scalar_tensor_tensor(out=tmp[i][:], in0=hb[i][:], scalar=st[:, tg, 3:4], in1=gb[:],
                                                                   op0=ALU.mult, op1=ALU.mult),
                 r=[("fhb", i), ("fst", tg), "fgb"], w=[("ftmp", i)])
            P.dma("sp", dst[tg * 128:(tg + 1) * 128, :], tmp[i][:], r=[("ftmp", i)], w=["out"])
        P.barrier()


def build_program():
    nc = bass.Bass("TRN2", target_bir_lowering=False)
    k = K()
    k.nc = nc
    I = lambda name, shape, dt=F32: nc.dram_tensor(name, shape, dt, kind="ExternalInput").ap()
    S = lambda name, shape, dt=F32: nc.dram_tensor(name, shape, dt, kind="Internal").ap()
    k.x = I("x", [L, D])
    k.ctx = I("ctx", [M, D])
    k.c = I("c", [D])
    k.c_ctx = I("c_ctx", [D])
    k.ada_w = I("ada_w", [2, D, NM])
    k.ada_b = I("ada_b", [2, NM])
    k.norm_mix_g = I("norm_mix_g", [2, D])
    k.norm_ffn_g = I("norm_ffn_g", [2, D])
    k.ssd_w_in = I("ssd_w_in", [D, 10368])
    k.ssd_conv_w = I("ssd_conv_w", [5, 6144])
    k.ssd_conv_b = I("ssd_conv_b", [6144])
    k.ssd_dt_bias = I("ssd_dt_bias", [2, 64])
    k.ssd_a_log = I("ssd_a_log", [2, 64])
    k.ssd_d = I("ssd_d", [64])
    k.ssd_norm_g = I("ssd_norm_g", [4096])
    k.ssd_w_out = I("ssd_w_out", [4096, D])
    k.ret_w_in = I("ret_w_in", [D, 12288])
    k.ret_decay = I("ret_decay", [2, 8])
    k.ret_gn_g = I("ret_gn_g", [4096])
    k.ret_w_out = I("ret_w_out", [4096, D])
    k.moe_w_router = I("moe_w_router", [2, D, NE])
    k.moe_w_gate = I("moe_w_gate", [2, NE, D, D])
    k.moe_w_up = I("moe_w_up", [2, NE, D, D])
    k.moe_w_down = I("moe_w_down", [2, NE, D, D])
    k.final_norm_g = I("final_norm_g", [D])
    k.c_ident = I("c_ident", [128, 128])
    k.c_tri = I("c_tri", [5, 128, 128])
    k.c_iota = I("c_iota", [128, 288])
    k.c_jidx = I("c_jidx", [128, 3])
    k.c_sel = I("c_sel", [16, 16, 128])
    k.c_rope = I("c_rope", [2, L, 128])
    k.c_rel = I("c_rel", [2, 128, 128])
    k.c_cnt = I("c_cnt", [128, 2])
    k.out = nc.dram_tensor("out", [L, D], F32, kind="ExternalOutput").ap()
    k.mod = S("mod", [2, 2, NM])
    k.xs_tok = S("xs_tok", [T, 4096], BF16)
    k.b_tok = S("b_tok", [T, 1024], BF16)
    k.bT_s = S("bT_s", [8, 128, T], BF16)
    k.cT_s = S("cT_s", [8, 128, T], BF16)
    k.sz_tok = S("sz_tok", [T, 4096], BF16)
    k.y_s = S("y_s", [2, T, 4096])
    k.hres = S("hres", [T, D])
    k.xeT_d = S("xeT_d", [NE, 128, 16, 288], BF16)
    k.ye_d = S("ye_d", [NE, 128, 3, D], BF16)
    k.qT_s = S("qT_s", [128, 8, 2, L], BF16)
    k.kT_s = S("kT_s", [128, 8, 2, L], BF16)
    k.k_tok = S("k_tok", [T, 2048], BF16)
    k.v_tok = S("v_tok", [T, 4096], BF16)
    k.sg_tok = S("sg_tok", [L, 4096], BF16)
    k.o_s = S("o_s", [2, L, 4096])
    with ExitStack() as es:
        k.P = P = Prog(nc, es)
        k.ps = [es.enter_context(nc.psum_tensor(f"ps{i}", [128, 512], F32)) for i in range(8)]
        build_consts(k, es)
        phase_mod(k, 0)
        phase_mod(k, 1)
        P.barrier()
        with ExitStack() as es1:
            dt_tok = sb(nc, es1, "dt_tok", [128, NT, 128], F32)
            dtA_tok = sb(nc, es1, "dtA_tok", [128, NT, 128], F32)
            k.dt_pre = (dt_tok, dtA_tok)
            with ExitStack() as es0:
                aT = sb(nc, es0, "aT", [128, 16, T], BF16)
                phase_norm_T(k, 0, k.x, k.ctx, k.norm_mix_g[0], 1, 0, aT)
                P.barrier()
                phase_ssd_inproj(k, None, aT)
            phase_ssd_scan(k, dt_tok, dtA_tok)
        phase_ssd_out(k, 0, k.x, k.ctx)
        phase_moe(k, 0, True)
        with ExitStack() as es0:
            aT = sb(nc, es0, "aT", [128, 16, T], BF16)
            phase_norm_T(k, 1, k.hres[0:L], k.hres[L:T], k.norm_mix_g[1], 1, 0, aT)
            P.barrier()
            phase_ret_inproj(k, aT)
        phase_ret_scan(k)
        phase_ret_out(k, 1)
        phase_moe(k, 1, False)
        phase_final_norm(k, k.hres, k.out)
        P.wait_all("sp")
        P.run()
    return nc


def kernel(**inputs):
    f = lambda a: np.ascontiguousarray(np.asarray(a, dtype=np.float32))
    nc = build_program()
    iota, jidx, sel = host_moe_consts()
    rope, rel, cnt = host_ret_consts()
    shared = {
        "c_ctx": f(inputs["c_ctx"]), "ada_w": f(inputs["ada_w"]), "ada_b": f(inputs["ada_b"]),
        "norm_mix_g": f(inputs["norm_mix_g"]), "norm_ffn_g": f(inputs["norm_ffn_g"]),
        "ssd_w_in": f(inputs["ssd_w_in"][0]), "ssd_conv_w": f(inputs["ssd_conv_w"][0]),
        "ssd_conv_b": f(inputs["ssd_conv_b"][0]), "ssd_dt_bias": f(inputs["ssd_dt_bias"][0]), "ssd_a_log": f(inputs["ssd_a_log"][0]),
        "ssd_d": f(inputs["ssd_d"][0]), "ssd_norm_g": f(inputs["ssd_norm_g"][0]), "ssd_w_out": f(inputs["ssd_w_out"][0]),
        "ret_w_in": f(inputs["ret_w_in"][0]), "ret_decay": f(inputs["ret_decay"][0]), "ret_gn_g": f(inputs["ret_gn_g"][0]),
        "ret_w_out": f(inputs["ret_w_out"][0]),
        "moe_w_router": f(inputs["moe_w_router"]), "moe_w_gate": f(inputs["moe_w_gate"]), "moe_w_up": f(inputs["moe_w_up"]),
        "moe_w_down": f(inputs["moe_w_down"]),
        "final_norm_g": f(inputs["final_norm_g"]),
        "c_ident": np.eye(128, dtype=np.float32), "c_tri": host_tri(), "c_iota": iota, "c_jidx": jidx, "c_sel": sel,
        "c_rope": rope, "c_rel": rel, "c_cnt": cnt,
    }
    in_maps = []
    for b in range(8):
        m = dict(shared)
        m["x"] = f(inputs["x"][b])
        m["ctx"] = f(inputs["ctx"][b])
        m["c"] = f(inputs["c"][b])
        in_maps.append(m)
    res = run_bass_kernel_spmd(nc, in_maps, core_ids=list(range(8)))
    return np.stack([np.asarray(r["out"], dtype=np.float32) for r in res.results], axis=0)
```
